# Optimizing a Trainium2 kernel written in Bass

```python
import math
import jax, jax.numpy as jnp
from jax import lax
import numpy as np

D_MODEL = 2048
BATCH = 2
SEQ = 16384
DEPTH = 1

HEAD_DIM = 128
N_HEADS_A = 8
N_HEADS_B = 8
MIX_WIDTH = (N_HEADS_A + N_HEADS_B) * HEAD_DIM
KV_RANK = 256
IDX_HEADS = 16
IDX_DIM = 64
TOPK_MAX = 256
N_BUCKETS = 32
MAX_DISTANCE = 128
D_FF = 5632
CONV_WIDTH = 3
Q_BLOCK = 128
EPS = 1e-6

COL_SIZES = (
    N_HEADS_A * HEAD_DIM,
    KV_RANK,
    IDX_HEADS * IDX_DIM,
    IDX_DIM,
    IDX_HEADS,
    N_HEADS_B * HEAD_DIM,
    N_HEADS_B * HEAD_DIM,
    N_HEADS_B * HEAD_DIM,
    N_HEADS_B,
)
D_IN = sum(COL_SIZES)

kernel_name = 'hymba_dsa_fox_convffn_layer'


def rmsnorm(x, g):
    xf = x.astype(jnp.float32)
    y = xf * lax.rsqrt(jnp.mean(xf * xf, axis=-1, keepdims=True) + EPS)
    return (y * g.astype(jnp.float32)).astype(x.dtype)


def t5_bucket(n):
    max_exact = N_BUCKETS // 2
    nf = jnp.maximum(n, 1).astype(jnp.float32)
    large = max_exact + (jnp.log(nf / max_exact) / math.log(MAX_DISTANCE / max_exact)
                         * (N_BUCKETS - max_exact)).astype(jnp.int32)
    large = jnp.minimum(large, N_BUCKETS - 1)
    return jnp.where(n < max_exact, n, large)


def to_blocks(a):
    b, l = a.shape[0], a.shape[1]
    a = a.reshape((b, l // Q_BLOCK, Q_BLOCK) + a.shape[2:])
    return jnp.moveaxis(a, 1, 0)


def from_blocks(a):
    a = jnp.moveaxis(a, 0, 1)
    return a.reshape((a.shape[0], a.shape[1] * a.shape[2]) + a.shape[3:])


def dsa_mixer(q, c_kv, q_idx, k_idx, w_idx, w_uk, w_uv, t5_table):
    L = q.shape[1]
    nb = L // Q_BLOCK
    topk = min(TOPK_MAX, L // 4)
    key_pos = jnp.arange(L)
    q_lat = jnp.einsum('bthd,hdr->bthr', q, w_uk)

    def block(args):
        blk, ql, qi, wi = args
        t_pos = blk * Q_BLOCK + jnp.arange(Q_BLOCK)
        causal = key_pos[None, :] <= t_pos[:, None]
        dots = jnp.einsum('bthd,bsd->bths', qi, k_idx).astype(jnp.float32) * (IDX_DIM ** -0.5)
        score = jnp.einsum('bth,bths->bts', wi.astype(jnp.float32) * (IDX_HEADS ** -0.5),
                           jax.nn.relu(dots))
        score = jnp.where(causal[None], score, -jnp.inf)
        _, sel = lax.top_k(score, topk)
        c_sel = jax.vmap(lambda c, i: c[i])(c_kv, sel)
        dist = t_pos[None, :, None] - sel
        valid = dist >= 0
        bias = t5_table[t5_bucket(jnp.maximum(dist, 0))]
        logits = (jnp.einsum('bthr,btkr->bthk', ql, c_sel).astype(jnp.float32) * (HEAD_DIM ** -0.5)
                  + jnp.moveaxis(bias, -1, 2).astype(jnp.float32))
        logits = jnp.where(valid[:, :, None, :], logits, -jnp.inf)
        p = jax.nn.softmax(logits, axis=-1).astype(c_sel.dtype)
        return jnp.einsum('bthk,btkr->bthr', p, c_sel)

    o_lat = from_blocks(lax.map(block, (jnp.arange(nb), to_blocks(q_lat),
                                        to_blocks(q_idx), to_blocks(w_idx))))
    return jnp.einsum('bthr,hrd->bthd', o_lat, w_uv)


def fox_mixer(q, k, v, log_f):
    L = q.shape[1]
    nb = L // Q_BLOCK
    key_pos = jnp.arange(L)
    cum = jnp.cumsum(log_f.astype(jnp.float32), axis=1)
    cum_k = jnp.moveaxis(cum, 1, 2)

    def block(args):
        blk, qb, cq = args
        t_pos = blk * Q_BLOCK + jnp.arange(Q_BLOCK)
        causal = key_pos[None, :] <= t_pos[:, None]
        decay = jnp.moveaxis(cq, 1, 2)[..., None] - cum_k[:, :, None, :]
        logits = jnp.einsum('bthd,bshd->bhts', qb, k).astype(jnp.float32) * (HEAD_DIM ** -0.5) + decay
        logits = jnp.where(causal[None, None], logits, -jnp.inf)
        p = jax.nn.softmax(logits, axis=-1).astype(v.dtype)
        return jnp.einsum('bhts,bshd->bthd', p, v)

    return from_blocks(lax.map(block, (jnp.arange(nb), to_blocks(q), to_blocks(cum))))


def conv_ffn(h, w_up, conv_w, conv_b, w_down):
    L = h.shape[1]
    u = h @ w_up
    up = jnp.pad(u, ((0, 0), (CONV_WIDTH - 1, 0), (0, 0)))
    acc = conv_b
    for j in range(CONV_WIDTH):
        acc = acc + conv_w[j] * up[:, j:j + L]
    gate, val = jnp.split(acc, 2, axis=-1)
    return (jax.nn.silu(gate) * val) @ w_down


def setup_inputs(seed: int = 0) -> dict:
    key = jax.random.key(seed)
    ks = jax.random.split(key, 15)
    f32 = jnp.float32
    n = lambda k, s: jax.random.normal(k, s, f32)
    return {
        'x': n(ks[0], (BATCH, SEQ, D_MODEL)),
        'w_in': n(ks[1], (DEPTH, D_MODEL, D_IN)) * D_MODEL ** -0.5,
        'kv_norm_g': 1.0 + 0.02 * n(ks[2], (DEPTH, KV_RANK)),
        'w_uk': n(ks[3], (DEPTH, N_HEADS_A, HEAD_DIM, KV_RANK)) * KV_RANK ** -0.5,
        'w_uv': n(ks[4], (DEPTH, N_HEADS_A, KV_RANK, HEAD_DIM)) * KV_RANK ** -0.5,
        't5_table': 0.5 * n(ks[5], (N_BUCKETS, N_HEADS_A)),
        'fgate_b': 3.0 + 0.1 * n(ks[6], (DEPTH, N_HEADS_B)),
        'w_o': n(ks[7], (DEPTH, MIX_WIDTH, D_MODEL)) * MIX_WIDTH ** -0.5,
        'norm1_g': 1.0 + 0.02 * n(ks[8], (DEPTH, D_MODEL)),
        'norm2_g': 1.0 + 0.02 * n(ks[9], (DEPTH, D_MODEL)),
        'w_up': n(ks[10], (DEPTH, D_MODEL, 2 * D_FF)) * D_MODEL ** -0.5,
        'conv_w': 0.5 * n(ks[11], (DEPTH, CONV_WIDTH, 2 * D_FF)),
        'conv_b': 0.02 * n(ks[12], (DEPTH, 2 * D_FF)),
        'w_down': n(ks[13], (DEPTH, D_FF, D_MODEL)) * D_FF ** -0.5,
        'final_g': 1.0 + 0.02 * n(ks[14], (D_MODEL,)),
    }


def reference(x, w_in, kv_norm_g, w_uk, w_uv, t5_table, fgate_b, w_o, norm1_g, norm2_g,
              w_up, conv_w, conv_b, w_down, final_g):
    B, L, _ = x.shape
    split_points = [int(s) for s in np.cumsum(COL_SIZES)[:-1]]
    for l in range(DEPTH):
        h = rmsnorm(x, norm1_g[l])
        proj = h @ w_in[l]
        aq, akv, aiq, aik, aiw, bq, bk, bv, bf = jnp.split(proj, split_points, axis=-1)
        c_kv = rmsnorm(akv, kv_norm_g[l])
        o_a = dsa_mixer(aq.reshape(B, L, N_HEADS_A, HEAD_DIM), c_kv,
                        aiq.reshape(B, L, IDX_HEADS, IDX_DIM), aik, aiw,
                        w_uk[l], w_uv[l], t5_table)
        log_f = jax.nn.log_sigmoid(bf.astype(jnp.float32) + fgate_b[l].astype(jnp.float32))
        o_b = fox_mixer(bq.reshape(B, L, N_HEADS_B, HEAD_DIM),
                        bk.reshape(B, L, N_HEADS_B, HEAD_DIM),
                        bv.reshape(B, L, N_HEADS_B, HEAD_DIM), log_f)
        mix = jnp.concatenate([o_a.reshape(B, L, -1), o_b.reshape(B, L, -1)], axis=-1)
        x = x + mix @ w_o[l]
        x = x + conv_ffn(rmsnorm(x, norm2_g[l]), w_up[l], conv_w[l], conv_b[l], w_down[l])
    return rmsnorm(x, final_g)
```

```python
import math
from contextlib import ExitStack
import numpy as np
import ml_dtypes
import concourse.bass as bass
import concourse.mybir as mybir
from concourse.bass_utils import run_bass_kernel_spmd

F32 = mybir.dt.float32
BF16 = mybir.dt.bfloat16
AF = mybir.ActivationFunctionType
ALU = mybir.AluOpType
AX = mybir.AxisListType
ENG = ['pe', 'act', 'dve', 'pool', 'sp']

D = 2048
KT = 16
DFF = 5632
NFB = 44
TOPK = 256
NEGBIG = -30000.0
SCALE = 128.0 ** -0.5
C_AQ, C_AKV, C_AIQ, C_AIK, C_AIW, C_BQ, C_BK, C_BV, C_BF = 0, 1024, 1280, 2304, 2368, 2384, 3408, 4432, 5456
DEBUG = False


def geom(L, NT):
    NKB = L // 128
    NCH = L // 512
    g = {}
    g['nch'] = [min(NCH, (2040 * m + 2040 + 511) // 512) for m in range(NT)]
    g['nkb'] = [4 * c for c in g['nch']]
    g['kb0'] = [max(0, (2040 * m - 257) // 128 + 1) for m in range(NT)]
    g['nu'] = [g['nkb'][m] - g['kb0'][m] for m in range(NT)]
    g['NU'] = max(max(g['nu']), 1)
    g['c0'] = [[min(g['nch'][m], max(0, (2040 * m + 128 * gg - 513) // 512 + 1)) for gg in range(4)] for m in range(NT)]
    g['NMC'] = max(1, max(g['nch'][m] - g['c0'][m][gg] for m in range(NT) for gg in range(4)))
    return g


class Prog:
    def __init__(self, nc, es):
        self.nc, self.es = nc, es
        self.q = {e: [] for e in ENG}
        self.cnt, self.sems = {}, {}
        self.waited = {e: {} for e in ENG}
        self.bufs = {}
        for e in ENG[:4]:
            self._newsem(e)

    def _newsem(self, name):
        self.sems[name] = self.es.enter_context(self.nc.semaphore(name))
        self.cnt[name] = 0

    def _emit(self, eng, fn, reads, writes, semname, inc):
        deps = []
        for k in reads:
            b = self.bufs.get(k)
            if b and b[0]:
                deps.append(b[0])
        for k in writes:
            b = self.bufs.get(k)
            if b:
                if b[0]:
                    deps.append(b[0])
                deps.extend(b[1])
        for (sn, val, peng) in deps:
            if peng == 'pe' and eng == 'pe':
                continue
            if self.waited[eng].get(sn, 0) >= val:
                continue
            self.waited[eng][sn] = val
            self.q[eng].append(('w', sn, val))
        self.cnt[semname] += inc
        tk = (semname, self.cnt[semname], eng)
        self.q[eng].append(('o', fn, semname, inc))
        for k in reads:
            self.bufs.setdefault(k, [None, []])[1].append(tk)
        for k in writes:
            self.bufs[k] = [tk, []]
        return tk

    def op(self, eng, fn, reads=(), writes=()):
        return self._emit(eng, fn, list(reads), list(writes), eng, 1)

    def dma(self, q, fn, reads, writes, sem):
        if sem not in self.sems:
            self._newsem(sem)
        return self._emit(q, fn, list(reads), list(writes), sem, 16)

    def flush(self):
        for e in ENG:
            for sn, v in self.cnt.items():
                if v > 0 and self.waited[e].get(sn, 0) < v:
                    self.waited[e][sn] = v
                    self.q[e].append(('w', sn, v))
        qs = self.q
        sems = self.sems

        def mk(e):
            def body(eng):
                for it in qs[e]:
                    if it[0] == 'w':
                        eng.wait_ge(sems[it[1]], it[2])
                    else:
                        it[1](eng).then_inc(sems[it[2]], it[3])
            return body
        with self.nc.Block() as block:
            block.tensor(mk('pe'))
            block.scalar(mk('act'))
            block.vector(mk('dve'))
            block.gpsimd(mk('pool'))
            block.sync(mk('sp'))
        self.q = {e: [] for e in ENG}
        self.bufs = {}


def build(L, NT):
    G = geom(L, NT)
    NKB, NCH = L // 128, L // 512
    NU, NMC = G['NU'], G['NMC']
    nc = bass.Bass("TRN2", target_bir_lowering=False)
    es = ExitStack()

    def din(name, shape, dt=F32):
        return nc.dram_tensor(name, shape, dt, kind="ExternalInput").ap()

    def dsc(name, shape, dt=BF16):
        return nc.dram_tensor(name, shape, dt, kind=("ExternalOutput" if DEBUG else "Internal")).ap()

    xk = din("xk", [L, D]); xq = din("xq", [NT * 512, D])
    w_in = din("w_in", [D, 5464]); w_o = din("w_o", [D, D]); w_up = din("w_up", [D, 2 * DFF]); w_down = din("w_down", [DFF, D])
    w_uk = din("w_uk", [8, 128, 256]); w_uv = din("w_uv", [8, 256, 128])
    g1T = din("g1T", [128, 16]); g2T = din("g2T", [128, 16]); gkvb = din("gkvb", [128, 256]); fgb = din("fgb", [8, 1])
    convw = din("convw", [128, 88, 3]); convb = din("convb", [128, 88]); gfb = din("gfb", [128, D]); b31 = din("b31", [128, 8])
    identb = din("identb", [128, 128], BF16); antib = din("antib", [128, 128], BF16)
    identf = din("identf", [128, 128]); onesf = din("onesf", [128, 128])
    hank_fox = din("hank_fox", [NT, NU, 640], BF16); hank_t5 = din("hank_t5", [NT, NU, 8, 640])
    hank_idx = din("hank_idx", [NT, 4, NMC, 640], BF16); ohk = din("ohk", [NT, 128, NKB * 8])
    out = nc.dram_tensor("out", [NT * 512, D], F32, kind="ExternalOutput").ap()

    WB128 = dsc("wb128", [35, 128, 16, 128]); WB256 = dsc("wb256", [13, 128, 16, 256])
    WBUP = dsc("wbup", [88, 128, 16, 128]); WBDN = dsc("wbdn", [DFF, D])
    WUK = dsc("wukb", [8, 128, 256]); WUV = dsc("wuvb", [8, 256, 128])
    KTs = dsc("kts", [8, 128, L]); VAs = dsc("vas", [8, L, 130]); CTs = dsc("cts", [2, 128, L]); CAs = dsc("cas", [L, 258])
    KITs = dsc("kits", [128, L])
    BQTs = dsc("bqts", [8, 128, 512]); QLTs = dsc("qlts", [8, 128, 2, 512]); QITs = dsc("qits", [8, 128, 512])
    AIWs = dsc("aiws", [512, 16], F32)
    NSs = dsc("nss", [4, 128, L]); MIXs = dsc("mixs", [16, 128, 512]); XMs = dsc("xms", [512, D], F32)
    H2s = dsc("h2s", [128, 16, 512])

    P = Prog(nc, es)

    def sbg(name, shape, dt):
        return es.enter_context(nc.sbuf_tensor(name, shape, dt))

    IDB = sbg("IDB", [128, 128], BF16); ANB = sbg("ANB", [128, 128], BF16)
    IDF = sbg("IDF", [128, 128], F32); ONF = sbg("ONF", [128, 128], F32)
    G1B = sbg("G1B", [128, 16, 128], BF16); G2B = sbg("G2B", [128, 16, 128], BF16)
    GT = sbg("GT", [128, 32], F32); GKV = sbg("GKV", [128, 256], F32); FGB = sbg("FGB", [8, 1], F32)
    CW = sbg("CW", [128, 88, 3], F32); CB = sbg("CB", [128, 88], F32); B31 = sbg("B31", [128, 8], F32)
    CONST = sbg("CONST", [128, 8], F32)
    ONB = sbg("ONB", [128, 128], BF16)
    CUMK = sbg("CUMK", [128, NKB, 8], F32)
    ZERO, ONE, EPS, TINY = CONST[:, 0:1], CONST[:, 1:2], CONST[:, 2:3], CONST[:, 3:4]

    def mm(out_, lhsT, rhs, st, sp, r, w, skip=False):
        P.op('pe', lambda e: e.matmul(out_, lhsT=lhsT, rhs=rhs, start=st, stop=sp, skip_group_check=skip), r, w)

    def trb(out_, in_, r, w):
        P.op('pe', lambda e: e.transpose(out_, in_, IDB[:]), r, w)

    def act(out_, in_, func, r, w, bias=None, scale=None, accum=None):
        kw = {}
        if bias is not None:
            kw['bias'] = bias
        if scale is not None:
            kw['scale'] = scale
        if accum is not None:
            kw['accum_out'] = accum
        P.op('act', lambda e: e.activation(out=out_, in_=in_, func=func, **kw), r, w)

    def dma(q, out_, in_, r, w, sem):
        P.dma(q, lambda e: e.dma_start(out=out_, in_=in_), r, w, sem)

    def vop(eng, name, r, w, *a, **k):
        P.op(eng, lambda e: getattr(e, name)(*a, **k), r, w)

    def stage_consts():
        dma('sp', IDB[:], identb, [], ['C'], 'c0'); dma('sp', ANB[:], antib, [], ['C'], 'c0')
        dma('sp', IDF[:], identf, [], ['C'], 'c0'); dma('sp', ONF[:], onesf, [], ['C'], 'c0')
        dma('sp', GT[:, 0:16], g1T, [], ['C'], 'c0'); dma('sp', GT[:, 16:32], g2T, [], ['C'], 'c0')
        dma('sp', GKV[:], gkvb, [], ['C'], 'c0'); dma('sp', FGB[:], fgb, [], ['C'], 'c0')
        dma('sp', CW[:], convw, [], ['C'], 'c0'); dma('sp', CB[:], convb, [], ['C'], 'c0'); dma('sp', B31[:], b31, [], ['C'], 'c0')
        for i, v in enumerate([0.0, 1.0, 1e-6, 1e-18, -1.0]):
            vop('dve', 'memset', [], ['C2'], CONST[:, i:i + 1], v)
        vop('dve', 'memset', [], ['C2'], ONB[:], 1.0)
        for k in range(16):
            vop('dve', 'tensor_scalar', ['C', 'C2'], ['C3'], out=G1B[:, k, :], in0=ONB[:], scalar1=GT[:, k:k + 1], scalar2=None, op0=ALU.mult)
            vop('dve', 'tensor_scalar', ['C', 'C2'], ['C3'], out=G2B[:, k, :], in0=ONB[:], scalar1=GT[:, 16 + k:17 + k], scalar2=None, op0=ALU.mult)
        vop('dve', 'tensor_scalar', ['C'], ['C4'], out=FGB[:], in0=FGB[:], scalar1=-1.0, scalar2=None, op0=ALU.mult)
        win3 = w_in.rearrange("(k p) c -> p k c", p=128)

        def cvt(dst, src):
            dma('pool', dst, src, [], ['W'], 'wc')
        blk = 0
        for (c0, n, w) in [(C_AQ, 8, 128), (C_AIQ, 8, 128)]:
            for i in range(n):
                cvt(WB128[blk], win3[:, :, c0 + i * w:c0 + (i + 1) * w]); blk += 1
        cvt(WB128[16][:, :, 0:64], win3[:, :, C_AIK:C_AIK + 64]); cvt(WB128[16][:, :, 64:128], win3[:, :, C_AIK:C_AIK + 64])
        blk = 17
        for (c0, n, w) in [(C_BQ, 8, 128), (C_BK, 8, 128)]:
            for i in range(n):
                cvt(WB128[blk], win3[:, :, c0 + i * w:c0 + (i + 1) * w]); blk += 1
        cvt(WB128[33][:, :, 0:8], win3[:, :, C_BF:C_BF + 8])
        cvt(WB128[34][:, :, 0:16], win3[:, :, C_AIW:C_AIW + 16])
        cvt(WB256[0], win3[:, :, C_AKV:C_AKV + 256])
        for i in range(4):
            cvt(WB256[1 + i], win3[:, :, C_BV + i * 256:C_BV + (i + 1) * 256])
        wo3 = w_o.rearrange("(k p) c -> p k c", p=128)
        for i in range(8):
            cvt(WB256[5 + i], wo3[:, :, i * 256:(i + 1) * 256])
        wup3 = w_up.rearrange("(k p) c -> p k c", p=128)
        for i in range(88):
            cvt(WBUP[i], wup3[:, :, i * 128:(i + 1) * 128])
        for i in range(NFB):
            cvt(WBDN[i * 128:(i + 1) * 128, :], w_down[i * 128:(i + 1) * 128, :])
        for h in range(8):
            cvt(WUK[h], w_uk[h]); cvt(WUV[h], w_uv[h])
        P.flush()

    def norm_T(st, src_fn, GB, hT, hkey, tag):
        XS, XB, ST, PB = st['XS'], st['XB'], st['ST'], st['PB']
        PBb = PB[:].bitcast(BF16)
        for sub in range(4):
            s2 = sub % 2
            xs = XS[:, s2, :]
            dma('sp', xs, src_fn(sub), [], ['XS%d' % s2], 'xs%d' % s2)
            act(XB[:], xs, AF.Square, ['XS%d' % s2], ['XB', 'SSQ'], accum=ST[:, 0:1])
            act(ST[:, 1:2], ST[:, 0:1], AF.Sqrt, ['SSQ', 'C2'], ['SQ'], bias=EPS, scale=1.0 / D)
            vop('dve', 'reciprocal', ['SQ'], ['RSTD'], out=ST[:, 2:3], in_=ST[:, 1:2])
            act(XB[:], xs, AF.Copy, ['XS%d' % s2, 'RSTD'], ['XB'], scale=ST[:, 2:3])
            for k in range(16):
                trb(PBb[:, k // 8, (k % 8) * 128:(k % 8 + 1) * 128], XB[:, k * 128:(k + 1) * 128], ['XB', 'C'], ['PB'])
            vop('dve', 'tensor_tensor', ['PB', 'C3'], [hkey], out=hT[:, :, sub * 128:(sub + 1) * 128],
                in0=PBb.rearrange("p a (k c) -> p (a k) c", c=128), in1=GB[:], op=ALU.mult)

    uniq = [0]

    def alloc(stack, name, shape, dt):
        uniq[0] += 1
        return stack.enter_context(nc.sbuf_tensor("%s_%d" % (name, uniq[0]), shape, dt))

    def palloc(stack, name, shape, dt=F32):
        uniq[0] += 1
        return stack.enter_context(nc.psum_tensor("%s_%d" % (name, uniq[0]), shape, dt))

    def stage_K():
        with ExitStack() as s:
            st = dict(XS=alloc(s, "kXS", [128, 2, D], F32), XB=alloc(s, "kXB", [128, D], BF16), ST=alloc(s, "kST", [128, 8], F32),
                      PB=palloc(s, "kPB", [128, 2, 512]))
            PA = palloc(s, "kPA", [128, 2, 512]); PC = palloc(s, "kPC", [128, 2, 512])
            HT = alloc(s, "kHT", [128, 2, 16, 512], BF16)
            W1 = alloc(s, "kW1", [128, 3, 16, 128], BF16); W2 = alloc(s, "kW2", [128, 2, 16, 256], BF16)
            SK = alloc(s, "kSK", [128, 2, 512], BF16); SV = alloc(s, "kSV", [128, 2, 8, 130], BF16)
            SCA = alloc(s, "kSCA", [128, 2, 258], BF16); SCT = alloc(s, "kSCT", [128, 2, 2, 512], BF16)
            AK = alloc(s, "kAK", [128, 256], F32); JK = alloc(s, "kJK", [128, 256], BF16); ST2 = alloc(s, "kST2", [128, 8], F32)
            LF = alloc(s, "kLF", [8, 2, 512], F32); CUMR = alloc(s, "kCUMR", [8, 2, 512], F32); ONES8 = alloc(s, "kON8", [8, 512], F32)
            vop('dve', 'memset', [], ['ON8'], ONES8[:], 1.0)
            for sl in range(2):
                vop('dve', 'memset', [], ['SV%d' % sl], SV[:, sl, :, 128:130], 1.0)
                vop('dve', 'memset', [], ['SCA%d' % sl], SCA[:, sl, 256:258], 1.0)
            cnt = {'w1': 0, 'w2': 0, 'pa': 0, 'pc': 0, 'sk': 0, 'sv': 0, 'sca': 0}

            def nxt(k, n):
                v = cnt[k] % n
                cnt[k] += 1
                return v

            def loadw1(blk):
                sl = nxt('w1', 3)
                dma('sp', W1[:, sl], WB128[blk], [], ['W1_%d' % sl], 'w1_%d' % sl)
                return sl

            def loadw2(blk):
                sl = nxt('w2', 2)
                dma('sp', W2[:, sl], WB256[blk], [], ['W2_%d' % sl], 'w2_%d' % sl)
                return sl
            for i in range(NCH):
                hs = i % 2
                hT = HT[:, hs]
                hk = 'HT%d' % hs
                norm_T(st, lambda sub: xk[i * 512 + sub * 128:i * 512 + (sub + 1) * 128, :], G1B, hT, hk, 'k')
                for blk, kind, h in [(25 + h, 'k', h) for h in range(8)] + [(16, 'ki', 0)]:
                    ws = loadw1(blk)
                    pa = nxt('pa', 2)
                    for k in range(16):
                        mm(PA[:, pa, :], W1[:, ws, k, :], hT[:, k, :], k == 0, k == 15, ['W1_%d' % ws, hk], ['PA%d' % pa])
                    sk = nxt('sk', 2)
                    act(SK[:, sk, :], PA[:, pa, :], AF.Copy, ['PA%d' % pa], ['SK%d' % sk])
                    dst = KTs[h][:, i * 512:(i + 1) * 512] if kind == 'k' else KITs[:, i * 512:(i + 1) * 512]
                    dma('sp', dst, SK[:, sk, :], ['SK%d' % sk], [], 'sko%d' % sk)
                ws = loadw1(33)
                pa = nxt('pa', 2)
                for k in range(16):
                    mm(PA[0:8, pa, :], W1[:, ws, k, 0:8], hT[:, k, :], k == 0, k == 15, ['W1_%d' % ws, hk], ['PA%d' % pa])
                cs = i % 2
                act(LF[:, cs, :], PA[0:8, pa, :], AF.Exp, ['PA%d' % pa, 'C4'], ['LF%d' % cs], bias=FGB[:, 0:1], scale=-1.0)
                act(LF[:, cs, :], LF[:, cs, :], AF.Ln, ['LF%d' % cs, 'C2'], ['LF%d' % cs], bias=ONE[0:8, :])
                init = 0.0 if i == 0 else CUMR[:, 1 - cs, 511:512]
                vop('dve', 'tensor_tensor_scan', ['LF%d' % cs, 'ON8', 'CR%d' % (1 - cs)], ['CR%d' % cs], out=CUMR[:, cs, :], data0=ONES8[:], data1=LF[:, cs, :],
                    initial=init, op0=ALU.mult, op1=ALU.subtract)
                pc = nxt('pc', 2)
                for j in range(4):
                    P.op('pe', (lambda j=j, pc=pc, cs=cs: (lambda e: e.transpose(PC[:, pc, j * 8:(j + 1) * 8], CUMR[:, cs, j * 128:(j + 1) * 128], IDF[0:8, 0:8])))(),
                         ['CR%d' % cs, 'C'], ['PC%d' % pc])
                vop('dve', 'tensor_copy', ['PC%d' % pc], ['CUMK'], out=CUMK[:, i * 4:(i + 1) * 4, :], in_=PC[:, pc, 0:32].rearrange("p (j h) -> p j h", h=8))
                for sub in range(4):
                    svs = nxt('sv', 2)
                    for vb in range(4):
                        ws = loadw2(1 + vb)
                        pa = nxt('pa', 2)
                        for k in range(16):
                            mm(PA[:, pa, 0:256], hT[:, k, sub * 128:(sub + 1) * 128], W2[:, ws, k, :], k == 0, k == 15, ['W2_%d' % ws, hk], ['PA%d' % pa])
                        act(SV[:, svs, 2 * vb:2 * vb + 2, 0:128], PA[:, pa, 0:256].rearrange("p (h c) -> p h c", c=128), AF.Copy, ['PA%d' % pa], ['SV%d' % svs])
                    dma('sp', VAs[:, i * 512 + sub * 128:i * 512 + (sub + 1) * 128, :].rearrange("h p c -> p h c"), SV[:, svs], ['SV%d' % svs], [], 'svo%d' % svs)
                    ws = loadw2(0)
                    pa = nxt('pa', 2)
                    for k in range(16):
                        mm(PA[:, pa, 0:256], hT[:, k, sub * 128:(sub + 1) * 128], W2[:, ws, k, :], k == 0, k == 15, ['W2_%d' % ws, hk], ['PA%d' % pa])
                    act(JK[:], PA[:, pa, 0:256], AF.Square, ['PA%d' % pa], ['JK', 'S2a'], accum=ST2[:, 0:1])
                    act(ST2[:, 1:2], ST2[:, 0:1], AF.Sqrt, ['S2a', 'C2'], ['S2b'], bias=EPS, scale=1.0 / 256)
                    vop('dve', 'reciprocal', ['S2b'], ['S2c'], out=ST2[:, 2:3], in_=ST2[:, 1:2])
                    cas = nxt('sca', 2)
                    vop('dve', 'scalar_tensor_tensor', ['PA%d' % pa, 'S2c', 'C'], ['SCA%d' % cas], out=SCA[:, cas, 0:256], in0=PA[:, pa, 0:256], scalar=ST2[:, 2:3],
                        in1=GKV[:], op0=ALU.mult, op1=ALU.mult)
                    dma('sp', CAs[i * 512 + sub * 128:i * 512 + (sub + 1) * 128, :], SCA[:, cas, :], ['SCA%d' % cas], [], 'cao%d' % cas)
                    PCb = PC[:].bitcast(BF16)
                    pc = nxt('pc', 2)
                    for a in range(2):
                        trb(PCb[:, pc, a * 128:(a + 1) * 128], SCA[:, cas, a * 128:(a + 1) * 128], ['SCA%d' % cas, 'C'], ['PC%d' % pc])
                    act(SCT[:, hs, :, sub * 128:(sub + 1) * 128], PCb[:, pc, 0:256].rearrange("p (a c) -> p a c", c=128), AF.Copy, ['PC%d' % pc], ['SCT%d' % hs])
                dma('sp', CTs[:, :, i * 512:(i + 1) * 512].rearrange("a p c -> p a c"), SCT[:, hs], ['SCT%d' % hs], [], 'cto%d' % hs)
            P.flush()


    def hank(t, off):
        return bass.AP(tensor=t.tensor, offset=off, ap=[[1, 128], [1, 512]])

    class Ctr:
        def __init__(self):
            self.c = {}

        def __call__(self, k, n):
            v = self.c.get(k, 0)
            self.c[k] = v + 1
            return v % n

    def stage_Q(m):
        with ExitStack() as s:
            st = dict(XS=alloc(s, "qXS", [128, 2, D], F32), XB=alloc(s, "qXB", [128, D], BF16), ST=alloc(s, "qST", [128, 8], F32),
                      PB=palloc(s, "qPB", [128, 2, 512]))
            PA = palloc(s, "qPA", [128, 2, 512]); PC = palloc(s, "qPC", [128, 2, 512])
            HT = alloc(s, "qHT", [128, 16, 512], BF16)
            W1 = alloc(s, "qW1", [128, 3, 16, 128], BF16)
            SK = alloc(s, "qSK", [128, 2, 512], BF16); AQ = alloc(s, "qAQ", [128, 2, 512], BF16)
            WK = alloc(s, "qWK", [128, 2, 256], BF16); QLS = alloc(s, "qQLS", [128, 2, 2, 512], BF16)
            AW = alloc(s, "qAW", [128, 2, 16], F32)
            nx = Ctr()
            norm_T(st, lambda sub: xq[m * 512 + sub * 128:m * 512 + (sub + 1) * 128, :], G1B, HT, 'HT', 'q')

            def proj(blk):
                ws = nx('w1', 3)
                dma('sp', W1[:, ws], WB128[blk], [], ['W1_%d' % ws], 'w1_%d' % ws)
                pa = nx('pa', 2)
                for k in range(16):
                    mm(PA[:, pa, :], W1[:, ws, k, :], HT[:, k, :], k == 0, k == 15, ['W1_%d' % ws, 'HT'], ['PA%d' % pa])
                return pa
            for h in range(8):
                pa = proj(17 + h)
                sk = nx('sk', 2)
                act(SK[:, sk, :], PA[:, pa, :], AF.Copy, ['PA%d' % pa], ['SK%d' % sk])
                dma('sp', BQTs[h], SK[:, sk, :], ['SK%d' % sk], [], 'sko%d' % sk)
            for hp in range(8):
                pa = proj(8 + hp)
                sk = nx('sk', 2)
                act(SK[:, sk, :], PA[:, pa, :], AF.Copy, ['PA%d' % pa], ['SK%d' % sk])
                dma('sp', QITs[hp], SK[:, sk, :], ['SK%d' % sk], [], 'sko%d' % sk)
            for h in range(8):
                pa = proj(h)
                aq = nx('aq', 2)
                act(AQ[:, aq, :], PA[:, pa, :], AF.Copy, ['PA%d' % pa], ['AQ%d' % aq])
                wk = nx('wk', 2)
                dma('sp', WK[:, wk, :], WUK[h], [], ['WK%d' % wk], 'wk%d' % wk)
                qs = nx('ql', 2)
                for a in range(2):
                    pc = nx('pc', 2)
                    mm(PC[:, pc, :], WK[:, wk, a * 128:(a + 1) * 128], AQ[:, aq, :], True, True, ['WK%d' % wk, 'AQ%d' % aq], ['PC%d' % pc])
                    act(QLS[:, qs, a, :], PC[:, pc, :], AF.Copy, ['PC%d' % pc], ['QLS%d' % qs])
                dma('sp', QLTs[h], QLS[:, qs], ['QLS%d' % qs], [], 'qlo%d' % qs)
            ws = nx('w1', 3)
            dma('sp', W1[:, ws], WB128[34], [], ['W1_%d' % ws], 'w1_%d' % ws)
            for sub in range(4):
                pa = nx('pa', 2)
                for k in range(16):
                    mm(PA[:, pa, 0:16], HT[:, k, sub * 128:(sub + 1) * 128], W1[:, ws, k, 0:16], k == 0, k == 15, ['W1_%d' % ws, 'HT'], ['PA%d' % pa])
                aw = nx('aw', 2)
                act(AW[:, aw, :], PA[:, pa, 0:16], AF.Copy, ['PA%d' % pa], ['AW%d' % aw])
                dma('sp', AIWs[sub * 128:(sub + 1) * 128, :], AW[:, aw, :], ['AW%d' % aw], [], 'awo%d' % aw)
            P.flush()

    def stage_IF(m):
        nch, nkb, kb0 = G['nch'][m], G['nkb'][m], G['kb0'][m]
        n = nch * 512
        with ExitStack() as s:
            SC = alloc(s, "iSC", [128, L], F32)
            QI = alloc(s, "iQI", [128, 8, 512], BF16); AWt = alloc(s, "iAW", [128, 4, 16], F32)
            DW = alloc(s, "iDW", [128, 2, 16, 128], BF16); KI = alloc(s, "iKI", [128, 3, 512], BF16)
            RR = alloc(s, "iRR", [128, 4, 512], BF16); HI = alloc(s, "iHI", [128, 2, 512], BF16)
            M8 = alloc(s, "iM8", [128, 8], F32); NSB = alloc(s, "iNSB", [128, 2, 2048], BF16)
            BQ = alloc(s, "fBQ", [128, 2, 512], BF16); KTc = alloc(s, "fKT", [128, 2, 512], BF16)
            VAc = alloc(s, "fVA", [128, 2, 4, 130], BF16); PTB = alloc(s, "fPT", [128, 3, 512], BF16)
            HF = alloc(s, "fHF", [128, 2, 512], BF16); FB = alloc(s, "fFB", [128, 8, NKB], F32)
            OHs = alloc(s, "fOH", [128, NKB * 8], F32); TMP = alloc(s, "fTMP", [128, NKB * 8], F32)
            CR = alloc(s, "fCR", [128, 16], F32); OL = alloc(s, "fOL", [128, 2, 128], BF16)
            MX = alloc(s, "fMX", [128, 2, 512], BF16); LD = alloc(s, "fLD", [128, 4], F32)
            PD = palloc(s, "iPD", [128, 2, 512]); PSc = palloc(s, "iPS", [128, 2, 512]); ACC = palloc(s, "iACC", [128, 4, 512])
            PSb = PSc[:].bitcast(BF16)
            nx = Ctr()
            dma('sp', QI[:], QITs.rearrange("h p c -> p h c"), [], ['QI'], 'qi')
            dma('sp', AWt[:], AIWs.rearrange("(g p) c -> p g c", p=128), [], ['AWt'], 'awt')
            dma('sp', OHs[:], ohk[m], [], ['OH'], 'oh')
            vop('dve', 'tensor_tensor', ['OH'], ['TMP'], out=TMP[:], in0=CUMK[:].rearrange("p k h -> p (k h)"), in1=OHs[:], op=ALU.mult)
            vop('dve', 'tensor_reduce', ['TMP'], ['CR0'], out=CR[:, 0:8], in_=TMP[:].rearrange("p (k h) -> p h k", h=8), axis=AX.X, op=ALU.add)
            mm(PSc[:, 0, 0:8], ONF[:], CR[:, 0:8], True, True, ['CR0'], ['PS0'])
            act(CR[:, 8:16], PSc[:, 0, 0:8], AF.Copy, ['PS0'], ['CR1'])
            for h in range(8):
                vop('dve', 'tensor_scalar', ['CR1'], ['FB'], out=FB[:, h, 0:nkb], in0=CUMK[:, 0:nkb, h], scalar1=-1.0, scalar2=CR[:, 8 + h:9 + h],
                    op0=ALU.mult, op1=ALU.add)

            def fox_head(h):
                bs = nx('bq', 2)
                dma('sp', BQ[:, bs, :], BQTs[h], [], ['BQ%d' % bs], 'bq%d' % bs)
                for c in range(nch):
                    ks = nx('kt', 2)
                    dma('sp', KTc[:, ks, :], KTs[h][:, c * 512:(c + 1) * 512], [], ['KT%d' % ks], 'kt%d' % ks)
                    dma('sp', VAc[:, ks], VAs[h][c * 512:(c + 1) * 512, :].rearrange("(k p) c -> p k c", p=128), [], ['VA%d' % ks], 'va%d' % ks)
                    for kbl in range(4):
                        kb = 4 * c + kbl
                        near = kb >= kb0
                        pd = nx('pd', 2)
                        mm(PD[:, pd, :], KTc[:, ks, kbl * 128:(kbl + 1) * 128], BQ[:, bs, :], True, not near, ['KT%d' % ks, 'BQ%d' % bs], ['PD%d' % pd])
                        if near:
                            hs = nx('hf', 2)
                            dma('sp', HF[:, hs, :], hank(hank_fox, (m * NU + kb - kb0) * 640), [], ['HF%d' % hs], 'hf%d' % hs)
                            mm(PD[:, pd, :], ANB[:], HF[:, hs, :], False, True, ['HF%d' % hs], ['PD%d' % pd])
                        ps = nx('pt', 3)
                        act(PTB[:, ps, :], PD[:, pd, :], AF.Exp, ['PD%d' % pd, 'FB'], ['PT%d' % ps], bias=FB[:, h, kb:kb + 1], scale=SCALE)
                        for g in range(4):
                            mm(ACC[:, g, 0:129], PTB[:, ps, g * 128:(g + 1) * 128], VAc[:, ks, kbl, 0:129], kb == 0, kb == nkb - 1,
                               ['PT%d' % ps, 'VA%d' % ks], ['ACC'])
                ms = nx('mx', 2)
                for g in range(4):
                    act(LD[:, 0:1], ACC[:, g, 128:129], AF.Ln, ['ACC'], ['LD'], bias=TINY)
                    act(LD[:, 1:2], LD[:, 0:1], AF.Exp, ['LD'], ['RD'], scale=-1.0)
                    os_ = nx('ol', 2)
                    act(OL[:, os_, :], ACC[:, g, 0:128], AF.Copy, ['ACC', 'RD'], ['OL%d' % os_], scale=LD[:, 1:2])
                    p2 = nx('ps', 2)
                    trb(PSb[:, p2, 0:128], OL[:, os_, :], ['OL%d' % os_], ['PS%d' % p2])
                    act(MX[:, ms, g * 128:(g + 1) * 128], PSb[:, p2, 0:128], AF.Copy, ['PS%d' % p2], ['MX%d' % ms])
                dma('sp', MIXs[8 + h], MX[:, ms, :], ['MX%d' % ms], [], 'mxo%d' % ms)

            for g in range(4):
                ds = nx('dw', 2)
                for h in range(16):
                    vop('pool', 'tensor_scalar', ['AWt'], ['DW%d' % ds], out=DW[:, ds, h, :], in0=IDB[:], scalar1=AWt[:, g, h:h + 1], scalar2=None, op0=ALU.mult)
                for c in range(nch):
                    ki = nx('ki', 3)
                    dma('sp', KI[:, ki, :], KITs[:, c * 512:(c + 1) * 512], [], ['KI%d' % ki], 'ki%d' % ki)
                    masked = c >= G['c0'][m][g]
                    p2 = nx('ps', 2)
                    for h in range(16):
                        hp, half = h // 2, h % 2
                        pd = nx('pd', 2)
                        mm(PD[:, pd, :], QI[half * 64:(half + 1) * 64, hp, g * 128:(g + 1) * 128], KI[half * 64:(half + 1) * 64, ki, :], True, True,
                           ['QI', 'KI%d' % ki], ['PD%d' % pd])
                        rs = nx('rr', 4)
                        act(RR[:, rs, :], PD[:, pd, :], AF.Relu, ['PD%d' % pd], ['RR%d' % rs])
                        mm(PSc[:, p2, :], DW[:, ds, h, :], RR[:, rs, :], h == 0, (h == 15 and not masked), ['DW%d' % ds, 'RR%d' % rs], ['PS%d' % p2])
                    if masked:
                        hs = nx('hi', 2)
                        dma('sp', HI[:, hs, :], hank(hank_idx, ((m * 4 + g) * NMC + c - G['c0'][m][g]) * 640), [], ['HI%d' % hs], 'hi%d' % hs)
                        mm(PSc[:, p2, :], ANB[:], HI[:, hs, :], False, True, ['HI%d' % hs], ['PS%d' % p2])
                    act(SC[:, c * 512:(c + 1) * 512], PSc[:, p2, :], AF.Copy, ['PS%d' % p2], ['SC'])
                fox_head(2 * g)
                fox_head(2 * g + 1)
                for r in range(TOPK // 8):
                    vop('dve', 'max', ['SC'], ['M8'], out=M8[:], in_=SC[:, 0:n])
                    vop('dve', 'match_replace', ['SC', 'M8'], ['SC'], out=SC[:, 0:n], in_to_replace=M8[:], in_values=SC[:, 0:n], imm_value=-3.0e38)
                for c2 in range(0, n, 2048):
                    w_ = min(2048, n - c2)
                    ns = nx('nsb', 2)
                    vop('dve', 'tensor_scalar', ['SC'], ['NSB%d' % ns], out=NSB[:, ns, 0:w_], in0=SC[:, c2:c2 + w_], scalar1=-1.0e35, scalar2=NEGBIG,
                        op0=ALU.is_gt, op1=ALU.mult)
                    dma('sp', NSs[g][:, c2:c2 + w_], NSB[:, ns, 0:w_], ['NSB%d' % ns], [], 'nso%d' % ns)
            P.flush()

    def stage_D(m):
        nch, nkb, kb0 = G['nch'][m], G['nkb'][m], G['kb0'][m]
        with ExitStack() as s:
            QL = alloc(s, "dQL", [128, 2, 2, 512], BF16); CTc = alloc(s, "dCT", [128, 2, 2, 512], BF16)
            CAc = alloc(s, "dCA", [128, 2, 4, 258], BF16); NSC = alloc(s, "dNS", [128, 2, 4, 512], BF16)
            H5f = alloc(s, "dH5f", [128, 2, 512], F32); H5b = alloc(s, "dH5b", [128, 2, 512], BF16)
            PTB = alloc(s, "dPT", [128, 3, 512], BF16); OLa = alloc(s, "dOL", [128, 2, 256], BF16)
            OLT = alloc(s, "dOLT", [128, 2, 512], BF16); WV = alloc(s, "dWV", [128, 2, 2, 128], BF16)
            MX = alloc(s, "dMX", [128, 2, 512], BF16); LD = alloc(s, "dLD", [128, 4], F32)
            PD = palloc(s, "dPD", [128, 2, 512]); PSc = palloc(s, "dPS", [128, 2, 512]); ACC = palloc(s, "dACC", [128, 4, 512])
            PSb = PSc[:].bitcast(BF16)
            nx = Ctr()
            for h in range(8):
                qs = nx('ql', 2)
                dma('sp', QL[:, qs], QLTs[h], [], ['QL%d' % qs], 'ql%d' % qs)
                wv = nx('wv', 2)
                dma('sp', WV[:, wv], WUV[h].rearrange("(a p) d -> p a d", p=128), [], ['WV%d' % wv], 'wv%d' % wv)
                for c in range(nch):
                    cs = nx('ct', 2)
                    dma('sp', CTc[:, cs], CTs[:, :, c * 512:(c + 1) * 512].rearrange("a p c -> p a c"), [], ['CT%d' % cs], 'ct%d' % cs)
                    dma('sp', CAc[:, cs], CAs[c * 512:(c + 1) * 512, :].rearrange("(k p) c -> p k c", p=128), [], ['CA%d' % cs], 'ca%d' % cs)
                    dma('sp', NSC[:, cs], NSs[:, :, c * 512:(c + 1) * 512].rearrange("g q c -> q g c"), [], ['NS%d' % cs], 'ns%d' % cs)
                    for kbl in range(4):
                        kb = 4 * c + kbl
                        near = kb >= kb0
                        pd = nx('pd', 2)
                        ksl = slice(kbl * 128, (kbl + 1) * 128)
                        mm(PD[:, pd, :], CTc[:, cs, 0, ksl], QL[:, qs, 0, :], True, False, ['CT%d' % cs, 'QL%d' % qs], ['PD%d' % pd])
                        mm(PD[:, pd, :], CTc[:, cs, 1, ksl], QL[:, qs, 1, :], False, False, ['CT%d' % cs, 'QL%d' % qs], ['PD%d' % pd])
                        for g in range(4):
                            mm(PD[:, pd, g * 128:(g + 1) * 128], NSC[:, cs, g, ksl], IDB[:], False, (g == 3 and not near), ['NS%d' % cs], ['PD%d' % pd], skip=True)
                        if near:
                            hs = nx('h5', 2)
                            dma('sp', H5f[:, hs, :], hank(hank_t5, ((m * NU + kb - kb0) * 8 + h) * 640), [], ['H5f%d' % hs], 'h5f%d' % hs)
                            vop('pool', 'tensor_scalar', ['H5f%d' % hs], ['H5b%d' % hs], out=H5b[:, hs, :], in0=H5f[:, hs, :], scalar1=1.0 / SCALE, scalar2=None, op0=ALU.mult)
                            mm(PD[:, pd, :], ANB[:], H5b[:, hs, :], False, True, ['H5b%d' % hs], ['PD%d' % pd], skip=True)
                        ps = nx('pt', 3)
                        act(PTB[:, ps, :], PD[:, pd, :], AF.Exp, ['PD%d' % pd], ['PT%d' % ps], bias=(ZERO if near else B31[:, h:h + 1]), scale=SCALE)
                        for g in range(4):
                            mm(ACC[:, g, 0:257], PTB[:, ps, g * 128:(g + 1) * 128], CAc[:, cs, kbl, 0:257], kb == 0, kb == nkb - 1,
                               ['PT%d' % ps, 'CA%d' % cs], ['ACC'])
                for g in range(4):
                    act(LD[:, 0:1], ACC[:, g, 256:257], AF.Ln, ['ACC'], ['LD'], bias=TINY)
                    act(LD[:, 1:2], LD[:, 0:1], AF.Exp, ['LD'], ['RD'], scale=-1.0)
                    os_ = nx('ol', 2)
                    act(OLa[:, os_, :], ACC[:, g, 0:256], AF.Copy, ['ACC', 'RD'], ['OL%d' % os_], scale=LD[:, 1:2])
                    p2 = nx('ps', 2)
                    for a in range(2):
                        trb(PSb[:, p2, a * 128:(a + 1) * 128], OLa[:, os_, a * 128:(a + 1) * 128], ['OL%d' % os_], ['PS%d' % p2])
                    act(OLT[:, :, g * 128:(g + 1) * 128], PSb[:, p2, 0:256].rearrange("p (a c) -> p a c", c=128), AF.Copy, ['PS%d' % p2], ['OLT'])
                p2 = nx('ps', 2)
                for a in range(2):
                    mm(PSc[:, p2, :], WV[:, wv, a, :], OLT[:, a, :], a == 0, a == 1, ['WV%d' % wv, 'OLT'], ['PS%d' % p2])
                ms = nx('mx', 2)
                act(MX[:, ms, :], PSc[:, p2, :], AF.Copy, ['PS%d' % p2], ['MX%d' % ms])
                dma('sp', MIXs[h], MX[:, ms, :], ['MX%d' % ms], [], 'mxo%d' % ms)
            P.flush()

    def stage_F(m):
        with ExitStack() as s:
            st = dict(XB=alloc(s, "oXB", [128, D], BF16), ST=alloc(s, "oST", [128, 8], F32), PB=palloc(s, "oPB", [128, 2, 512]))
            PA = palloc(s, "oPA", [128, 2, 512]); ACC = palloc(s, "oACC", [128, 4, 512])
            MT = alloc(s, "oMT", [128, 16, 512], BF16); W2 = alloc(s, "oW2", [128, 2, 16, 256], BF16)
            XM = alloc(s, "oXM", [128, 4, D], F32); H2 = alloc(s, "oH2", [128, 16, 512], BF16)
            W1 = alloc(s, "oW1", [128, 4, 16, 128], BF16)
            GA = alloc(s, "oGA", [128, 512], F32); VV = alloc(s, "oVV", [128, 512], F32); SG = alloc(s, "oSG", [128, 512], F32)
            AT = alloc(s, "oAT", [128, NFB, 512], BF16); WD = alloc(s, "oWD", [128, 3, 512], BF16)
            YO = alloc(s, "oYO", [128, 2, D], F32); GF = alloc(s, "oGF", [128, D], F32)
            nx = Ctr()
            PBb = st['PB'][:].bitcast(BF16)
            dma('sp', MT[:], MIXs.rearrange("k p c -> p k c"), [], ['MT'], 'mt')
            dma('sp', GF[:], gfb, [], ['GF'], 'gf')
            for sub in range(4):
                dma('sp', XM[:, sub, :], xq[m * 512 + sub * 128:m * 512 + (sub + 1) * 128, :], [], ['XM%d' % sub], 'xm%d' % sub)
            vop('dve', 'memset', [], ['AT'], AT[:, :, 0:2], 0.0)
            for oc in range(8):
                ws = nx('w2', 2)
                dma('sp', W2[:, ws], WB256[5 + oc], [], ['W2_%d' % ws], 'w2_%d' % ws)
                for sub in range(4):
                    for k in range(16):
                        mm(ACC[:, sub, 0:256], MT[:, k, sub * 128:(sub + 1) * 128], W2[:, ws, k, :], k == 0, k == 15, ['MT', 'W2_%d' % ws], ['ACC%d' % sub])
                    vop('dve', 'tensor_tensor', ['ACC%d' % sub, 'XM%d' % sub], ['XM%d' % sub], out=XM[:, sub, oc * 256:(oc + 1) * 256], in0=ACC[:, sub, 0:256],
                        in1=XM[:, sub, oc * 256:(oc + 1) * 256], op=ALU.add)
            XB, ST = st['XB'], st['ST']
            for sub in range(4):
                xs = XM[:, sub, :]
                act(XB[:], xs, AF.Square, ['XM%d' % sub], ['XB', 'SSQ'], accum=ST[:, 0:1])
                act(ST[:, 1:2], ST[:, 0:1], AF.Sqrt, ['SSQ'], ['SQ'], bias=EPS, scale=1.0 / D)
                vop('dve', 'reciprocal', ['SQ'], ['RSTD'], out=ST[:, 2:3], in_=ST[:, 1:2])
                act(XB[:], xs, AF.Copy, ['XM%d' % sub, 'RSTD'], ['XB'], scale=ST[:, 2:3])
                for k in range(16):
                    trb(PBb[:, k // 8, (k % 8) * 128:(k % 8 + 1) * 128], XB[:, k * 128:(k + 1) * 128], ['XB'], ['PB'])
                vop('dve', 'tensor_tensor', ['PB'], ['H2'], out=H2[:, :, sub * 128:(sub + 1) * 128],
                    in0=PBb.rearrange("p a (k c) -> p (a k) c", c=128), in1=G2B[:], op=ALU.mult)
            for fb in range(NFB):
                for which, dst in ((0, GA), (1, VV)):
                    blk = fb + which * NFB
                    ws = nx('w1', 4)
                    dma('sp', W1[:, ws], WBUP[blk], [], ['W1_%d' % ws], 'w1_%d' % ws)
                    pa = nx('pa', 2)
                    for k in range(16):
                        mm(PA[:, pa, :], W1[:, ws, k, :], H2[:, k, :], k == 0, k == 15, ['W1_%d' % ws, 'H2'], ['PA%d' % pa])
                    key = 'GA' if which == 0 else 'VV'
                    act(dst[:, 2:512], PA[:, pa, 2:512], AF.Identity, ['PA%d' % pa], [key], bias=CB[:, blk:blk + 1], scale=CW[:, blk, 2:3])
                    vop('dve', 'scalar_tensor_tensor', ['PA%d' % pa, key], [key], out=dst[:, 2:512], in0=PA[:, pa, 1:511], scalar=CW[:, blk, 1:2],
                        in1=dst[:, 2:512], op0=ALU.mult, op1=ALU.add)
                    vop('dve', 'scalar_tensor_tensor', ['PA%d' % pa, key], [key], out=dst[:, 2:512], in0=PA[:, pa, 0:510], scalar=CW[:, blk, 0:1],
                        in1=dst[:, 2:512], op0=ALU.mult, op1=ALU.add)
                act(SG[:, 2:512], GA[:, 2:512], AF.Silu, ['GA'], ['SG'])
                vop('dve', 'tensor_tensor', ['SG', 'VV', 'AT'], ['AT%d' % fb], out=AT[:, fb, 2:512], in0=SG[:, 2:512], in1=VV[:, 2:512], op=ALU.mult)
            for oc in range(4):
                for fb in range(NFB):
                    ws = nx('wd', 3)
                    dma('sp', WD[:, ws, :], WBDN[fb * 128:(fb + 1) * 128, oc * 512:(oc + 1) * 512], [], ['WD%d' % ws], 'wd%d' % ws)
                    for sub in range(4):
                        mm(ACC[:, sub, :], AT[:, fb, sub * 128:(sub + 1) * 128], WD[:, ws, :], fb == 0, fb == NFB - 1, ['AT%d' % fb, 'AT', 'WD%d' % ws], ['ACC%d' % sub])
                for sub in range(4):
                    vop('dve', 'tensor_tensor', ['ACC%d' % sub, 'XM%d' % sub], ['XM%d' % sub], out=XM[:, sub, oc * 512:(oc + 1) * 512], in0=ACC[:, sub, :],
                        in1=XM[:, sub, oc * 512:(oc + 1) * 512], op=ALU.add)
            for sub in range(4):
                xs = XM[:, sub, :]
                ys = nx('yo', 2)
                act(YO[:, ys, :], xs, AF.Square, ['XM%d' % sub], ['YO%d' % ys, 'SSQ'], accum=ST[:, 0:1])
                act(ST[:, 1:2], ST[:, 0:1], AF.Sqrt, ['SSQ'], ['SQ'], bias=EPS, scale=1.0 / D)
                vop('dve', 'reciprocal', ['SQ'], ['RSTD'], out=ST[:, 2:3], in_=ST[:, 1:2])
                vop('dve', 'scalar_tensor_tensor', ['XM%d' % sub, 'RSTD', 'GF'], ['YO%d' % ys], out=YO[:, ys, :], in0=xs, scalar=ST[:, 2:3], in1=GF[:],
                    op0=ALU.mult, op1=ALU.mult)
                dma('sp', out[m * 512 + sub * 128:m * 512 + (sub + 1) * 128, :], YO[:, ys, :], ['YO%d' % ys], [], 'yoo%d' % ys)
            P.flush()

    stage_consts()
    stage_K()
    for m in range(NT):
        stage_Q(m)
        stage_IF(m)
        stage_D(m)
        stage_F(m)
    es.close()
    return nc, G


def t5_bucket_np(n):
    n = np.asarray(n)
    max_exact = 16
    nf = np.maximum(n, 1).astype(np.float32)
    large = max_exact + (np.log(nf / np.float32(max_exact)) / np.float32(math.log(128 / max_exact)) * np.float32(32 - max_exact)).astype(np.int32)
    large = np.minimum(large, 31)
    return np.where(n < max_exact, n, large)


def host_prep(inputs, L, NT, G):
    bf = ml_dtypes.bfloat16
    x = np.asarray(inputs['x'], np.float32)
    NKB = L // 128
    NU, NMC = G['NU'], G['NMC']
    t5 = np.asarray(inputs['t5_table'], np.float32)
    common = {
        'w_in': np.ascontiguousarray(inputs['w_in'][0]), 'w_o': np.ascontiguousarray(inputs['w_o'][0]),
        'w_up': np.ascontiguousarray(inputs['w_up'][0]), 'w_down': np.ascontiguousarray(inputs['w_down'][0]),
        'w_uk': np.ascontiguousarray(inputs['w_uk'][0]), 'w_uv': np.ascontiguousarray(inputs['w_uv'][0]),
        'g1T': np.ascontiguousarray(np.asarray(inputs['norm1_g'][0]).reshape(16, 128).T),
        'g2T': np.ascontiguousarray(np.asarray(inputs['norm2_g'][0]).reshape(16, 128).T),
        'gkvb': np.ascontiguousarray(np.broadcast_to(np.asarray(inputs['kv_norm_g'][0])[None, :], (128, 256))),
        'fgb': np.ascontiguousarray(np.asarray(inputs['fgate_b'][0]).reshape(8, 1)),
        'convw': np.ascontiguousarray(np.asarray(inputs['conv_w'][0]).reshape(3, 88, 128).transpose(2, 1, 0)),
        'convb': np.ascontiguousarray(np.asarray(inputs['conv_b'][0]).reshape(88, 128).T),
        'gfb': np.ascontiguousarray(np.broadcast_to(np.asarray(inputs['final_g'])[None, :], (128, D))),
        'b31': np.ascontiguousarray(np.broadcast_to(t5[31][None, :], (128, 8))),
        'identb': np.eye(128, dtype=np.float32).astype(bf), 'antib': np.eye(128, dtype=np.float32)[::-1].copy().astype(bf),
        'identf': np.eye(128, dtype=np.float32), 'onesf': np.ones((128, 128), np.float32),
    }
    common = {k: np.asarray(v, np.float32) if v.dtype not in (bf,) else v for k, v in common.items()}
    v = np.arange(640)
    in_maps = []
    for c in range(8):
        b, j = c // 4, c % 4
        xqc = np.zeros((NT * 512, D), np.float32)
        hf = np.zeros((NT, NU, 640), np.float32)
        ht = np.zeros((NT, NU, 8, 640), np.float32)
        hi = np.zeros((NT, 4, NMC, 640), np.float32)
        oh = np.zeros((NT, 128, NKB, 8), np.float32)
        for m in range(NT):
            p0 = 510 * (4 * m + j) - 2
            pos = p0 + np.arange(512)
            ok = (pos >= 0) & (pos < L)
            xqc[m * 512:(m + 1) * 512][ok] = x[b, pos[ok]]
            pref = min(max(p0 + 256, 0), L - 1)
            oh[m, pref % 128, pref // 128, :] = 1.0
            for i in range(G['nu'][m]):
                kb = G['kb0'][m] + i
                dist = v - 127 + (p0 - 128 * kb)
                hf[m, i] = np.where(dist >= 0, 0.0, NEGBIG)
                bk = t5_bucket_np(np.maximum(dist, 0))
                ht[m, i] = np.where(dist[None, :] >= 0, t5[bk].T, NEGBIG)
            for g in range(4):
                for ci in range(G['nch'][m] - G['c0'][m][g]):
                    cc = G['c0'][m][g] + ci
                    dist = 127 - v + (p0 + 128 * g - 512 * cc)
                    hi[m, g, ci] = np.where(dist >= 0, 0.0, -1e30)
        d = dict(common)
        d.update({'xk': np.ascontiguousarray(x[b]), 'xq': xqc, 'hank_fox': hf.astype(bf), 'hank_t5': ht,
                  'hank_idx': hi.astype(bf), 'ohk': oh.reshape(NT, 128, NKB * 8)})
        in_maps.append(d)
    return in_maps


_CACHE = {}


def kernel(**inputs):
    x = np.asarray(inputs['x'])
    B, L, _ = x.shape
    ntile = (L + 2 + 509) // 510
    NT = (ntile + 3) // 4
    key = (L, NT)
    if key not in _CACHE:
        _CACHE[key] = build(L, NT)
    nc, G = _CACHE[key]
    in_maps = host_prep(inputs, L, NT, G)
    res = run_bass_kernel_spmd(nc, in_maps, core_ids=list(range(8)))
    if DEBUG:
        return res
    outp = np.zeros((B, L, D), np.float32)
    for c in range(8):
        b, j = c // 4, c % 4
        o = res.results[c]['out']
        for m in range(NT):
            p0 = 510 * (4 * m + j) - 2
            pos = p0 + np.arange(2, 512)
            ok = (pos >= 0) & (pos < L)
            outp[b, pos[ok]] = o[m * 512 + 2:(m + 1) * 512][ok]
    return outp
```

```python
import math
from contextlib import ExitStack
import numpy as np
import ml_dtypes
import concourse.bass as bass
import concourse.mybir as mybir
from concourse.bass_utils import run_bass_kernel_spmd

F32 = mybir.dt.float32
BF16 = mybir.dt.bfloat16
AF = mybir.ActivationFunctionType
ALU = mybir.AluOpType
AX = mybir.AxisListType
ENG = ['pe', 'act', 'dve', 'pool', 'sp']

D = 2048
KT = 16
DFF = 5632
NFB = 44
TOPK = 256
NEGBIG = -30000.0
SCALE = 128.0 ** -0.5
C_AQ, C_AKV, C_AIQ, C_AIK, C_AIW, C_BQ, C_BK, C_BV, C_BF = 0, 1024, 1280, 2304, 2368, 2384, 3408, 4432, 5456
DEBUG = False


def geom(L, NT):
    NKB = L // 128
    NCH = L // 512
    g = {}
    g['nch'] = [min(NCH, (2040 * m + 2040 + 511) // 512) for m in range(NT)]
    g['nkb'] = [4 * c for c in g['nch']]
    g['kb0'] = [max(0, (2040 * m - 257) // 128 + 1) for m in range(NT)]
    g['nu'] = [g['nkb'][m] - g['kb0'][m] for m in range(NT)]
    g['NU'] = max(max(g['nu']), 1)
    g['c0'] = [[min(g['nch'][m], max(0, (2040 * m + 128 * gg - 513) // 512 + 1)) for gg in range(4)] for m in range(NT)]
    g['NMC'] = max(1, max(g['nch'][m] - g['c0'][m][gg] for m in range(NT) for gg in range(4)))
    return g


class Prog:
    def __init__(self, nc, es):
        self.nc, self.es = nc, es
        self.q = {e: [] for e in ENG}
        self.cnt, self.sems = {}, {}
        self.waited = {e: {} for e in ENG}
        self.bufs = {}
        for e in ENG[:4]:
            self._newsem(e)

    def _newsem(self, name):
        self.sems[name] = self.es.enter_context(self.nc.semaphore(name))
        self.cnt[name] = 0

    def _emit(self, eng, fn, reads, writes, semname, inc):
        deps = []
        for k in reads:
            b = self.bufs.get(k)
            if b and b[0]:
                deps.append(b[0])
        for k in writes:
            b = self.bufs.get(k)
            if b:
                if b[0]:
                    deps.append(b[0])
                deps.extend(b[1])
        for (sn, val, peng) in deps:
            if peng == 'pe' and eng == 'pe':
                continue
            if self.waited[eng].get(sn, 0) >= val:
                continue
            self.waited[eng][sn] = val
            self.q[eng].append(('w', sn, val))
        self.cnt[semname] += inc
        tk = (semname, self.cnt[semname], eng)
        self.q[eng].append(('o', fn, semname, inc))
        for k in reads:
            self.bufs.setdefault(k, [None, []])[1].append(tk)
        for k in writes:
            self.bufs[k] = [tk, []]
        return tk

    def op(self, eng, fn, reads=(), writes=()):
        return self._emit(eng, fn, list(reads), list(writes), eng, 1)

    def dma(self, q, fn, reads, writes, sem):
        if sem not in self.sems:
            self._newsem(sem)
        return self._emit(q, fn, list(reads), list(writes), sem, 16)

    def flush(self):
        for e in ENG:
            for sn, v in self.cnt.items():
                if v > 0 and self.waited[e].get(sn, 0) < v:
                    self.waited[e][sn] = v
                    self.q[e].append(('w', sn, v))
        qs = self.q
        sems = self.sems

        def mk(e):
            def body(eng):
                for it in qs[e]:
                    if it[0] == 'w':
                        eng.wait_ge(sems[it[1]], it[2])
                    else:
                        it[1](eng).then_inc(sems[it[2]], it[3])
            return body
        with self.nc.Block() as block:
            block.tensor(mk('pe'))
            block.scalar(mk('act'))
            block.vector(mk('dve'))
            block.gpsimd(mk('pool'))
            block.sync(mk('sp'))
        self.q = {e: [] for e in ENG}
        self.bufs = {}


def build(L, NT):
    G = geom(L, NT)
    NKB, NCH = L // 128, L // 512
    NU, NMC = G['NU'], G['NMC']
    nc = bass.Bass("TRN2", target_bir_lowering=False)
    es = ExitStack()

    def din(name, shape, dt=F32):
        return nc.dram_tensor(name, shape, dt, kind="ExternalInput").ap()

    def dsc(name, shape, dt=BF16):
        return nc.dram_tensor(name, shape, dt, kind=("ExternalOutput" if DEBUG else "Internal")).ap()

    xk = din("xk", [L, D]); xq = din("xq", [NT * 512, D])
    w_in = din("w_in", [D, 5464]); w_o = din("w_o", [D, D]); w_up = din("w_up", [D, 2 * DFF]); w_down = din("w_down", [DFF, D])
    w_uk = din("w_uk", [8, 128, 256]); w_uv = din("w_uv", [8, 256, 128])
    g1T = din("g1T", [128, 16]); g2T = din("g2T", [128, 16]); gkvb = din("gkvb", [128, 256]); fgb = din("fgb", [8, 1])
    convw = din("convw", [128, 88, 3]); convb = din("convb", [128, 88]); gfb = din("gfb", [128, D]); b31 = din("b31", [128, 8])
    identb = din("identb", [128, 128], BF16); antib = din("antib", [128, 128], BF16)
    identf = din("identf", [128, 128]); onesf = din("onesf", [128, 128])
    hank_fox = din("hank_fox", [NT, NU, 640], BF16); hank_t5 = din("hank_t5", [NT, NU, 8, 640])
    hank_idx = din("hank_idx", [NT, 4, NMC, 640], BF16); ohk = din("ohk", [NT, 128, NKB * 8])
    out = nc.dram_tensor("out", [NT * 512, D], F32, kind="ExternalOutput").ap()

    WB128 = dsc("wb128", [35, 128, 16, 128]); WB256 = dsc("wb256", [13, 128, 16, 256])
    WBUP = dsc("wbup", [88, 128, 16, 128]); WBDN = dsc("wbdn", [DFF, D])
    WUK = dsc("wukb", [8, 128, 256]); WUV = dsc("wuvb", [8, 256, 128])
    KTs = dsc("kts", [8, 128, L]); VAs = dsc("vas", [8, L, 130]); CTs = dsc("cts", [2, 128, L]); CAs = dsc("cas", [L, 258])
    KITs = dsc("kits", [128, L])
    BQTs = dsc("bqts", [8, 128, 512]); QLTs = dsc("qlts", [8, 128, 2, 512]); QITs = dsc("qits", [8, 128, 512])
    AIWs = dsc("aiws", [512, 16], F32)
    NSs = dsc("nss", [4, 128, L]); MIXs = dsc("mixs", [16, 128, 512]); XMs = dsc("xms", [512, D], F32)
    H2s = dsc("h2s", [128, 16, 512])

    P = Prog(nc, es)

    def sbg(name, shape, dt):
        return es.enter_context(nc.sbuf_tensor(name, shape, dt))

    IDB = sbg("IDB", [128, 128], BF16); ANB = sbg("ANB", [128, 128], BF16)
    IDF = sbg("IDF", [128, 128], F32); ONF = sbg("ONF", [128, 128], F32)
    G1B = sbg("G1B", [128, 16, 128], BF16); G2B = sbg("G2B", [128, 16, 128], BF16)
    GT = sbg("GT", [128, 32], F32); GKV = sbg("GKV", [128, 256], F32); FGB = sbg("FGB", [8, 1], F32)
    CW = sbg("CW", [128, 88, 3], F32); CB = sbg("CB", [128, 88], F32); B31 = sbg("B31", [128, 8], F32)
    CONST = sbg("CONST", [128, 8], F32)
    ONB = sbg("ONB", [128, 128], BF16)
    CUMK = sbg("CUMK", [128, NKB, 8], F32)
    ZERO, ONE, EPS, TINY = CONST[:, 0:1], CONST[:, 1:2], CONST[:, 2:3], CONST[:, 3:4]

    def mm(out_, lhsT, rhs, st, sp, r, w, skip=False):
        P.op('pe', lambda e: e.matmul(out_, lhsT=lhsT, rhs=rhs, start=st, stop=sp, skip_group_check=skip), r, w)

    def trb(out_, in_, r, w):
        P.op('pe', lambda e: e.transpose(out_, in_, IDB[:]), r, w)

    def act(out_, in_, func, r, w, bias=None, scale=None, accum=None):
        kw = {}
        if bias is not None:
            kw['bias'] = bias
        if scale is not None:
            kw['scale'] = scale
        if accum is not None:
            kw['accum_out'] = accum
        P.op('act', lambda e: e.activation(out=out_, in_=in_, func=func, **kw), r, w)

    def dma(q, out_, in_, r, w, sem):
        P.dma(q, lambda e: e.dma_start(out=out_, in_=in_), r, w, sem)

    def vop(eng, name, r, w, *a, **k):
        P.op(eng, lambda e: getattr(e, name)(*a, **k), r, w)

    def stage_consts():
        dma('sp', IDB[:], identb, [], ['C'], 'c0'); dma('sp', ANB[:], antib, [], ['C'], 'c0')
        dma('sp', IDF[:], identf, [], ['C'], 'c0'); dma('sp', ONF[:], onesf, [], ['C'], 'c0')
        dma('sp', GT[:, 0:16], g1T, [], ['C'], 'c0'); dma('sp', GT[:, 16:32], g2T, [], ['C'], 'c0')
        dma('sp', GKV[:], gkvb, [], ['C'], 'c0'); dma('sp', FGB[:], fgb, [], ['C'], 'c0')
        dma('sp', CW[:], convw, [], ['C'], 'c0'); dma('sp', CB[:], convb, [], ['C'], 'c0'); dma('sp', B31[:], b31, [], ['C'], 'c0')
        for i, v in enumerate([0.0, 1.0, 1e-6, 1e-18, -1.0]):
            vop('dve', 'memset', [], ['C2'], CONST[:, i:i + 1], v)
        vop('dve', 'memset', [], ['C2'], ONB[:], 1.0)
        for k in range(16):
            vop('dve', 'tensor_scalar', ['C', 'C2'], ['C3'], out=G1B[:, k, :], in0=ONB[:], scalar1=GT[:, k:k + 1], scalar2=None, op0=ALU.mult)
            vop('dve', 'tensor_scalar', ['C', 'C2'], ['C3'], out=G2B[:, k, :], in0=ONB[:], scalar1=GT[:, 16 + k:17 + k], scalar2=None, op0=ALU.mult)
        vop('dve', 'tensor_scalar', ['C'], ['C4'], out=FGB[:], in0=FGB[:], scalar1=-1.0, scalar2=None, op0=ALU.mult)
        win3 = w_in.rearrange("(k p) c -> p k c", p=128)

        def cvt(dst, src):
            dma('pool', dst, src, [], ['W'], 'wc')
        blk = 0
        for (c0, n, w) in [(C_AQ, 8, 128), (C_AIQ, 8, 128)]:
            for i in range(n):
                cvt(WB128[blk], win3[:, :, c0 + i * w:c0 + (i + 1) * w]); blk += 1
        cvt(WB128[16][:, :, 0:64], win3[:, :, C_AIK:C_AIK + 64]); cvt(WB128[16][:, :, 64:128], win3[:, :, C_AIK:C_AIK + 64])
        blk = 17
        for (c0, n, w) in [(C_BQ, 8, 128), (C_BK, 8, 128)]:
            for i in range(n):
                cvt(WB128[blk], win3[:, :, c0 + i * w:c0 + (i + 1) * w]); blk += 1
        cvt(WB128[33][:, :, 0:8], win3[:, :, C_BF:C_BF + 8])
        cvt(WB128[34][:, :, 0:16], win3[:, :, C_AIW:C_AIW + 16])
        cvt(WB256[0], win3[:, :, C_AKV:C_AKV + 256])
        for i in range(4):
            cvt(WB256[1 + i], win3[:, :, C_BV + i * 256:C_BV + (i + 1) * 256])
        wo3 = w_o.rearrange("(k p) c -> p k c", p=128)
        for i in range(8):
            cvt(WB256[5 + i], wo3[:, :, i * 256:(i + 1) * 256])
        wup3 = w_up.rearrange("(k p) c -> p k c", p=128)
        for i in range(88):
            cvt(WBUP[i], wup3[:, :, i * 128:(i + 1) * 128])
        for i in range(NFB):
            cvt(WBDN[i * 128:(i + 1) * 128, :], w_down[i * 128:(i + 1) * 128, :])
        for h in range(8):
            cvt(WUK[h], w_uk[h]); cvt(WUV[h], w_uv[h])
        P.flush()

    def norm_T(st, src_fn, GB, hT, hkey, tag):
        XS, XB, ST, PB = st['XS'], st['XB'], st['ST'], st['PB']
        PBb = PB[:].bitcast(BF16)
        for sub in range(4):
            s2 = sub % 2
            xs = XS[:, s2, :]
            dma('sp', xs, src_fn(sub), [], ['XS%d' % s2], 'xs%d' % s2)
            act(XB[:], xs, AF.Square, ['XS%d' % s2], ['XB', 'SSQ'], accum=ST[:, 0:1])
            act(ST[:, 1:2], ST[:, 0:1], AF.Sqrt, ['SSQ', 'C2'], ['SQ'], bias=EPS, scale=1.0 / D)
            vop('dve', 'reciprocal', ['SQ'], ['RSTD'], out=ST[:, 2:3], in_=ST[:, 1:2])
            act(XB[:], xs, AF.Copy, ['XS%d' % s2, 'RSTD'], ['XB'], scale=ST[:, 2:3])
            for k in range(16):
                trb(PBb[:, k // 8, (k % 8) * 128:(k % 8 + 1) * 128], XB[:, k * 128:(k + 1) * 128], ['XB', 'C'], ['PB'])
            vop('dve', 'tensor_tensor', ['PB', 'C3'], [hkey], out=hT[:, :, sub * 128:(sub + 1) * 128],
                in0=PBb.rearrange("p a (k c) -> p (a k) c", c=128), in1=GB[:], op=ALU.mult)

    uniq = [0]

    def alloc(stack, name, shape, dt):
        uniq[0] += 1
        return stack.enter_context(nc.sbuf_tensor("%s_%d" % (name, uniq[0]), shape, dt))

    def palloc(stack, name, shape, dt=F32):
        uniq[0] += 1
        return stack.enter_context(nc.psum_tensor("%s_%d" % (name, uniq[0]), shape, dt))

    def stage_K():
        with ExitStack() as s:
            st = dict(XS=alloc(s, "kXS", [128, 2, D], F32), XB=alloc(s, "kXB", [128, D], BF16), ST=alloc(s, "kST", [128, 8], F32),
                      PB=palloc(s, "kPB", [128, 2, 512]))
            PA = palloc(s, "kPA", [128, 2, 512]); PC = palloc(s, "kPC", [128, 2, 512])
            HT = alloc(s, "kHT", [128, 2, 16, 512], BF16)
            W1 = alloc(s, "kW1", [128, 3, 16, 128], BF16); W2 = alloc(s, "kW2", [128, 2, 16, 256], BF16)
            SK = alloc(s, "kSK", [128, 2, 512], BF16); SV = alloc(s, "kSV", [128, 2, 4, 8, 130], BF16)
            SCA = alloc(s, "kSCA", [128, 2, 258], BF16); SCT = alloc(s, "kSCT", [128, 2, 2, 512], BF16)
            AK = alloc(s, "kAK", [128, 256], F32); JK = alloc(s, "kJK", [128, 256], BF16); ST2 = alloc(s, "kST2", [128, 8], F32)
            LF = alloc(s, "kLF", [8, 2, 512], F32); CUMR = alloc(s, "kCUMR", [8, 2, 512], F32); ONES8 = alloc(s, "kON8", [8, 512], F32)
            vop('dve', 'memset', [], ['ON8'], ONES8[:], 1.0)
            for sl in range(2):
                for sub in range(4):
                    vop('dve', 'memset', [], ['SV%d_%d' % (sl, sub)], SV[:, sl, sub, :, 128:130], 1.0)
                vop('dve', 'memset', [], ['SCA%d' % sl], SCA[:, sl, 256:258], 1.0)
            cnt = {'w1': 0, 'w2': 0, 'pa': 0, 'pc': 0, 'sk': 0, 'sv': 0, 'sca': 0}

            def nxt(k, n):
                v = cnt[k] % n
                cnt[k] += 1
                return v

            def loadw1(blk):
                sl = nxt('w1', 3)
                dma('sp', W1[:, sl], WB128[blk], [], ['W1_%d' % sl], 'w1_%d' % sl)
                return sl

            def loadw2(blk):
                sl = nxt('w2', 2)
                dma('sp', W2[:, sl], WB256[blk], [], ['W2_%d' % sl], 'w2_%d' % sl)
                return sl
            for i in range(NCH):
                hs = i % 2
                hT = HT[:, hs]
                hk = 'HT%d' % hs
                norm_T(st, lambda sub: xk[i * 512 + sub * 128:i * 512 + (sub + 1) * 128, :], G1B, hT, hk, 'k')
                for blk, kind, h in [(25 + h, 'k', h) for h in range(8)] + [(16, 'ki', 0)]:
                    ws = loadw1(blk)
                    pa = nxt('pa', 2)
                    for k in range(16):
                        mm(PA[:, pa, :], W1[:, ws, k, :], hT[:, k, :], k == 0, k == 15, ['W1_%d' % ws, hk], ['PA%d' % pa])
                    sk = nxt('sk', 2)
                    act(SK[:, sk, :], PA[:, pa, :], AF.Copy, ['PA%d' % pa], ['SK%d' % sk])
                    dst = KTs[h][:, i * 512:(i + 1) * 512] if kind == 'k' else KITs[:, i * 512:(i + 1) * 512]
                    dma('sp', dst, SK[:, sk, :], ['SK%d' % sk], [], 'sko%d' % sk)
                ws = loadw1(33)
                pa = nxt('pa', 2)
                for k in range(16):
                    mm(PA[0:8, pa, :], W1[:, ws, k, 0:8], hT[:, k, :], k == 0, k == 15, ['W1_%d' % ws, hk], ['PA%d' % pa])
                cs = i % 2
                act(LF[:, cs, :], PA[0:8, pa, :], AF.Exp, ['PA%d' % pa, 'C4'], ['LF%d' % cs], bias=FGB[:, 0:1], scale=-1.0)
                act(LF[:, cs, :], LF[:, cs, :], AF.Ln, ['LF%d' % cs, 'C2'], ['LF%d' % cs], bias=ONE[0:8, :])
                init = 0.0 if i == 0 else CUMR[:, 1 - cs, 511:512]
                vop('dve', 'tensor_tensor_scan', ['LF%d' % cs, 'ON8', 'CR%d' % (1 - cs)], ['CR%d' % cs], out=CUMR[:, cs, :], data0=ONES8[:], data1=LF[:, cs, :],
                    initial=init, op0=ALU.mult, op1=ALU.subtract)
                pc = nxt('pc', 2)
                for j in range(4):
                    P.op('pe', (lambda j=j, pc=pc, cs=cs: (lambda e: e.transpose(PC[:, pc, j * 8:(j + 1) * 8], CUMR[:, cs, j * 128:(j + 1) * 128], IDF[0:8, 0:8])))(),
                         ['CR%d' % cs, 'C'], ['PC%d' % pc])
                vop('dve', 'tensor_copy', ['PC%d' % pc], ['CUMK'], out=CUMK[:, i * 4:(i + 1) * 4, :], in_=PC[:, pc, 0:32].rearrange("p (j h) -> p j h", h=8))
                for vb in range(4):
                    ws = loadw2(1 + vb)
                    for sub in range(4):
                        pa = nxt('pa', 2)
                        for k in range(16):
                            mm(PA[:, pa, 0:256], hT[:, k, sub * 128:(sub + 1) * 128], W2[:, ws, k, :], k == 0, k == 15, ['W2_%d' % ws, hk], ['PA%d' % pa])
                        act(SV[:, hs, sub, 2 * vb:2 * vb + 2, 0:128], PA[:, pa, 0:256].rearrange("p (h c) -> p h c", c=128), AF.Copy, ['PA%d' % pa],
                            ['SV%d_%d' % (hs, sub)])
                for sub in range(4):
                    dma('sp', VAs[:, i * 512 + sub * 128:i * 512 + (sub + 1) * 128, :].rearrange("h p c -> p h c"), SV[:, hs, sub], ['SV%d_%d' % (hs, sub)], [],
                        'svo%d_%d' % (hs, sub))
                ws = loadw2(0)
                PCb = PC[:].bitcast(BF16)
                for sub in range(4):
                    pa = nxt('pa', 2)
                    for k in range(16):
                        mm(PA[:, pa, 0:256], hT[:, k, sub * 128:(sub + 1) * 128], W2[:, ws, k, :], k == 0, k == 15, ['W2_%d' % ws, hk], ['PA%d' % pa])
                    act(JK[:], PA[:, pa, 0:256], AF.Square, ['PA%d' % pa], ['JK', 'S2a'], accum=ST2[:, 0:1])
                    act(ST2[:, 1:2], ST2[:, 0:1], AF.Sqrt, ['S2a', 'C2'], ['S2b'], bias=EPS, scale=1.0 / 256)
                    vop('dve', 'reciprocal', ['S2b'], ['S2c'], out=ST2[:, 2:3], in_=ST2[:, 1:2])
                    cas = nxt('sca', 2)
                    vop('dve', 'scalar_tensor_tensor', ['PA%d' % pa, 'S2c', 'C'], ['SCA%d' % cas], out=SCA[:, cas, 0:256], in0=PA[:, pa, 0:256], scalar=ST2[:, 2:3],
                        in1=GKV[:], op0=ALU.mult, op1=ALU.mult)
                    dma('sp', CAs[i * 512 + sub * 128:i * 512 + (sub + 1) * 128, :], SCA[:, cas, :], ['SCA%d' % cas], [], 'cao%d' % cas)
                    pc = nxt('pc', 2)
                    for a in range(2):
                        trb(PCb[:, pc, a * 128:(a + 1) * 128], SCA[:, cas, a * 128:(a + 1) * 128], ['SCA%d' % cas, 'C'], ['PC%d' % pc])
                    act(SCT[:, hs, :, sub * 128:(sub + 1) * 128], PCb[:, pc, 0:256].rearrange("p (a c) -> p a c", c=128), AF.Copy, ['PC%d' % pc], ['SCT%d' % hs])
                dma('sp', CTs[:, :, i * 512:(i + 1) * 512].rearrange("a p c -> p a c"), SCT[:, hs], ['SCT%d' % hs], [], 'cto%d' % hs)
            P.flush()


    def hank(t, off):
        return bass.AP(tensor=t.tensor, offset=off, ap=[[1, 128], [1, 512]])

    class Ctr:
        def __init__(self):
            self.c = {}

        def __call__(self, k, n):
            v = self.c.get(k, 0)
            self.c[k] = v + 1
            return v % n

    def stage_Q(m):
        with ExitStack() as s:
            st = dict(XS=alloc(s, "qXS", [128, 2, D], F32), XB=alloc(s, "qXB", [128, D], BF16), ST=alloc(s, "qST", [128, 8], F32),
                      PB=palloc(s, "qPB", [128, 2, 512]))
            PA = palloc(s, "qPA", [128, 2, 512]); PC = palloc(s, "qPC", [128, 2, 512])
            HT = alloc(s, "qHT", [128, 16, 512], BF16)
            W1 = alloc(s, "qW1", [128, 3, 16, 128], BF16)
            SK = alloc(s, "qSK", [128, 2, 512], BF16); AQ = alloc(s, "qAQ", [128, 2, 512], BF16)
            WK = alloc(s, "qWK", [128, 2, 256], BF16); QLS = alloc(s, "qQLS", [128, 2, 2, 512], BF16)
            AW = alloc(s, "qAW", [128, 2, 16], F32)
            nx = Ctr()
            norm_T(st, lambda sub: xq[m * 512 + sub * 128:m * 512 + (sub + 1) * 128, :], G1B, HT, 'HT', 'q')

            def proj(blk):
                ws = nx('w1', 3)
                dma('sp', W1[:, ws], WB128[blk], [], ['W1_%d' % ws], 'w1_%d' % ws)
                pa = nx('pa', 2)
                for k in range(16):
                    mm(PA[:, pa, :], W1[:, ws, k, :], HT[:, k, :], k == 0, k == 15, ['W1_%d' % ws, 'HT'], ['PA%d' % pa])
                return pa
            for h in range(8):
                pa = proj(17 + h)
                sk = nx('sk', 2)
                act(SK[:, sk, :], PA[:, pa, :], AF.Copy, ['PA%d' % pa], ['SK%d' % sk])
                dma('sp', BQTs[h], SK[:, sk, :], ['SK%d' % sk], [], 'sko%d' % sk)
            for hp in range(8):
                pa = proj(8 + hp)
                sk = nx('sk', 2)
                act(SK[:, sk, :], PA[:, pa, :], AF.Copy, ['PA%d' % pa], ['SK%d' % sk])
                dma('sp', QITs[hp], SK[:, sk, :], ['SK%d' % sk], [], 'sko%d' % sk)
            for h in range(8):
                pa = proj(h)
                aq = nx('aq', 2)
                act(AQ[:, aq, :], PA[:, pa, :], AF.Copy, ['PA%d' % pa], ['AQ%d' % aq])
                wk = nx('wk', 2)
                dma('sp', WK[:, wk, :], WUK[h], [], ['WK%d' % wk], 'wk%d' % wk)
                qs = nx('ql', 2)
                for a in range(2):
                    pc = nx('pc', 2)
                    mm(PC[:, pc, :], WK[:, wk, a * 128:(a + 1) * 128], AQ[:, aq, :], True, True, ['WK%d' % wk, 'AQ%d' % aq], ['PC%d' % pc])
                    act(QLS[:, qs, a, :], PC[:, pc, :], AF.Copy, ['PC%d' % pc], ['QLS%d' % qs])
                dma('sp', QLTs[h], QLS[:, qs], ['QLS%d' % qs], [], 'qlo%d' % qs)
            ws = nx('w1', 3)
            dma('sp', W1[:, ws], WB128[34], [], ['W1_%d' % ws], 'w1_%d' % ws)
            for sub in range(4):
                pa = nx('pa', 2)
                for k in range(16):
                    mm(PA[:, pa, 0:16], HT[:, k, sub * 128:(sub + 1) * 128], W1[:, ws, k, 0:16], k == 0, k == 15, ['W1_%d' % ws, 'HT'], ['PA%d' % pa])
                aw = nx('aw', 2)
                act(AW[:, aw, :], PA[:, pa, 0:16], AF.Copy, ['PA%d' % pa], ['AW%d' % aw])
                dma('sp', AIWs[sub * 128:(sub + 1) * 128, :], AW[:, aw, :], ['AW%d' % aw], [], 'awo%d' % aw)
            P.flush()

    def stage_IF(m):
        nch, nkb, kb0 = G['nch'][m], G['nkb'][m], G['kb0'][m]
        n = nch * 512
        with ExitStack() as s:
            SC = alloc(s, "iSC", [128, 2, L], F32)
            QI = alloc(s, "iQI", [128, 8, 512], BF16); AWt = alloc(s, "iAW", [128, 4, 16], F32)
            DW = alloc(s, "iDW", [128, 2, 16, 128], BF16); KI = alloc(s, "iKI", [128, 3, 512], BF16)
            RR = alloc(s, "iRR", [128, 4, 512], BF16); HI = alloc(s, "iHI", [128, 2, 512], BF16)
            M8 = alloc(s, "iM8", [128, 8], F32); NSB = alloc(s, "iNSB", [128, 2, 1024], BF16)
            BQ = alloc(s, "fBQ", [128, 2, 512], BF16); KTc = alloc(s, "fKT", [128, 3, 512], BF16)
            VAc = alloc(s, "fVA", [128, 3, 4, 130], BF16); PTB = alloc(s, "fPT", [128, 3, 512], BF16)
            HF = alloc(s, "fHF", [128, 2, 512], BF16); FB = alloc(s, "fFB", [128, 8, NKB], F32)
            OHs = alloc(s, "fOH", [128, NKB * 8], F32); TMP = alloc(s, "fTMP", [128, NKB * 8], F32)
            CR = alloc(s, "fCR", [128, 16], F32); OL = alloc(s, "fOL", [128, 2, 128], BF16)
            MX = alloc(s, "fMX", [128, 2, 512], BF16); LD = alloc(s, "fLD", [128, 4], F32)
            PD = palloc(s, "iPD", [128, 2, 512]); PSc = palloc(s, "iPS", [128, 2, 512]); ACC = palloc(s, "iACC", [128, 4, 512])
            PSb = PSc[:].bitcast(BF16)
            nx = Ctr()
            dma('sp', QI[:], QITs.rearrange("h p c -> p h c"), [], ['QI'], 'qi')
            dma('sp', AWt[:], AIWs.rearrange("(g p) c -> p g c", p=128), [], ['AWt'], 'awt')
            dma('sp', OHs[:], ohk[m], [], ['OH'], 'oh')
            vop('dve', 'tensor_tensor', ['OH'], ['TMP'], out=TMP[:], in0=CUMK[:].rearrange("p k h -> p (k h)"), in1=OHs[:], op=ALU.mult)
            vop('dve', 'tensor_reduce', ['TMP'], ['CR0'], out=CR[:, 0:8], in_=TMP[:].rearrange("p (k h) -> p h k", h=8), axis=AX.X, op=ALU.add)
            mm(PSc[:, 0, 0:8], ONF[:], CR[:, 0:8], True, True, ['CR0'], ['PS0'])
            act(CR[:, 8:16], PSc[:, 0, 0:8], AF.Copy, ['PS0'], ['CR1'])
            for h in range(8):
                vop('dve', 'tensor_scalar', ['CR1'], ['FB'], out=FB[:, h, 0:nkb], in0=CUMK[:, 0:nkb, h], scalar1=-1.0, scalar2=CR[:, 8 + h:9 + h],
                    op0=ALU.mult, op1=ALU.add)

            def fox_head(h):
                bs = nx('bq', 2)
                dma('sp', BQ[:, bs, :], BQTs[h], [], ['BQ%d' % bs], 'bq%d' % bs)
                pend = None

                def pv(ps, ks, kbl, kb):
                    for g in range(4):
                        mm(ACC[:, g, 0:129], PTB[:, ps, g * 128:(g + 1) * 128], VAc[:, ks, kbl, 0:129], kb == 0, kb == nkb - 1,
                           ['PT%d' % ps, 'VA%d' % ks], ['ACC'])
                for c in range(nch):
                    ks = nx('kt', 3)
                    dma('sp', KTc[:, ks, :], KTs[h][:, c * 512:(c + 1) * 512], [], ['KT%d' % ks], 'kt%d' % ks)
                    dma('sp', VAc[:, ks], VAs[h][c * 512:(c + 1) * 512, :].rearrange("(k p) c -> p k c", p=128), [], ['VA%d' % ks], 'va%d' % ks)
                    for kbl in range(4):
                        kb = 4 * c + kbl
                        near = kb >= kb0
                        pd = nx('pd', 2)
                        mm(PD[:, pd, :], KTc[:, ks, kbl * 128:(kbl + 1) * 128], BQ[:, bs, :], True, not near, ['KT%d' % ks, 'BQ%d' % bs], ['PD%d' % pd])
                        if near:
                            hs = nx('hf', 2)
                            dma('sp', HF[:, hs, :], hank(hank_fox, (m * NU + kb - kb0) * 640), [], ['HF%d' % hs], 'hf%d' % hs)
                            mm(PD[:, pd, :], ANB[:], HF[:, hs, :], False, True, ['HF%d' % hs], ['PD%d' % pd])
                        ps = nx('pt', 3)
                        act(PTB[:, ps, :], PD[:, pd, :], AF.Exp, ['PD%d' % pd, 'FB'], ['PT%d' % ps], bias=FB[:, h, kb:kb + 1], scale=SCALE)
                        if pend is not None:
                            pv(*pend)
                        pend = (ps, ks, kbl, kb)
                pv(*pend)
                ms = nx('mx', 2)
                for g in range(4):
                    act(LD[:, 0:1], ACC[:, g, 128:129], AF.Ln, ['ACC'], ['LD'], bias=TINY)
                    act(LD[:, 1:2], LD[:, 0:1], AF.Exp, ['LD'], ['RD'], scale=-1.0)
                    os_ = nx('ol', 2)
                    act(OL[:, os_, :], ACC[:, g, 0:128], AF.Copy, ['ACC', 'RD'], ['OL%d' % os_], scale=LD[:, 1:2])
                    p2 = nx('ps', 2)
                    trb(PSb[:, p2, 0:128], OL[:, os_, :], ['OL%d' % os_], ['PS%d' % p2])
                    act(MX[:, ms, g * 128:(g + 1) * 128], PSb[:, p2, 0:128], AF.Copy, ['PS%d' % p2], ['MX%d' % ms])
                dma('sp', MIXs[8 + h], MX[:, ms, :], ['MX%d' % ms], [], 'mxo%d' % ms)

            for g in range(4):
                ds = nx('dw', 2)
                sg = g % 2
                SCg = SC[:, sg, :]
                sck = 'SC%d' % sg
                for h in range(16):
                    vop('pool', 'tensor_scalar', ['AWt'], ['DW%d' % ds], out=DW[:, ds, h, :], in0=IDB[:], scalar1=AWt[:, g, h:h + 1], scalar2=None, op0=ALU.mult)
                pend = None

                def fin(c, h, rs, p2, ki):
                    masked = c >= G['c0'][m][g]
                    mm(PSc[:, p2, :], DW[:, ds, h, :], RR[:, rs, :], h == 0, (h == 15 and not masked), ['DW%d' % ds, 'RR%d' % rs], ['PS%d' % p2])
                    if h == 15:
                        if masked:
                            hs = nx('hi', 2)
                            dma('sp', HI[:, hs, :], hank(hank_idx, ((m * 4 + g) * NMC + c - G['c0'][m][g]) * 640), [], ['HI%d' % hs], 'hi%d' % hs)
                            mm(PSc[:, p2, :], ANB[:], HI[:, hs, :], False, True, ['HI%d' % hs], ['PS%d' % p2])
                        act(SCg[:, c * 512:(c + 1) * 512], PSc[:, p2, :], AF.Copy, ['PS%d' % p2], [sck])
                for c in range(nch):
                    ki = nx('ki', 3)
                    dma('sp', KI[:, ki, :], KITs[:, c * 512:(c + 1) * 512], [], ['KI%d' % ki], 'ki%d' % ki)
                    p2 = nx('ps', 2)
                    for h in range(16):
                        hp, half = h // 2, h % 2
                        pd = nx('pd', 2)
                        mm(PD[:, pd, :], QI[half * 64:(half + 1) * 64, hp, g * 128:(g + 1) * 128], KI[half * 64:(half + 1) * 64, ki, :], True, True,
                           ['QI', 'KI%d' % ki], ['PD%d' % pd])
                        rs = nx('rr', 4)
                        act(RR[:, rs, :], PD[:, pd, :], AF.Relu, ['PD%d' % pd], ['RR%d' % rs])
                        if pend is not None:
                            fin(*pend)
                        pend = (c, h, rs, p2, ki)
                fin(*pend)
                fox_head(2 * g)
                fox_head(2 * g + 1)
                for r in range(TOPK // 8):
                    vop('dve', 'max', [sck], ['M8'], out=M8[:], in_=SCg[:, 0:n])
                    vop('dve', 'match_replace', [sck, 'M8'], [sck], out=SCg[:, 0:n], in_to_replace=M8[:], in_values=SCg[:, 0:n], imm_value=-3.0e38)
                for c2 in range(0, n, 1024):
                    w_ = min(1024, n - c2)
                    ns = nx('nsb', 2)
                    vop('dve', 'tensor_scalar', [sck], ['NSB%d' % ns], out=NSB[:, ns, 0:w_], in0=SCg[:, c2:c2 + w_], scalar1=-1.0e35, scalar2=NEGBIG,
                        op0=ALU.is_gt, op1=ALU.mult)
                    dma('sp', NSs[g][:, c2:c2 + w_], NSB[:, ns, 0:w_], ['NSB%d' % ns], [], 'nso%d' % ns)
            P.flush()

    def stage_D(m):
        nch, nkb, kb0 = G['nch'][m], G['nkb'][m], G['kb0'][m]
        with ExitStack() as s:
            QL = alloc(s, "dQL", [128, 2, 2, 512], BF16); CTc = alloc(s, "dCT", [128, 3, 2, 512], BF16)
            CAc = alloc(s, "dCA", [128, 3, 4, 258], BF16); NSC = alloc(s, "dNS", [128, 3, 4, 512], BF16)
            H5f = alloc(s, "dH5f", [128, 2, 512], F32); H5b = alloc(s, "dH5b", [128, 2, 512], BF16)
            PTB = alloc(s, "dPT", [128, 3, 512], BF16); OLa = alloc(s, "dOL", [128, 2, 256], BF16)
            OLT = alloc(s, "dOLT", [128, 2, 512], BF16); WV = alloc(s, "dWV", [128, 2, 2, 128], BF16)
            MX = alloc(s, "dMX", [128, 2, 512], BF16); LD = alloc(s, "dLD", [128, 4], F32)
            PD = palloc(s, "dPD", [128, 2, 512]); PSc = palloc(s, "dPS", [128, 2, 512]); ACC = palloc(s, "dACC", [128, 4, 512])
            PSb = PSc[:].bitcast(BF16)
            nx = Ctr()
            for h in range(8):
                qs = nx('ql', 2)
                dma('sp', QL[:, qs], QLTs[h], [], ['QL%d' % qs], 'ql%d' % qs)
                wv = nx('wv', 2)
                dma('sp', WV[:, wv], WUV[h].rearrange("(a p) d -> p a d", p=128), [], ['WV%d' % wv], 'wv%d' % wv)
                pend = None

                def pvd(ps, cs, kbl, kb):
                    for g in range(4):
                        mm(ACC[:, g, 0:257], PTB[:, ps, g * 128:(g + 1) * 128], CAc[:, cs, kbl, 0:257], kb == 0, kb == nkb - 1,
                           ['PT%d' % ps, 'CA%d' % cs], ['ACC'])
                for c in range(nch):
                    cs = nx('ct', 3)
                    dma('sp', CTc[:, cs], CTs[:, :, c * 512:(c + 1) * 512].rearrange("a p c -> p a c"), [], ['CT%d' % cs], 'ct%d' % cs)
                    dma('sp', CAc[:, cs], CAs[c * 512:(c + 1) * 512, :].rearrange("(k p) c -> p k c", p=128), [], ['CA%d' % cs], 'ca%d' % cs)
                    dma('sp', NSC[:, cs], NSs[:, :, c * 512:(c + 1) * 512].rearrange("g q c -> q g c"), [], ['NS%d' % cs], 'ns%d' % cs)
                    for kbl in range(4):
                        kb = 4 * c + kbl
                        near = kb >= kb0
                        pd = nx('pd', 2)
                        ksl = slice(kbl * 128, (kbl + 1) * 128)
                        mm(PD[:, pd, :], CTc[:, cs, 0, ksl], QL[:, qs, 0, :], True, False, ['CT%d' % cs, 'QL%d' % qs], ['PD%d' % pd])
                        mm(PD[:, pd, :], CTc[:, cs, 1, ksl], QL[:, qs, 1, :], False, False, ['CT%d' % cs, 'QL%d' % qs], ['PD%d' % pd])
                        for g in range(4):
                            mm(PD[:, pd, g * 128:(g + 1) * 128], NSC[:, cs, g, ksl], IDB[:], False, (g == 3 and not near), ['NS%d' % cs], ['PD%d' % pd], skip=True)
                        if near:
                            hs = nx('h5', 2)
                            dma('sp', H5f[:, hs, :], hank(hank_t5, ((m * NU + kb - kb0) * 8 + h) * 640), [], ['H5f%d' % hs], 'h5f%d' % hs)
                            vop('pool', 'tensor_scalar', ['H5f%d' % hs], ['H5b%d' % hs], out=H5b[:, hs, :], in0=H5f[:, hs, :], scalar1=1.0 / SCALE, scalar2=None, op0=ALU.mult)
                            mm(PD[:, pd, :], ANB[:], H5b[:, hs, :], False, True, ['H5b%d' % hs], ['PD%d' % pd], skip=True)
                        ps = nx('pt', 3)
                        act(PTB[:, ps, :], PD[:, pd, :], AF.Exp, ['PD%d' % pd], ['PT%d' % ps], bias=(ZERO if near else B31[:, h:h + 1]), scale=SCALE)
                        if pend is not None:
                            pvd(*pend)
                        pend = (ps, cs, kbl, kb)
                pvd(*pend)
                for g in range(4):
                    act(LD[:, 0:1], ACC[:, g, 256:257], AF.Ln, ['ACC'], ['LD'], bias=TINY)
                    act(LD[:, 1:2], LD[:, 0:1], AF.Exp, ['LD'], ['RD'], scale=-1.0)
                    os_ = nx('ol', 2)
                    act(OLa[:, os_, :], ACC[:, g, 0:256], AF.Copy, ['ACC', 'RD'], ['OL%d' % os_], scale=LD[:, 1:2])
                    p2 = nx('ps', 2)
                    for a in range(2):
                        trb(PSb[:, p2, a * 128:(a + 1) * 128], OLa[:, os_, a * 128:(a + 1) * 128], ['OL%d' % os_], ['PS%d' % p2])
                    act(OLT[:, :, g * 128:(g + 1) * 128], PSb[:, p2, 0:256].rearrange("p (a c) -> p a c", c=128), AF.Copy, ['PS%d' % p2], ['OLT'])
                p2 = nx('ps', 2)
                for a in range(2):
                    mm(PSc[:, p2, :], WV[:, wv, a, :], OLT[:, a, :], a == 0, a == 1, ['WV%d' % wv, 'OLT'], ['PS%d' % p2])
                ms = nx('mx', 2)
                act(MX[:, ms, :], PSc[:, p2, :], AF.Copy, ['PS%d' % p2], ['MX%d' % ms])
                dma('sp', MIXs[h], MX[:, ms, :], ['MX%d' % ms], [], 'mxo%d' % ms)
            P.flush()

    def stage_F(m):
        with ExitStack() as s:
            st = dict(XB=alloc(s, "oXB", [128, D], BF16), ST=alloc(s, "oST", [128, 8], F32), PB=palloc(s, "oPB", [128, 2, 512]))
            PA = palloc(s, "oPA", [128, 2, 512]); ACC = palloc(s, "oACC", [128, 4, 512])
            MT = alloc(s, "oMT", [128, 16, 512], BF16); W2 = alloc(s, "oW2", [128, 2, 16, 256], BF16)
            XM = alloc(s, "oXM", [128, 4, D], F32); H2 = alloc(s, "oH2", [128, 16, 512], BF16)
            W1 = alloc(s, "oW1", [128, 4, 16, 128], BF16)
            GA = alloc(s, "oGA", [128, 512], F32); VV = alloc(s, "oVV", [128, 512], F32); SG = alloc(s, "oSG", [128, 512], F32)
            AT = alloc(s, "oAT", [128, NFB, 512], BF16); WD = alloc(s, "oWD", [128, 3, 512], BF16)
            YO = alloc(s, "oYO", [128, 2, D], F32); GF = alloc(s, "oGF", [128, D], F32)
            nx = Ctr()
            PBb = st['PB'][:].bitcast(BF16)
            dma('sp', MT[:], MIXs.rearrange("k p c -> p k c"), [], ['MT'], 'mt')
            dma('sp', GF[:], gfb, [], ['GF'], 'gf')
            for sub in range(4):
                dma('sp', XM[:, sub, :], xq[m * 512 + sub * 128:m * 512 + (sub + 1) * 128, :], [], ['XM%d' % sub], 'xm%d' % sub)
            vop('dve', 'memset', [], ['AT'], AT[:, :, 0:2], 0.0)
            for oc in range(8):
                ws = nx('w2', 2)
                dma('sp', W2[:, ws], WB256[5 + oc], [], ['W2_%d' % ws], 'w2_%d' % ws)
                for sub in range(4):
                    for k in range(16):
                        mm(ACC[:, sub, 0:256], MT[:, k, sub * 128:(sub + 1) * 128], W2[:, ws, k, :], k == 0, k == 15, ['MT', 'W2_%d' % ws], ['ACC%d' % sub])
                    vop('dve', 'tensor_tensor', ['ACC%d' % sub, 'XM%d' % sub], ['XM%d' % sub], out=XM[:, sub, oc * 256:(oc + 1) * 256], in0=ACC[:, sub, 0:256],
                        in1=XM[:, sub, oc * 256:(oc + 1) * 256], op=ALU.add)
            XB, ST = st['XB'], st['ST']
            for sub in range(4):
                xs = XM[:, sub, :]
                act(XB[:], xs, AF.Square, ['XM%d' % sub], ['XB', 'SSQ'], accum=ST[:, 0:1])
                act(ST[:, 1:2], ST[:, 0:1], AF.Sqrt, ['SSQ'], ['SQ'], bias=EPS, scale=1.0 / D)
                vop('dve', 'reciprocal', ['SQ'], ['RSTD'], out=ST[:, 2:3], in_=ST[:, 1:2])
                act(XB[:], xs, AF.Copy, ['XM%d' % sub, 'RSTD'], ['XB'], scale=ST[:, 2:3])
                for k in range(16):
                    trb(PBb[:, k // 8, (k % 8) * 128:(k % 8 + 1) * 128], XB[:, k * 128:(k + 1) * 128], ['XB'], ['PB'])
                vop('dve', 'tensor_tensor', ['PB'], ['H2'], out=H2[:, :, sub * 128:(sub + 1) * 128],
                    in0=PBb.rearrange("p a (k c) -> p (a k) c", c=128), in1=G2B[:], op=ALU.mult)
            for fb in range(NFB):
                for which, dst in ((0, GA), (1, VV)):
                    blk = fb + which * NFB
                    ws = nx('w1', 4)
                    dma('sp', W1[:, ws], WBUP[blk], [], ['W1_%d' % ws], 'w1_%d' % ws)
                    pa = nx('pa', 2)
                    for k in range(16):
                        mm(PA[:, pa, :], W1[:, ws, k, :], H2[:, k, :], k == 0, k == 15, ['W1_%d' % ws, 'H2'], ['PA%d' % pa])
                    key = 'GA' if which == 0 else 'VV'
                    act(dst[:, 2:512], PA[:, pa, 2:512], AF.Identity, ['PA%d' % pa], [key], bias=CB[:, blk:blk + 1], scale=CW[:, blk, 2:3])
                    vop('dve', 'scalar_tensor_tensor', ['PA%d' % pa, key], [key], out=dst[:, 2:512], in0=PA[:, pa, 1:511], scalar=CW[:, blk, 1:2],
                        in1=dst[:, 2:512], op0=ALU.mult, op1=ALU.add)
                    vop('dve', 'scalar_tensor_tensor', ['PA%d' % pa, key], [key], out=dst[:, 2:512], in0=PA[:, pa, 0:510], scalar=CW[:, blk, 0:1],
                        in1=dst[:, 2:512], op0=ALU.mult, op1=ALU.add)
                act(SG[:, 2:512], GA[:, 2:512], AF.Silu, ['GA'], ['SG'])
                vop('dve', 'tensor_tensor', ['SG', 'VV', 'AT'], ['AT%d' % fb], out=AT[:, fb, 2:512], in0=SG[:, 2:512], in1=VV[:, 2:512], op=ALU.mult)
            for oc in range(4):
                for fb in range(NFB):
                    ws = nx('wd', 3)
                    dma('sp', WD[:, ws, :], WBDN[fb * 128:(fb + 1) * 128, oc * 512:(oc + 1) * 512], [], ['WD%d' % ws], 'wd%d' % ws)
                    for sub in range(4):
                        mm(ACC[:, sub, :], AT[:, fb, sub * 128:(sub + 1) * 128], WD[:, ws, :], fb == 0, fb == NFB - 1, ['AT%d' % fb, 'AT', 'WD%d' % ws], ['ACC%d' % sub])
                for sub in range(4):
                    vop('dve', 'tensor_tensor', ['ACC%d' % sub, 'XM%d' % sub], ['XM%d' % sub], out=XM[:, sub, oc * 512:(oc + 1) * 512], in0=ACC[:, sub, :],
                        in1=XM[:, sub, oc * 512:(oc + 1) * 512], op=ALU.add)
            for sub in range(4):
                xs = XM[:, sub, :]
                ys = nx('yo', 2)
                act(YO[:, ys, :], xs, AF.Square, ['XM%d' % sub], ['YO%d' % ys, 'SSQ'], accum=ST[:, 0:1])
                act(ST[:, 1:2], ST[:, 0:1], AF.Sqrt, ['SSQ'], ['SQ'], bias=EPS, scale=1.0 / D)
                vop('dve', 'reciprocal', ['SQ'], ['RSTD'], out=ST[:, 2:3], in_=ST[:, 1:2])
                vop('dve', 'scalar_tensor_tensor', ['XM%d' % sub, 'RSTD', 'GF'], ['YO%d' % ys], out=YO[:, ys, :], in0=xs, scalar=ST[:, 2:3], in1=GF[:],
                    op0=ALU.mult, op1=ALU.mult)
                dma('sp', out[m * 512 + sub * 128:m * 512 + (sub + 1) * 128, :], YO[:, ys, :], ['YO%d' % ys], [], 'yoo%d' % ys)
            P.flush()

    stage_consts()
    stage_K()
    for m in range(NT):
        stage_Q(m)
        stage_IF(m)
        stage_D(m)
        stage_F(m)
    es.close()
    return nc, G


def t5_bucket_np(n):
    n = np.asarray(n)
    max_exact = 16
    nf = np.maximum(n, 1).astype(np.float32)
    large = max_exact + (np.log(nf / np.float32(max_exact)) / np.float32(math.log(128 / max_exact)) * np.float32(32 - max_exact)).astype(np.int32)
    large = np.minimum(large, 31)
    return np.where(n < max_exact, n, large)


def host_prep(inputs, L, NT, G):
    bf = ml_dtypes.bfloat16
    x = np.asarray(inputs['x'], np.float32)
    NKB = L // 128
    NU, NMC = G['NU'], G['NMC']
    t5 = np.asarray(inputs['t5_table'], np.float32)
    common = {
        'w_in': np.ascontiguousarray(inputs['w_in'][0]), 'w_o': np.ascontiguousarray(inputs['w_o'][0]),
        'w_up': np.ascontiguousarray(inputs['w_up'][0]), 'w_down': np.ascontiguousarray(inputs['w_down'][0]),
        'w_uk': np.ascontiguousarray(inputs['w_uk'][0]), 'w_uv': np.ascontiguousarray(inputs['w_uv'][0]),
        'g1T': np.ascontiguousarray(np.asarray(inputs['norm1_g'][0]).reshape(16, 128).T),
        'g2T': np.ascontiguousarray(np.asarray(inputs['norm2_g'][0]).reshape(16, 128).T),
        'gkvb': np.ascontiguousarray(np.broadcast_to(np.asarray(inputs['kv_norm_g'][0])[None, :], (128, 256))),
        'fgb': np.ascontiguousarray(np.asarray(inputs['fgate_b'][0]).reshape(8, 1)),
        'convw': np.ascontiguousarray(np.asarray(inputs['conv_w'][0]).reshape(3, 88, 128).transpose(2, 1, 0)),
        'convb': np.ascontiguousarray(np.asarray(inputs['conv_b'][0]).reshape(88, 128).T),
        'gfb': np.ascontiguousarray(np.broadcast_to(np.asarray(inputs['final_g'])[None, :], (128, D))),
        'b31': np.ascontiguousarray(np.broadcast_to(t5[31][None, :], (128, 8))),
        'identb': np.eye(128, dtype=np.float32).astype(bf), 'antib': np.eye(128, dtype=np.float32)[::-1].copy().astype(bf),
        'identf': np.eye(128, dtype=np.float32), 'onesf': np.ones((128, 128), np.float32),
    }
    common = {k: np.asarray(v, np.float32) if v.dtype not in (bf,) else v for k, v in common.items()}
    v = np.arange(640)
    in_maps = []
    for c in range(8):
        b, j = c // 4, c % 4
        xqc = np.zeros((NT * 512, D), np.float32)
        hf = np.zeros((NT, NU, 640), np.float32)
        ht = np.zeros((NT, NU, 8, 640), np.float32)
        hi = np.zeros((NT, 4, NMC, 640), np.float32)
        oh = np.zeros((NT, 128, NKB, 8), np.float32)
        for m in range(NT):
            p0 = 510 * (4 * m + j) - 2
            pos = p0 + np.arange(512)
            ok = (pos >= 0) & (pos < L)
            xqc[m * 512:(m + 1) * 512][ok] = x[b, pos[ok]]
            pref = min(max(p0 + 256, 0), L - 1)
            oh[m, pref % 128, pref // 128, :] = 1.0
            for i in range(G['nu'][m]):
                kb = G['kb0'][m] + i
                dist = v - 127 + (p0 - 128 * kb)
                hf[m, i] = np.where(dist >= 0, 0.0, NEGBIG)
                bk = t5_bucket_np(np.maximum(dist, 0))
                ht[m, i] = np.where(dist[None, :] >= 0, t5[bk].T, NEGBIG)
            for g in range(4):
                for ci in range(G['nch'][m] - G['c0'][m][g]):
                    cc = G['c0'][m][g] + ci
                    dist = 127 - v + (p0 + 128 * g - 512 * cc)
                    hi[m, g, ci] = np.where(dist >= 0, 0.0, -1e30)
        d = dict(common)
        d.update({'xk': np.ascontiguousarray(x[b]), 'xq': xqc, 'hank_fox': hf.astype(bf), 'hank_t5': ht,
                  'hank_idx': hi.astype(bf), 'ohk': oh.reshape(NT, 128, NKB * 8)})
        in_maps.append(d)
    return in_maps


_CACHE = {}


def kernel(**inputs):
    x = np.asarray(inputs['x'])
    B, L, _ = x.shape
    ntile = (L + 2 + 509) // 510
    NT = (ntile + 3) // 4
    key = (L, NT)
    if key not in _CACHE:
        _CACHE[key] = build(L, NT)
    nc, G = _CACHE[key]
    in_maps = host_prep(inputs, L, NT, G)
    res = run_bass_kernel_spmd(nc, in_maps, core_ids=list(range(8)))
    if DEBUG:
        return res
    outp = np.zeros((B, L, D), np.float32)
    for c in range(8):
        b, j = c // 4, c % 4
        o = res.results[c]['out']
        for m in range(NT):
            p0 = 510 * (4 * m + j) - 2
            pos = p0 + np.arange(2, 512)
            ok = (pos >= 0) & (pos < L)
            outp[b, pos[ok]] = o[m * 512 + 2:(m + 1) * 512][ok]
    return outp
```

```python
import math
from contextlib import ExitStack
import numpy as np
import ml_dtypes
import concourse.bass as bass
import concourse.mybir as mybir
from concourse.bass_utils import run_bass_kernel_spmd

F32 = mybir.dt.float32
BF16 = mybir.dt.bfloat16
AF = mybir.ActivationFunctionType
ALU = mybir.AluOpType
AX = mybir.AxisListType
ENG = ['pe', 'act', 'dve', 'pool', 'sp']

D = 2048
KT = 16
DFF = 5632
NFB = 44
TOPK = 256
NEGBIG = -30000.0
SCALE = 128.0 ** -0.5
C_AQ, C_AKV, C_AIQ, C_AIK, C_AIW, C_BQ, C_BK, C_BV, C_BF = 0, 1024, 1280, 2304, 2368, 2384, 3408, 4432, 5456
DEBUG = False


def geom(L, NT):
    NKB = L // 128
    NCH = L // 512
    g = {}
    g['nch'] = [min(NCH, (2040 * m + 2040 + 511) // 512) for m in range(NT)]
    g['nkb'] = [4 * c for c in g['nch']]
    g['kb0'] = [max(0, (2040 * m - 257) // 128 + 1) for m in range(NT)]
    g['nu'] = [g['nkb'][m] - g['kb0'][m] for m in range(NT)]
    g['NU'] = max(max(g['nu']), 1)
    g['c0'] = [[min(g['nch'][m], max(0, (2040 * m + 128 * gg - 513) // 512 + 1)) for gg in range(4)] for m in range(NT)]
    g['NMC'] = max(1, max(g['nch'][m] - g['c0'][m][gg] for m in range(NT) for gg in range(4)))
    return g


class Prog:
    def __init__(self, nc, es):
        self.nc, self.es = nc, es
        self.q = {e: [] for e in ENG}
        self.cnt, self.sems = {}, {}
        self.waited = {e: {} for e in ENG}
        self.bufs = {}
        for e in ENG[:4]:
            self._newsem(e)

    def _newsem(self, name):
        self.sems[name] = self.es.enter_context(self.nc.semaphore(name))
        self.cnt[name] = 0

    def _emit(self, eng, fn, reads, writes, semname, inc):
        deps = []
        for k in reads:
            b = self.bufs.get(k)
            if b and b[0]:
                deps.append(b[0])
        for k in writes:
            b = self.bufs.get(k)
            if b:
                if b[0]:
                    deps.append(b[0])
                deps.extend(b[1])
        for (sn, val, peng) in deps:
            if peng == 'pe' and eng == 'pe':
                continue
            if self.waited[eng].get(sn, 0) >= val:
                continue
            self.waited[eng][sn] = val
            self.q[eng].append(('w', sn, val))
        self.cnt[semname] += inc
        tk = (semname, self.cnt[semname], eng)
        self.q[eng].append(('o', fn, semname, inc))
        for k in reads:
            self.bufs.setdefault(k, [None, []])[1].append(tk)
        for k in writes:
            self.bufs[k] = [tk, []]
        return tk

    def op(self, eng, fn, reads=(), writes=()):
        return self._emit(eng, fn, list(reads), list(writes), eng, 1)

    def dma(self, q, fn, reads, writes, sem):
        if sem not in self.sems:
            self._newsem(sem)
        return self._emit(q, fn, list(reads), list(writes), sem, 16)

    def flush(self):
        for e in ENG:
            for sn, v in self.cnt.items():
                if v > 0 and self.waited[e].get(sn, 0) < v:
                    self.waited[e][sn] = v
                    self.q[e].append(('w', sn, v))
        qs = self.q
        sems = self.sems

        def mk(e):
            def body(eng):
                for it in qs[e]:
                    if it[0] == 'w':
                        eng.wait_ge(sems[it[1]], it[2])
                    else:
                        it[1](eng).then_inc(sems[it[2]], it[3])
            return body
        with self.nc.Block() as block:
            block.tensor(mk('pe'))
            block.scalar(mk('act'))
            block.vector(mk('dve'))
            block.gpsimd(mk('pool'))
            block.sync(mk('sp'))
        self.q = {e: [] for e in ENG}
        self.bufs = {}


def build(L, NT):
    G = geom(L, NT)
    NKB, NCH = L // 128, L // 512
    NU, NMC = G['NU'], G['NMC']
    nc = bass.Bass("TRN2", target_bir_lowering=False)
    es = ExitStack()

    def din(name, shape, dt=F32):
        return nc.dram_tensor(name, shape, dt, kind="ExternalInput").ap()

    def dsc(name, shape, dt=BF16):
        return nc.dram_tensor(name, shape, dt, kind=("ExternalOutput" if DEBUG else "Internal")).ap()

    xk = din("xk", [L, D]); xq = din("xq", [NT * 512, D])
    w_in = din("w_in", [D, 5464]); w_o = din("w_o", [D, D]); w_up = din("w_up", [D, 2 * DFF]); w_down = din("w_down", [DFF, D])
    w_uk = din("w_uk", [8, 128, 256]); w_uv = din("w_uv", [8, 256, 128])
    g1T = din("g1T", [128, 16]); g2T = din("g2T", [128, 16]); gkvb = din("gkvb", [128, 256]); fgb = din("fgb", [8, 1])
    convw = din("convw", [128, 88, 3]); convb = din("convb", [128, 88]); gfb = din("gfb", [128, D]); b31 = din("b31", [128, 8])
    identb = din("identb", [128, 128], BF16); antib = din("antib", [128, 128], BF16)
    identf = din("identf", [128, 128]); onesf = din("onesf", [128, 128])
    hank_fox = din("hank_fox", [NT, NU, 640], BF16); hank_t5 = din("hank_t5", [NT, NU, 8, 640])
    hank_idx = din("hank_idx", [NT, 4, NMC, 640], BF16); ohk = din("ohk", [NT, 128, NKB * 8])
    out = nc.dram_tensor("out", [NT * 512, D], F32, kind="ExternalOutput").ap()

    WB128 = dsc("wb128", [35, 128, 16, 128]); WB256 = dsc("wb256", [13, 128, 16, 256])
    WBUP = dsc("wbup", [88, 128, 16, 128]); WBDN = dsc("wbdn", [DFF, D])
    WUK = dsc("wukb", [8, 128, 256]); WUV = dsc("wuvb", [8, 256, 128])
    KTs = dsc("kts", [8, 128, L]); VAs = dsc("vas", [8, L, 130]); CTs = dsc("cts", [2, 128, L]); CAs = dsc("cas", [L, 258])
    KITs = dsc("kits", [128, L])
    BQTs = dsc("bqts", [8, 128, 512]); QLTs = dsc("qlts", [8, 128, 2, 512]); QITs = dsc("qits", [8, 128, 512])
    AIWs = dsc("aiws", [512, 16], F32)
    NSs = dsc("nss", [4, 128, L]); MIXs = dsc("mixs", [16, 128, 512]); XMs = dsc("xms", [512, D], F32)
    H2s = dsc("h2s", [128, 16, 512])

    P = Prog(nc, es)

    def sbg(name, shape, dt):
        return es.enter_context(nc.sbuf_tensor(name, shape, dt))

    IDB = sbg("IDB", [128, 128], BF16); ANB = sbg("ANB", [128, 128], BF16)
    IDF = sbg("IDF", [128, 128], F32); ONF = sbg("ONF", [128, 128], F32)
    G1B = sbg("G1B", [128, 16, 128], BF16); G2B = sbg("G2B", [128, 16, 128], BF16)
    GT = sbg("GT", [128, 32], F32); GKV = sbg("GKV", [128, 256], F32); FGB = sbg("FGB", [8, 1], F32)
    CW = sbg("CW", [128, 88, 3], F32); CB = sbg("CB", [128, 88], F32); B31 = sbg("B31", [128, 8], F32)
    CONST = sbg("CONST", [128, 8], F32)
    ONB = sbg("ONB", [128, 128], BF16)
    CUMK = sbg("CUMK", [128, NKB, 8], F32)
    ZERO, ONE, EPS, TINY = CONST[:, 0:1], CONST[:, 1:2], CONST[:, 2:3], CONST[:, 3:4]

    def mm(out_, lhsT, rhs, st, sp, r, w, skip=False):
        P.op('pe', lambda e: e.matmul(out_, lhsT=lhsT, rhs=rhs, start=st, stop=sp, skip_group_check=skip), r, w)

    def trb(out_, in_, r, w):
        P.op('pe', lambda e: e.transpose(out_, in_, IDB[:]), r, w)

    def act(out_, in_, func, r, w, bias=None, scale=None, accum=None):
        kw = {}
        if bias is not None:
            kw['bias'] = bias
        if scale is not None:
            kw['scale'] = scale
        if accum is not None:
            kw['accum_out'] = accum
        P.op('act', lambda e: e.activation(out=out_, in_=in_, func=func, **kw), r, w)

    def dma(q, out_, in_, r, w, sem):
        P.dma(q, lambda e: e.dma_start(out=out_, in_=in_), r, w, sem)

    def vop(eng, name, r, w, *a, **k):
        P.op(eng, lambda e: getattr(e, name)(*a, **k), r, w)

    def stage_consts():
        dma('sp', IDB[:], identb, [], ['C'], 'c0'); dma('sp', ANB[:], antib, [], ['C'], 'c0')
        dma('sp', IDF[:], identf, [], ['C'], 'c0'); dma('sp', ONF[:], onesf, [], ['C'], 'c0')
        dma('sp', GT[:, 0:16], g1T, [], ['C'], 'c0'); dma('sp', GT[:, 16:32], g2T, [], ['C'], 'c0')
        dma('sp', GKV[:], gkvb, [], ['C'], 'c0'); dma('sp', FGB[:], fgb, [], ['C'], 'c0')
        dma('sp', CW[:], convw, [], ['C'], 'c0'); dma('sp', CB[:], convb, [], ['C'], 'c0'); dma('sp', B31[:], b31, [], ['C'], 'c0')
        for i, v in enumerate([0.0, 1.0, 1e-6, 1e-18, -1.0]):
            vop('dve', 'memset', [], ['C2'], CONST[:, i:i + 1], v)
        vop('dve', 'memset', [], ['C2'], ONB[:], 1.0)
        for k in range(16):
            vop('dve', 'tensor_scalar', ['C', 'C2'], ['C3'], out=G1B[:, k, :], in0=ONB[:], scalar1=GT[:, k:k + 1], scalar2=None, op0=ALU.mult)
            vop('dve', 'tensor_scalar', ['C', 'C2'], ['C3'], out=G2B[:, k, :], in0=ONB[:], scalar1=GT[:, 16 + k:17 + k], scalar2=None, op0=ALU.mult)
        vop('dve', 'tensor_scalar', ['C'], ['C4'], out=FGB[:], in0=FGB[:], scalar1=-1.0, scalar2=None, op0=ALU.mult)
        win3 = w_in.rearrange("(k p) c -> p k c", p=128)

        def cvt(dst, src):
            dma('pool', dst, src, [], ['W'], 'wc')
        blk = 0
        for (c0, n, w) in [(C_AQ, 8, 128), (C_AIQ, 8, 128)]:
            for i in range(n):
                cvt(WB128[blk], win3[:, :, c0 + i * w:c0 + (i + 1) * w]); blk += 1
        cvt(WB128[16][:, :, 0:64], win3[:, :, C_AIK:C_AIK + 64]); cvt(WB128[16][:, :, 64:128], win3[:, :, C_AIK:C_AIK + 64])
        blk = 17
        for (c0, n, w) in [(C_BQ, 8, 128), (C_BK, 8, 128)]:
            for i in range(n):
                cvt(WB128[blk], win3[:, :, c0 + i * w:c0 + (i + 1) * w]); blk += 1
        cvt(WB128[33][:, :, 0:8], win3[:, :, C_BF:C_BF + 8])
        cvt(WB128[34][:, :, 0:16], win3[:, :, C_AIW:C_AIW + 16])
        cvt(WB256[0], win3[:, :, C_AKV:C_AKV + 256])
        for i in range(4):
            cvt(WB256[1 + i], win3[:, :, C_BV + i * 256:C_BV + (i + 1) * 256])
        wo3 = w_o.rearrange("(k p) c -> p k c", p=128)
        for i in range(8):
            cvt(WB256[5 + i], wo3[:, :, i * 256:(i + 1) * 256])
        wup3 = w_up.rearrange("(k p) c -> p k c", p=128)
        for i in range(88):
            cvt(WBUP[i], wup3[:, :, i * 128:(i + 1) * 128])
        for i in range(NFB):
            cvt(WBDN[i * 128:(i + 1) * 128, :], w_down[i * 128:(i + 1) * 128, :])
        for h in range(8):
            cvt(WUK[h], w_uk[h]); cvt(WUV[h], w_uv[h])
        P.flush()

    def norm_T(st, src_fn, GB, hT, hkey, tag):
        XS, XB, ST, PB = st['XS'], st['XB'], st['ST'], st['PB']
        PBb = PB[:].bitcast(BF16)
        for sub in range(4):
            s2 = sub % 2
            xs = XS[:, s2, :]
            dma('sp', xs, src_fn(sub), [], ['XS%d' % s2], 'xs%d' % s2)
            act(XB[:], xs, AF.Square, ['XS%d' % s2], ['XB', 'SSQ'], accum=ST[:, 0:1])
            act(ST[:, 1:2], ST[:, 0:1], AF.Sqrt, ['SSQ', 'C2'], ['SQ'], bias=EPS, scale=1.0 / D)
            vop('dve', 'reciprocal', ['SQ'], ['RSTD'], out=ST[:, 2:3], in_=ST[:, 1:2])
            act(XB[:], xs, AF.Copy, ['XS%d' % s2, 'RSTD'], ['XB'], scale=ST[:, 2:3])
            for k in range(16):
                trb(PBb[:, k // 8, (k % 8) * 128:(k % 8 + 1) * 128], XB[:, k * 128:(k + 1) * 128], ['XB', 'C'], ['PB'])
            vop('dve', 'tensor_tensor', ['PB', 'C3'], [hkey], out=hT[:, :, sub * 128:(sub + 1) * 128],
                in0=PBb.rearrange("p a (k c) -> p (a k) c", c=128), in1=GB[:], op=ALU.mult)

    uniq = [0]

    def alloc(stack, name, shape, dt):
        uniq[0] += 1
        return stack.enter_context(nc.sbuf_tensor("%s_%d" % (name, uniq[0]), shape, dt))

    def palloc(stack, name, shape, dt=F32):
        uniq[0] += 1
        return stack.enter_context(nc.psum_tensor("%s_%d" % (name, uniq[0]), shape, dt))

    def stage_K():
        with ExitStack() as s:
            st = dict(XS=alloc(s, "kXS", [128, 2, D], F32), XB=alloc(s, "kXB", [128, D], BF16), ST=alloc(s, "kST", [128, 8], F32),
                      PB=palloc(s, "kPB", [128, 2, 512]))
            PA = palloc(s, "kPA", [128, 2, 512]); PC = palloc(s, "kPC", [128, 2, 512])
            HT = alloc(s, "kHT", [128, 2, 16, 512], BF16)
            W1 = alloc(s, "kW1", [128, 3, 16, 128], BF16); W2 = alloc(s, "kW2", [128, 2, 16, 256], BF16)
            SK = alloc(s, "kSK", [128, 2, 512], BF16); SV = alloc(s, "kSV", [128, 2, 4, 8, 130], BF16)
            SCA = alloc(s, "kSCA", [128, 2, 258], BF16); SCT = alloc(s, "kSCT", [128, 2, 2, 512], BF16)
            AK = alloc(s, "kAK", [128, 256], F32); JK = alloc(s, "kJK", [128, 256], BF16); ST2 = alloc(s, "kST2", [128, 8], F32)
            LF = alloc(s, "kLF", [8, 2, 512], F32); CUMR = alloc(s, "kCUMR", [8, 2, 512], F32); ONES8 = alloc(s, "kON8", [8, 512], F32)
            vop('dve', 'memset', [], ['ON8'], ONES8[:], 1.0)
            for sl in range(2):
                for sub in range(4):
                    vop('dve', 'memset', [], ['SV%d_%d' % (sl, sub)], SV[:, sl, sub, :, 128:130], 1.0)
                vop('dve', 'memset', [], ['SCA%d' % sl], SCA[:, sl, 256:258], 1.0)
            cnt = {'w1': 0, 'w2': 0, 'pa': 0, 'pc': 0, 'sk': 0, 'sv': 0, 'sca': 0}

            def nxt(k, n):
                v = cnt[k] % n
                cnt[k] += 1
                return v

            def loadw1(blk):
                sl = nxt('w1', 3)
                dma('sp', W1[:, sl], WB128[blk], [], ['W1_%d' % sl], 'w1_%d' % sl)
                return sl

            def loadw2(blk):
                sl = nxt('w2', 2)
                dma('sp', W2[:, sl], WB256[blk], [], ['W2_%d' % sl], 'w2_%d' % sl)
                return sl
            for i in range(NCH):
                hs = i % 2
                hT = HT[:, hs]
                hk = 'HT%d' % hs
                norm_T(st, lambda sub: xk[i * 512 + sub * 128:i * 512 + (sub + 1) * 128, :], G1B, hT, hk, 'k')
                for blk, kind, h in [(25 + h, 'k', h) for h in range(8)] + [(16, 'ki', 0)]:
                    ws = loadw1(blk)
                    pa = nxt('pa', 2)
                    for k in range(16):
                        mm(PA[:, pa, :], W1[:, ws, k, :], hT[:, k, :], k == 0, k == 15, ['W1_%d' % ws, hk], ['PA%d' % pa])
                    sk = nxt('sk', 2)
                    act(SK[:, sk, :], PA[:, pa, :], AF.Copy, ['PA%d' % pa], ['SK%d' % sk])
                    dst = KTs[h][:, i * 512:(i + 1) * 512] if kind == 'k' else KITs[:, i * 512:(i + 1) * 512]
                    dma('sp', dst, SK[:, sk, :], ['SK%d' % sk], [], 'sko%d' % sk)
                ws = loadw1(33)
                pa = nxt('pa', 2)
                for k in range(16):
                    mm(PA[0:8, pa, :], W1[:, ws, k, 0:8], hT[:, k, :], k == 0, k == 15, ['W1_%d' % ws, hk], ['PA%d' % pa])
                cs = i % 2
                act(LF[:, cs, :], PA[0:8, pa, :], AF.Exp, ['PA%d' % pa, 'C4'], ['LF%d' % cs], bias=FGB[:, 0:1], scale=-1.0)
                act(LF[:, cs, :], LF[:, cs, :], AF.Ln, ['LF%d' % cs, 'C2'], ['LF%d' % cs], bias=ONE[0:8, :])
                init = 0.0 if i == 0 else CUMR[:, 1 - cs, 511:512]
                vop('dve', 'tensor_tensor_scan', ['LF%d' % cs, 'ON8', 'CR%d' % (1 - cs)], ['CR%d' % cs], out=CUMR[:, cs, :], data0=ONES8[:], data1=LF[:, cs, :],
                    initial=init, op0=ALU.mult, op1=ALU.subtract)
                pc = nxt('pc', 2)
                for j in range(4):
                    P.op('pe', (lambda j=j, pc=pc, cs=cs: (lambda e: e.transpose(PC[:, pc, j * 8:(j + 1) * 8], CUMR[:, cs, j * 128:(j + 1) * 128], IDF[0:8, 0:8])))(),
                         ['CR%d' % cs, 'C'], ['PC%d' % pc])
                vop('dve', 'tensor_copy', ['PC%d' % pc], ['CUMK'], out=CUMK[:, i * 4:(i + 1) * 4, :], in_=PC[:, pc, 0:32].rearrange("p (j h) -> p j h", h=8))
                for vb in range(4):
                    ws = loadw2(1 + vb)
                    for sub in range(4):
                        pa = nxt('pa', 2)
                        for k in range(16):
                            mm(PA[:, pa, 0:256], hT[:, k, sub * 128:(sub + 1) * 128], W2[:, ws, k, :], k == 0, k == 15, ['W2_%d' % ws, hk], ['PA%d' % pa])
                        act(SV[:, hs, sub, 2 * vb:2 * vb + 2, 0:128], PA[:, pa, 0:256].rearrange("p (h c) -> p h c", c=128), AF.Copy, ['PA%d' % pa],
                            ['SV%d_%d' % (hs, sub)])
                for sub in range(4):
                    dma('sp', VAs[:, i * 512 + sub * 128:i * 512 + (sub + 1) * 128, :].rearrange("h p c -> p h c"), SV[:, hs, sub], ['SV%d_%d' % (hs, sub)], [],
                        'svo%d_%d' % (hs, sub))
                ws = loadw2(0)
                PCb = PC[:].bitcast(BF16)
                for sub in range(4):
                    pa = nxt('pa', 2)
                    for k in range(16):
                        mm(PA[:, pa, 0:256], hT[:, k, sub * 128:(sub + 1) * 128], W2[:, ws, k, :], k == 0, k == 15, ['W2_%d' % ws, hk], ['PA%d' % pa])
                    act(JK[:], PA[:, pa, 0:256], AF.Square, ['PA%d' % pa], ['JK', 'S2a'], accum=ST2[:, 0:1])
                    act(ST2[:, 1:2], ST2[:, 0:1], AF.Sqrt, ['S2a', 'C2'], ['S2b'], bias=EPS, scale=1.0 / 256)
                    vop('dve', 'reciprocal', ['S2b'], ['S2c'], out=ST2[:, 2:3], in_=ST2[:, 1:2])
                    cas = nxt('sca', 2)
                    vop('dve', 'scalar_tensor_tensor', ['PA%d' % pa, 'S2c', 'C'], ['SCA%d' % cas], out=SCA[:, cas, 0:256], in0=PA[:, pa, 0:256], scalar=ST2[:, 2:3],
                        in1=GKV[:], op0=ALU.mult, op1=ALU.mult)
                    dma('sp', CAs[i * 512 + sub * 128:i * 512 + (sub + 1) * 128, :], SCA[:, cas, :], ['SCA%d' % cas], [], 'cao%d' % cas)
                    pc = nxt('pc', 2)
                    for a in range(2):
                        trb(PCb[:, pc, a * 128:(a + 1) * 128], SCA[:, cas, a * 128:(a + 1) * 128], ['SCA%d' % cas, 'C'], ['PC%d' % pc])
                    act(SCT[:, hs, :, sub * 128:(sub + 1) * 128], PCb[:, pc, 0:256].rearrange("p (a c) -> p a c", c=128), AF.Copy, ['PC%d' % pc], ['SCT%d' % hs])
                dma('sp', CTs[:, :, i * 512:(i + 1) * 512].rearrange("a p c -> p a c"), SCT[:, hs], ['SCT%d' % hs], [], 'cto%d' % hs)
            P.flush()


    def hank(t, off):
        return bass.AP(tensor=t.tensor, offset=off, ap=[[1, 128], [1, 512]])

    class Ctr:
        def __init__(self):
            self.c = {}

        def __call__(self, k, n):
            v = self.c.get(k, 0)
            self.c[k] = v + 1
            return v % n

    def stage_Q(m):
        with ExitStack() as s:
            st = dict(XS=alloc(s, "qXS", [128, 2, D], F32), XB=alloc(s, "qXB", [128, D], BF16), ST=alloc(s, "qST", [128, 8], F32),
                      PB=palloc(s, "qPB", [128, 2, 512]))
            PA = palloc(s, "qPA", [128, 2, 512]); PC = palloc(s, "qPC", [128, 2, 512])
            HT = alloc(s, "qHT", [128, 16, 512], BF16)
            W1 = alloc(s, "qW1", [128, 3, 16, 128], BF16)
            SK = alloc(s, "qSK", [128, 2, 512], BF16); AQ = alloc(s, "qAQ", [128, 2, 512], BF16)
            WK = alloc(s, "qWK", [128, 2, 256], BF16); QLS = alloc(s, "qQLS", [128, 2, 2, 512], BF16)
            AW = alloc(s, "qAW", [128, 2, 16], F32)
            nx = Ctr()
            norm_T(st, lambda sub: xq[m * 512 + sub * 128:m * 512 + (sub + 1) * 128, :], G1B, HT, 'HT', 'q')

            def proj(blk):
                ws = nx('w1', 3)
                dma('sp', W1[:, ws], WB128[blk], [], ['W1_%d' % ws], 'w1_%d' % ws)
                pa = nx('pa', 2)
                for k in range(16):
                    mm(PA[:, pa, :], W1[:, ws, k, :], HT[:, k, :], k == 0, k == 15, ['W1_%d' % ws, 'HT'], ['PA%d' % pa])
                return pa
            for h in range(8):
                pa = proj(17 + h)
                sk = nx('sk', 2)
                act(SK[:, sk, :], PA[:, pa, :], AF.Copy, ['PA%d' % pa], ['SK%d' % sk])
                dma('sp', BQTs[h], SK[:, sk, :], ['SK%d' % sk], [], 'sko%d' % sk)
            for hp in range(8):
                pa = proj(8 + hp)
                sk = nx('sk', 2)
                act(SK[:, sk, :], PA[:, pa, :], AF.Copy, ['PA%d' % pa], ['SK%d' % sk])
                dma('sp', QITs[hp], SK[:, sk, :], ['SK%d' % sk], [], 'sko%d' % sk)
            for h in range(8):
                pa = proj(h)
                aq = nx('aq', 2)
                act(AQ[:, aq, :], PA[:, pa, :], AF.Copy, ['PA%d' % pa], ['AQ%d' % aq])
                wk = nx('wk', 2)
                dma('sp', WK[:, wk, :], WUK[h], [], ['WK%d' % wk], 'wk%d' % wk)
                qs = nx('ql', 2)
                for a in range(2):
                    pc = nx('pc', 2)
                    mm(PC[:, pc, :], WK[:, wk, a * 128:(a + 1) * 128], AQ[:, aq, :], True, True, ['WK%d' % wk, 'AQ%d' % aq], ['PC%d' % pc])
                    act(QLS[:, qs, a, :], PC[:, pc, :], AF.Copy, ['PC%d' % pc], ['QLS%d' % qs])
                dma('sp', QLTs[h], QLS[:, qs], ['QLS%d' % qs], [], 'qlo%d' % qs)
            ws = nx('w1', 3)
            dma('sp', W1[:, ws], WB128[34], [], ['W1_%d' % ws], 'w1_%d' % ws)
            for sub in range(4):
                pa = nx('pa', 2)
                for k in range(16):
                    mm(PA[:, pa, 0:16], HT[:, k, sub * 128:(sub + 1) * 128], W1[:, ws, k, 0:16], k == 0, k == 15, ['W1_%d' % ws, 'HT'], ['PA%d' % pa])
                aw = nx('aw', 2)
                act(AW[:, aw, :], PA[:, pa, 0:16], AF.Copy, ['PA%d' % pa], ['AW%d' % aw])
                dma('sp', AIWs[sub * 128:(sub + 1) * 128, :], AW[:, aw, :], ['AW%d' % aw], [], 'awo%d' % aw)
            P.flush()

    def stage_IF(m):
        nch, nkb, kb0 = G['nch'][m], G['nkb'][m], G['kb0'][m]
        n = nch * 512
        if nch <= 4:
            RD_ = 0
        else:
            mu = 256.0 / nch
            RD_ = int(math.ceil((mu + 8.0 * math.sqrt(mu)) / 8.0)) + 1
        with ExitStack() as s:
            SC = alloc(s, "iSC", [128, 2, L], F32)
            CAND = alloc(s, "iCAND", [128, max(NKB * 8, nch * 8 * RD_)], F32); WKb = alloc(s, "iWK", [128, max(512, NKB * 8)], F32)
            WK = WKb[:, 0:512]
            QI = alloc(s, "iQI", [128, 8, 512], BF16); AWt = alloc(s, "iAW", [128, 4, 16], F32)
            DW = alloc(s, "iDW", [128, 2, 16, 128], BF16); KI = alloc(s, "iKI", [128, 3, 512], BF16)
            RR = alloc(s, "iRR", [128, 4, 512], BF16); HI = alloc(s, "iHI", [128, 2, 512], BF16)
            M8 = alloc(s, "iM8", [128, 8], F32); NSB = alloc(s, "iNSB", [128, 2, 512], BF16)
            BQ = alloc(s, "fBQ", [128, 2, 512], BF16); KTc = alloc(s, "fKT", [128, 3, 512], BF16)
            VAc = alloc(s, "fVA", [128, 3, 4, 130], BF16); PTB = alloc(s, "fPT", [128, 3, 512], BF16)
            HF = alloc(s, "fHF", [128, 2, 512], BF16); FB = alloc(s, "fFB", [128, 8, NKB], F32)
            OHs = WKb[:, 0:NKB * 8]; TMP = CAND[:, 0:NKB * 8]
            CR = alloc(s, "fCR", [128, 16], F32); OL = alloc(s, "fOL", [128, 2, 128], BF16)
            MX = alloc(s, "fMX", [128, 2, 512], BF16); LD = alloc(s, "fLD", [128, 4], F32)
            PD = palloc(s, "iPD", [128, 2, 512]); PSc = palloc(s, "iPS", [128, 2, 512]); ACC = palloc(s, "iACC", [128, 4, 512])
            PSb = PSc[:].bitcast(BF16)
            nx = Ctr()
            dma('sp', QI[:], QITs.rearrange("h p c -> p h c"), [], ['QI'], 'qi')
            dma('sp', AWt[:], AIWs.rearrange("(g p) c -> p g c", p=128), [], ['AWt'], 'awt')
            dma('sp', OHs, ohk[m], [], ['WK'], 'oh')
            vop('dve', 'tensor_tensor', ['WK'], ['CAND'], out=TMP, in0=CUMK[:].rearrange("p k h -> p (k h)"), in1=OHs, op=ALU.mult)
            vop('dve', 'tensor_reduce', ['CAND'], ['CR0'], out=CR[:, 0:8], in_=TMP.rearrange("p (k h) -> p h k", h=8), axis=AX.X, op=ALU.add)
            mm(PSc[:, 0, 0:8], ONF[:], CR[:, 0:8], True, True, ['CR0'], ['PS0'])
            act(CR[:, 8:16], PSc[:, 0, 0:8], AF.Copy, ['PS0'], ['CR1'])
            for h in range(8):
                vop('dve', 'tensor_scalar', ['CR1'], ['FB'], out=FB[:, h, 0:nkb], in0=CUMK[:, 0:nkb, h], scalar1=-1.0, scalar2=CR[:, 8 + h:9 + h],
                    op0=ALU.mult, op1=ALU.add)

            def fox_head(h):
                bs = nx('bq', 2)
                dma('sp', BQ[:, bs, :], BQTs[h], [], ['BQ%d' % bs], 'bq%d' % bs)
                pend = None

                def pv(ps, ks, kbl, kb):
                    for g in range(4):
                        mm(ACC[:, g, 0:129], PTB[:, ps, g * 128:(g + 1) * 128], VAc[:, ks, kbl, 0:129], kb == 0, kb == nkb - 1,
                           ['PT%d' % ps, 'VA%d' % ks], ['ACC'])
                for c in range(nch):
                    ks = nx('kt', 3)
                    dma('sp', KTc[:, ks, :], KTs[h][:, c * 512:(c + 1) * 512], [], ['KT%d' % ks], 'kt%d' % ks)
                    dma('sp', VAc[:, ks], VAs[h][c * 512:(c + 1) * 512, :].rearrange("(k p) c -> p k c", p=128), [], ['VA%d' % ks], 'va%d' % ks)
                    for kbl in range(4):
                        kb = 4 * c + kbl
                        near = kb >= kb0
                        pd = nx('pd', 2)
                        mm(PD[:, pd, :], KTc[:, ks, kbl * 128:(kbl + 1) * 128], BQ[:, bs, :], True, not near, ['KT%d' % ks, 'BQ%d' % bs], ['PD%d' % pd])
                        if near:
                            hs = nx('hf', 2)
                            dma('sp', HF[:, hs, :], hank(hank_fox, (m * NU + kb - kb0) * 640), [], ['HF%d' % hs], 'hf%d' % hs)
                            mm(PD[:, pd, :], ANB[:], HF[:, hs, :], False, True, ['HF%d' % hs], ['PD%d' % pd])
                        ps = nx('pt', 3)
                        act(PTB[:, ps, :], PD[:, pd, :], AF.Exp, ['PD%d' % pd, 'FB'], ['PT%d' % ps], bias=FB[:, h, kb:kb + 1], scale=SCALE)
                        if pend is not None:
                            pv(*pend)
                        pend = (ps, ks, kbl, kb)
                pv(*pend)
                ms = nx('mx', 2)
                for g in range(4):
                    act(LD[:, 0:1], ACC[:, g, 128:129], AF.Ln, ['ACC'], ['LD'], bias=TINY)
                    act(LD[:, 1:2], LD[:, 0:1], AF.Exp, ['LD'], ['RD'], scale=-1.0)
                    os_ = nx('ol', 2)
                    act(OL[:, os_, :], ACC[:, g, 0:128], AF.Copy, ['ACC', 'RD'], ['OL%d' % os_], scale=LD[:, 1:2])
                    p2 = nx('ps', 2)
                    trb(PSb[:, p2, 0:128], OL[:, os_, :], ['OL%d' % os_], ['PS%d' % p2])
                    act(MX[:, ms, g * 128:(g + 1) * 128], PSb[:, p2, 0:128], AF.Copy, ['PS%d' % p2], ['MX%d' % ms])
                dma('sp', MIXs[8 + h], MX[:, ms, :], ['MX%d' % ms], [], 'mxo%d' % ms)

            for g in range(4):
                ds = nx('dw', 2)
                sg = g % 2
                SCg = SC[:, sg, :]
                sck = 'SC%d' % sg
                for h in range(16):
                    vop('pool', 'tensor_scalar', ['AWt'], ['DW%d' % ds], out=DW[:, ds, h, :], in0=IDB[:], scalar1=AWt[:, g, h:h + 1], scalar2=None, op0=ALU.mult)
                pend = None

                def fin(c, h, rs, p2, ki):
                    masked = c >= G['c0'][m][g]
                    mm(PSc[:, p2, :], DW[:, ds, h, :], RR[:, rs, :], h == 0, (h == 15 and not masked), ['DW%d' % ds, 'RR%d' % rs], ['PS%d' % p2])
                    if h == 15:
                        if masked:
                            hs = nx('hi', 2)
                            dma('sp', HI[:, hs, :], hank(hank_idx, ((m * 4 + g) * NMC + c - G['c0'][m][g]) * 640), [], ['HI%d' % hs], 'hi%d' % hs)
                            mm(PSc[:, p2, :], ANB[:], HI[:, hs, :], False, True, ['HI%d' % hs], ['PS%d' % p2])
                        ck = '%s_%d' % (sck, c)
                        act(SCg[:, c * 512:(c + 1) * 512], PSc[:, p2, :], AF.Copy, ['PS%d' % p2], [ck])
                        if RD_ > 0:
                            for j in range(RD_):
                                cand = CAND[:, c * 8 * RD_ + 8 * j:c * 8 * RD_ + 8 * j + 8]
                                srcv = SCg[:, c * 512:(c + 1) * 512] if j == 0 else WK
                                vop('dve', 'max', [ck, 'WK'], ['CAND'], out=cand, in_=srcv)
                                if j < RD_ - 1:
                                    vop('dve', 'match_replace', [ck, 'WK', 'CAND'], ['WK'], out=WK, in_to_replace=cand, in_values=srcv, imm_value=-3.0e38)
                for c in range(nch):
                    ki = nx('ki', 3)
                    dma('sp', KI[:, ki, :], KITs[:, c * 512:(c + 1) * 512], [], ['KI%d' % ki], 'ki%d' % ki)
                    p2 = nx('ps', 2)
                    for h in range(16):
                        hp, half = h // 2, h % 2
                        pd = nx('pd', 2)
                        mm(PD[:, pd, :], QI[half * 64:(half + 1) * 64, hp, g * 128:(g + 1) * 128], KI[half * 64:(half + 1) * 64, ki, :], True, True,
                           ['QI', 'KI%d' % ki], ['PD%d' % pd])
                        rs = nx('rr', 4)
                        act(RR[:, rs, :], PD[:, pd, :], AF.Relu, ['PD%d' % pd], ['RR%d' % rs])
                        if pend is not None:
                            fin(*pend)
                        pend = (c, h, rs, p2, ki)
                fin(*pend)
                fox_head(2 * g)
                fox_head(2 * g + 1)
                allk = ['%s_%d' % (sck, c) for c in range(nch)]
                if RD_ == 0:
                    for r in range(TOPK // 8):
                        vop('dve', 'max', allk, ['M8'], out=M8[:], in_=SCg[:, 0:n])
                        vop('dve', 'match_replace', allk + ['M8'], allk, out=SCg[:, 0:n], in_to_replace=M8[:], in_values=SCg[:, 0:n], imm_value=-3.0e38)
                else:
                    ncand = nch * 8 * RD_
                    for r in range(TOPK // 8):
                        vop('dve', 'max', ['CAND'], ['M8'], out=M8[:], in_=CAND[:, 0:ncand])
                        if r < TOPK // 8 - 1:
                            vop('dve', 'match_replace', ['CAND', 'M8'], ['CAND'], out=CAND[:, 0:ncand], in_to_replace=M8[:], in_values=CAND[:, 0:ncand], imm_value=-3.0e38)
                for c2 in range(0, n, 512):
                    w_ = min(512, n - c2)
                    ns = nx('nsb', 2)
                    if RD_ == 0:
                        vop('dve', 'tensor_scalar', allk, ['NSB%d' % ns], out=NSB[:, ns, 0:w_], in0=SCg[:, c2:c2 + w_], scalar1=-1.0e35, scalar2=NEGBIG,
                            op0=ALU.is_gt, op1=ALU.mult)
                    else:
                        vop('dve', 'tensor_scalar', allk + ['M8'], ['NSB%d' % ns], out=NSB[:, ns, 0:w_], in0=SCg[:, c2:c2 + w_], scalar1=M8[:, 7:8], scalar2=NEGBIG,
                            op0=ALU.is_lt, op1=ALU.mult)
                    dma('sp', NSs[g][:, c2:c2 + w_], NSB[:, ns, 0:w_], ['NSB%d' % ns], [], 'nso%d' % ns)
            P.flush()

    def stage_D(m):
        nch, nkb, kb0 = G['nch'][m], G['nkb'][m], G['kb0'][m]
        with ExitStack() as s:
            QL = alloc(s, "dQL", [128, 2, 2, 512], BF16); CTc = alloc(s, "dCT", [128, 3, 2, 512], BF16)
            CAc = alloc(s, "dCA", [128, 3, 4, 258], BF16); NSC = alloc(s, "dNS", [128, 3, 4, 512], BF16)
            H5f = alloc(s, "dH5f", [128, 2, 512], F32); H5b = alloc(s, "dH5b", [128, 2, 512], BF16)
            PTB = alloc(s, "dPT", [128, 3, 512], BF16); OLa = alloc(s, "dOL", [128, 2, 256], BF16)
            OLT = alloc(s, "dOLT", [128, 2, 512], BF16); WV = alloc(s, "dWV", [128, 2, 2, 128], BF16)
            MX = alloc(s, "dMX", [128, 2, 512], BF16); LD = alloc(s, "dLD", [128, 4], F32)
            PD = palloc(s, "dPD", [128, 2, 512]); PSc = palloc(s, "dPS", [128, 2, 512]); ACC = palloc(s, "dACC", [128, 4, 512])
            PSb = PSc[:].bitcast(BF16)
            nx = Ctr()
            for h in range(8):
                qs = nx('ql', 2)
                dma('sp', QL[:, qs], QLTs[h], [], ['QL%d' % qs], 'ql%d' % qs)
                wv = nx('wv', 2)
                dma('sp', WV[:, wv], WUV[h].rearrange("(a p) d -> p a d", p=128), [], ['WV%d' % wv], 'wv%d' % wv)
                pend = None

                def pvd(ps, cs, kbl, kb):
                    for g in range(4):
                        mm(ACC[:, g, 0:257], PTB[:, ps, g * 128:(g + 1) * 128], CAc[:, cs, kbl, 0:257], kb == 0, kb == nkb - 1,
                           ['PT%d' % ps, 'CA%d' % cs], ['ACC'])
                for c in range(nch):
                    cs = nx('ct', 3)
                    dma('sp', CTc[:, cs], CTs[:, :, c * 512:(c + 1) * 512].rearrange("a p c -> p a c"), [], ['CT%d' % cs], 'ct%d' % cs)
                    dma('sp', CAc[:, cs], CAs[c * 512:(c + 1) * 512, :].rearrange("(k p) c -> p k c", p=128), [], ['CA%d' % cs], 'ca%d' % cs)
                    dma('sp', NSC[:, cs], NSs[:, :, c * 512:(c + 1) * 512].rearrange("g q c -> q g c"), [], ['NS%d' % cs], 'ns%d' % cs)
                    for kbl in range(4):
                        kb = 4 * c + kbl
                        near = kb >= kb0
                        pd = nx('pd', 2)
                        ksl = slice(kbl * 128, (kbl + 1) * 128)
                        mm(PD[:, pd, :], CTc[:, cs, 0, ksl], QL[:, qs, 0, :], True, False, ['CT%d' % cs, 'QL%d' % qs], ['PD%d' % pd])
                        mm(PD[:, pd, :], CTc[:, cs, 1, ksl], QL[:, qs, 1, :], False, False, ['CT%d' % cs, 'QL%d' % qs], ['PD%d' % pd])
                        for g in range(4):
                            mm(PD[:, pd, g * 128:(g + 1) * 128], NSC[:, cs, g, ksl], IDB[:], False, (g == 3 and not near), ['NS%d' % cs], ['PD%d' % pd], skip=True)
                        if near:
                            hs = nx('h5', 2)
                            dma('sp', H5f[:, hs, :], hank(hank_t5, ((m * NU + kb - kb0) * 8 + h) * 640), [], ['H5f%d' % hs], 'h5f%d' % hs)
                            vop('pool', 'tensor_scalar', ['H5f%d' % hs], ['H5b%d' % hs], out=H5b[:, hs, :], in0=H5f[:, hs, :], scalar1=1.0 / SCALE, scalar2=None, op0=ALU.mult)
                            mm(PD[:, pd, :], ANB[:], H5b[:, hs, :], False, True, ['H5b%d' % hs], ['PD%d' % pd], skip=True)
                        ps = nx('pt', 3)
                        act(PTB[:, ps, :], PD[:, pd, :], AF.Exp, ['PD%d' % pd], ['PT%d' % ps], bias=(ZERO if near else B31[:, h:h + 1]), scale=SCALE)
                        if pend is not None:
                            pvd(*pend)
                        pend = (ps, cs, kbl, kb)
                pvd(*pend)
                for g in range(4):
                    act(LD[:, 0:1], ACC[:, g, 256:257], AF.Ln, ['ACC'], ['LD'], bias=TINY)
                    act(LD[:, 1:2], LD[:, 0:1], AF.Exp, ['LD'], ['RD'], scale=-1.0)
                    os_ = nx('ol', 2)
                    act(OLa[:, os_, :], ACC[:, g, 0:256], AF.Copy, ['ACC', 'RD'], ['OL%d' % os_], scale=LD[:, 1:2])
                    p2 = nx('ps', 2)
                    for a in range(2):
                        trb(PSb[:, p2, a * 128:(a + 1) * 128], OLa[:, os_, a * 128:(a + 1) * 128], ['OL%d' % os_], ['PS%d' % p2])
                    act(OLT[:, :, g * 128:(g + 1) * 128], PSb[:, p2, 0:256].rearrange("p (a c) -> p a c", c=128), AF.Copy, ['PS%d' % p2], ['OLT'])
                p2 = nx('ps', 2)
                for a in range(2):
                    mm(PSc[:, p2, :], WV[:, wv, a, :], OLT[:, a, :], a == 0, a == 1, ['WV%d' % wv, 'OLT'], ['PS%d' % p2])
                ms = nx('mx', 2)
                act(MX[:, ms, :], PSc[:, p2, :], AF.Copy, ['PS%d' % p2], ['MX%d' % ms])
                dma('sp', MIXs[h], MX[:, ms, :], ['MX%d' % ms], [], 'mxo%d' % ms)
            P.flush()

    def stage_F(m):
        with ExitStack() as s:
            st = dict(XB=alloc(s, "oXB", [128, D], BF16), ST=alloc(s, "oST", [128, 8], F32), PB=palloc(s, "oPB", [128, 2, 512]))
            PA = palloc(s, "oPA", [128, 2, 512]); ACC = palloc(s, "oACC", [128, 4, 512])
            MT = alloc(s, "oMT", [128, 16, 512], BF16); W2 = alloc(s, "oW2", [128, 2, 16, 256], BF16)
            XM = alloc(s, "oXM", [128, 4, D], F32); H2 = alloc(s, "oH2", [128, 16, 512], BF16)
            W1 = alloc(s, "oW1", [128, 4, 16, 128], BF16)
            GA = alloc(s, "oGA", [128, 512], F32); VV = alloc(s, "oVV", [128, 512], F32); SG = alloc(s, "oSG", [128, 512], F32)
            AT = alloc(s, "oAT", [128, NFB, 512], BF16); WD = alloc(s, "oWD", [128, 3, 512], BF16)
            YO = alloc(s, "oYO", [128, 2, D], F32); GF = alloc(s, "oGF", [128, D], F32)
            nx = Ctr()
            PBb = st['PB'][:].bitcast(BF16)
            dma('sp', MT[:], MIXs.rearrange("k p c -> p k c"), [], ['MT'], 'mt')
            dma('sp', GF[:], gfb, [], ['GF'], 'gf')
            for sub in range(4):
                dma('sp', XM[:, sub, :], xq[m * 512 + sub * 128:m * 512 + (sub + 1) * 128, :], [], ['XM%d' % sub], 'xm%d' % sub)
            vop('dve', 'memset', [], ['AT'], AT[:, :, 0:2], 0.0)
            for oc in range(8):
                ws = nx('w2', 2)
                dma('sp', W2[:, ws], WB256[5 + oc], [], ['W2_%d' % ws], 'w2_%d' % ws)
                for sub in range(4):
                    for k in range(16):
                        mm(ACC[:, sub, 0:256], MT[:, k, sub * 128:(sub + 1) * 128], W2[:, ws, k, :], k == 0, k == 15, ['MT', 'W2_%d' % ws], ['ACC%d' % sub])
                    vop('dve', 'tensor_tensor', ['ACC%d' % sub, 'XM%d' % sub], ['XM%d' % sub], out=XM[:, sub, oc * 256:(oc + 1) * 256], in0=ACC[:, sub, 0:256],
                        in1=XM[:, sub, oc * 256:(oc + 1) * 256], op=ALU.add)
            XB, ST = st['XB'], st['ST']
            for sub in range(4):
                xs = XM[:, sub, :]
                act(XB[:], xs, AF.Square, ['XM%d' % sub], ['XB', 'SSQ'], accum=ST[:, 0:1])
                act(ST[:, 1:2], ST[:, 0:1], AF.Sqrt, ['SSQ'], ['SQ'], bias=EPS, scale=1.0 / D)
                vop('dve', 'reciprocal', ['SQ'], ['RSTD'], out=ST[:, 2:3], in_=ST[:, 1:2])
                act(XB[:], xs, AF.Copy, ['XM%d' % sub, 'RSTD'], ['XB'], scale=ST[:, 2:3])
                for k in range(16):
                    trb(PBb[:, k // 8, (k % 8) * 128:(k % 8 + 1) * 128], XB[:, k * 128:(k + 1) * 128], ['XB'], ['PB'])
                vop('dve', 'tensor_tensor', ['PB'], ['H2'], out=H2[:, :, sub * 128:(sub + 1) * 128],
                    in0=PBb.rearrange("p a (k c) -> p (a k) c", c=128), in1=G2B[:], op=ALU.mult)
            for fb in range(NFB):
                for which, dst in ((0, GA), (1, VV)):
                    blk = fb + which * NFB
                    ws = nx('w1', 4)
                    dma('sp', W1[:, ws], WBUP[blk], [], ['W1_%d' % ws], 'w1_%d' % ws)
                    pa = nx('pa', 2)
                    for k in range(16):
                        mm(PA[:, pa, :], W1[:, ws, k, :], H2[:, k, :], k == 0, k == 15, ['W1_%d' % ws, 'H2'], ['PA%d' % pa])
                    key = 'GA' if which == 0 else 'VV'
                    act(dst[:, 2:512], PA[:, pa, 2:512], AF.Identity, ['PA%d' % pa], [key], bias=CB[:, blk:blk + 1], scale=CW[:, blk, 2:3])
                    vop('dve', 'scalar_tensor_tensor', ['PA%d' % pa, key], [key], out=dst[:, 2:512], in0=PA[:, pa, 1:511], scalar=CW[:, blk, 1:2],
                        in1=dst[:, 2:512], op0=ALU.mult, op1=ALU.add)
                    vop('dve', 'scalar_tensor_tensor', ['PA%d' % pa, key], [key], out=dst[:, 2:512], in0=PA[:, pa, 0:510], scalar=CW[:, blk, 0:1],
                        in1=dst[:, 2:512], op0=ALU.mult, op1=ALU.add)
                act(SG[:, 2:512], GA[:, 2:512], AF.Silu, ['GA'], ['SG'])
                vop('dve', 'tensor_tensor', ['SG', 'VV', 'AT'], ['AT%d' % fb], out=AT[:, fb, 2:512], in0=SG[:, 2:512], in1=VV[:, 2:512], op=ALU.mult)
            for oc in range(4):
                for fb in range(NFB):
                    ws = nx('wd', 3)
                    dma('sp', WD[:, ws, :], WBDN[fb * 128:(fb + 1) * 128, oc * 512:(oc + 1) * 512], [], ['WD%d' % ws], 'wd%d' % ws)
                    for sub in range(4):
                        mm(ACC[:, sub, :], AT[:, fb, sub * 128:(sub + 1) * 128], WD[:, ws, :], fb == 0, fb == NFB - 1, ['AT%d' % fb, 'AT', 'WD%d' % ws], ['ACC%d' % sub])
                for sub in range(4):
                    vop('dve', 'tensor_tensor', ['ACC%d' % sub, 'XM%d' % sub], ['XM%d' % sub], out=XM[:, sub, oc * 512:(oc + 1) * 512], in0=ACC[:, sub, :],
                        in1=XM[:, sub, oc * 512:(oc + 1) * 512], op=ALU.add)
            for sub in range(4):
                xs = XM[:, sub, :]
                ys = nx('yo', 2)
                act(YO[:, ys, :], xs, AF.Square, ['XM%d' % sub], ['YO%d' % ys, 'SSQ'], accum=ST[:, 0:1])
                act(ST[:, 1:2], ST[:, 0:1], AF.Sqrt, ['SSQ'], ['SQ'], bias=EPS, scale=1.0 / D)
                vop('dve', 'reciprocal', ['SQ'], ['RSTD'], out=ST[:, 2:3], in_=ST[:, 1:2])
                vop('dve', 'scalar_tensor_tensor', ['XM%d' % sub, 'RSTD', 'GF'], ['YO%d' % ys], out=YO[:, ys, :], in0=xs, scalar=ST[:, 2:3], in1=GF[:],
                    op0=ALU.mult, op1=ALU.mult)
                dma('sp', out[m * 512 + sub * 128:m * 512 + (sub + 1) * 128, :], YO[:, ys, :], ['YO%d' % ys], [], 'yoo%d' % ys)
            P.flush()

    stage_consts()
    stage_K()
    for m in range(NT):
        stage_Q(m)
        stage_IF(m)
        stage_D(m)
        stage_F(m)
    es.close()
    return nc, G


def t5_bucket_np(n):
    n = np.asarray(n)
    max_exact = 16
    nf = np.maximum(n, 1).astype(np.float32)
    large = max_exact + (np.log(nf / np.float32(max_exact)) / np.float32(math.log(128 / max_exact)) * np.float32(32 - max_exact)).astype(np.int32)
    large = np.minimum(large, 31)
    return np.where(n < max_exact, n, large)


def host_prep(inputs, L, NT, G):
    bf = ml_dtypes.bfloat16
    x = np.asarray(inputs['x'], np.float32)
    NKB = L // 128
    NU, NMC = G['NU'], G['NMC']
    t5 = np.asarray(inputs['t5_table'], np.float32)
    common = {
        'w_in': np.ascontiguousarray(inputs['w_in'][0]), 'w_o': np.ascontiguousarray(inputs['w_o'][0]),
        'w_up': np.ascontiguousarray(inputs['w_up'][0]), 'w_down': np.ascontiguousarray(inputs['w_down'][0]),
        'w_uk': np.ascontiguousarray(inputs['w_uk'][0]), 'w_uv': np.ascontiguousarray(inputs['w_uv'][0]),
        'g1T': np.ascontiguousarray(np.asarray(inputs['norm1_g'][0]).reshape(16, 128).T),
        'g2T': np.ascontiguousarray(np.asarray(inputs['norm2_g'][0]).reshape(16, 128).T),
        'gkvb': np.ascontiguousarray(np.broadcast_to(np.asarray(inputs['kv_norm_g'][0])[None, :], (128, 256))),
        'fgb': np.ascontiguousarray(np.asarray(inputs['fgate_b'][0]).reshape(8, 1)),
        'convw': np.ascontiguousarray(np.asarray(inputs['conv_w'][0]).reshape(3, 88, 128).transpose(2, 1, 0)),
        'convb': np.ascontiguousarray(np.asarray(inputs['conv_b'][0]).reshape(88, 128).T),
        'gfb': np.ascontiguousarray(np.broadcast_to(np.asarray(inputs['final_g'])[None, :], (128, D))),
        'b31': np.ascontiguousarray(np.broadcast_to(t5[31][None, :], (128, 8))),
        'identb': np.eye(128, dtype=np.float32).astype(bf), 'antib': np.eye(128, dtype=np.float32)[::-1].copy().astype(bf),
        'identf': np.eye(128, dtype=np.float32), 'onesf': np.ones((128, 128), np.float32),
    }
    common = {k: np.asarray(v, np.float32) if v.dtype not in (bf,) else v for k, v in common.items()}
    v = np.arange(640)
    in_maps = []
    for c in range(8):
        b, j = c // 4, c % 4
        xqc = np.zeros((NT * 512, D), np.float32)
        hf = np.zeros((NT, NU, 640), np.float32)
        ht = np.zeros((NT, NU, 8, 640), np.float32)
        hi = np.zeros((NT, 4, NMC, 640), np.float32)
        oh = np.zeros((NT, 128, NKB, 8), np.float32)
        for m in range(NT):
            p0 = 510 * (4 * m + j) - 2
            pos = p0 + np.arange(512)
            ok = (pos >= 0) & (pos < L)
            xqc[m * 512:(m + 1) * 512][ok] = x[b, pos[ok]]
            pref = min(max(p0 + 256, 0), L - 1)
            oh[m, pref % 128, pref // 128, :] = 1.0
            for i in range(G['nu'][m]):
                kb = G['kb0'][m] + i
                dist = v - 127 + (p0 - 128 * kb)
                hf[m, i] = np.where(dist >= 0, 0.0, NEGBIG)
                bk = t5_bucket_np(np.maximum(dist, 0))
                ht[m, i] = np.where(dist[None, :] >= 0, t5[bk].T, NEGBIG)
            for g in range(4):
                for ci in range(G['nch'][m] - G['c0'][m][g]):
                    cc = G['c0'][m][g] + ci
                    dist = 127 - v + (p0 + 128 * g - 512 * cc)
                    hi[m, g, ci] = np.where(dist >= 0, 0.0, -1e30)
        d = dict(common)
        d.update({'xk': np.ascontiguousarray(x[b]), 'xq': xqc, 'hank_fox': hf.astype(bf), 'hank_t5': ht,
                  'hank_idx': hi.astype(bf), 'ohk': oh.reshape(NT, 128, NKB * 8)})
        in_maps.append(d)
    return in_maps


_CACHE = {}


def kernel(**inputs):
    x = np.asarray(inputs['x'])
    B, L, _ = x.shape
    ntile = (L + 2 + 509) // 510
    NT = (ntile + 3) // 4
    key = (L, NT)
    if key not in _CACHE:
        _CACHE[key] = build(L, NT)
    nc, G = _CACHE[key]
    in_maps = host_prep(inputs, L, NT, G)
    res = run_bass_kernel_spmd(nc, in_maps, core_ids=list(range(8)))
    if DEBUG:
        return res
    outp = np.zeros((B, L, D), np.float32)
    for c in range(8):
        b, j = c // 4, c % 4
        o = res.results[c]['out']
        for m in range(NT):
            p0 = 510 * (4 * m + j) - 2
            pos = p0 + np.arange(2, 512)
            ok = (pos >= 0) & (pos < L)
            outp[b, pos[ok]] = o[m * 512 + 2:(m + 1) * 512][ok]
    return outp
```

```python
import math
from contextlib import ExitStack
import numpy as np
import ml_dtypes
import concourse.bass as bass
import concourse.mybir as mybir
from concourse.bass_utils import run_bass_kernel_spmd

F32 = mybir.dt.float32
BF16 = mybir.dt.bfloat16
AF = mybir.ActivationFunctionType
ALU = mybir.AluOpType
AX = mybir.AxisListType
ENG = ['pe', 'act', 'dve', 'pool', 'sp']

D = 2048
KT = 16
DFF = 5632
NFB = 44
TOPK = 256
NEGBIG = -30000.0
SCALE = 128.0 ** -0.5
C_AQ, C_AKV, C_AIQ, C_AIK, C_AIW, C_BQ, C_BK, C_BV, C_BF = 0, 1024, 1280, 2304, 2368, 2384, 3408, 4432, 5456
DEBUG = False


def geom(L, NT):
    NKB = L // 128
    NCH = L // 512
    g = {}
    g['nch'] = [min(NCH, (2040 * m + 2040 + 511) // 512) for m in range(NT)]
    g['nkb'] = [4 * c for c in g['nch']]
    g['kb0'] = [max(0, (2040 * m - 257) // 128 + 1) for m in range(NT)]
    g['nu'] = [g['nkb'][m] - g['kb0'][m] for m in range(NT)]
    g['NU'] = max(max(g['nu']), 1)
    g['c0'] = [[min(g['nch'][m], max(0, (2040 * m + 128 * gg - 513) // 512 + 1)) for gg in range(4)] for m in range(NT)]
    g['NMC'] = max(1, max(g['nch'][m] - g['c0'][m][gg] for m in range(NT) for gg in range(4)))
    return g


class Prog:
    def __init__(self, nc, es):
        self.nc, self.es = nc, es
        self.q = {e: [] for e in ENG}
        self.cnt, self.sems = {}, {}
        self.waited = {e: {} for e in ENG}
        self.bufs = {}
        for e in ENG[:4]:
            self._newsem(e)

    def _newsem(self, name):
        self.sems[name] = self.es.enter_context(self.nc.semaphore(name))
        self.cnt[name] = 0

    def _emit(self, eng, fn, reads, writes, semname, inc):
        deps = []
        for k in reads:
            b = self.bufs.get(k)
            if b and b[0]:
                deps.append(b[0])
        for k in writes:
            b = self.bufs.get(k)
            if b:
                if b[0]:
                    deps.append(b[0])
                deps.extend(b[1])
        for (sn, val, peng) in deps:
            if peng == 'pe' and eng == 'pe':
                continue
            if self.waited[eng].get(sn, 0) >= val:
                continue
            self.waited[eng][sn] = val
            self.q[eng].append(('w', sn, val))
        self.cnt[semname] += inc
        tk = (semname, self.cnt[semname], eng)
        self.q[eng].append(('o', fn, semname, inc))
        for k in reads:
            self.bufs.setdefault(k, [None, []])[1].append(tk)
        for k in writes:
            self.bufs[k] = [tk, []]
        return tk

    def op(self, eng, fn, reads=(), writes=()):
        return self._emit(eng, fn, list(reads), list(writes), eng, 1)

    def dma(self, q, fn, reads, writes, sem):
        if sem not in self.sems:
            self._newsem(sem)
        return self._emit(q, fn, list(reads), list(writes), sem, 16)

    def flush(self):
        for e in ENG:
            for sn, v in self.cnt.items():
                if v > 0 and self.waited[e].get(sn, 0) < v:
                    self.waited[e][sn] = v
                    self.q[e].append(('w', sn, v))
        qs = self.q
        sems = self.sems

        def mk(e):
            def body(eng):
                for it in qs[e]:
                    if it[0] == 'w':
                        eng.wait_ge(sems[it[1]], it[2])
                    else:
                        it[1](eng).then_inc(sems[it[2]], it[3])
            return body
        with self.nc.Block() as block:
            block.tensor(mk('pe'))
            block.scalar(mk('act'))
            block.vector(mk('dve'))
            block.gpsimd(mk('pool'))
            block.sync(mk('sp'))
        self.q = {e: [] for e in ENG}
        self.bufs = {}


def build(L, NT):
    G = geom(L, NT)
    NKB, NCH = L // 128, L // 512
    NU, NMC = G['NU'], G['NMC']
    nc = bass.Bass("TRN2", target_bir_lowering=False)
    es = ExitStack()

    def din(name, shape, dt=F32):
        return nc.dram_tensor(name, shape, dt, kind="ExternalInput").ap()

    def dsc(name, shape, dt=BF16):
        return nc.dram_tensor(name, shape, dt, kind=("ExternalOutput" if DEBUG else "Internal")).ap()

    xk = din("xk", [L, D]); xq = din("xq", [NT * 512, D])
    w_in = din("w_in", [D, 5464]); w_o = din("w_o", [D, D]); w_up = din("w_up", [D, 2 * DFF]); w_down = din("w_down", [DFF, D])
    w_uk = din("w_uk", [8, 128, 256]); w_uv = din("w_uv", [8, 256, 128])
    g1T = din("g1T", [128, 16]); g2T = din("g2T", [128, 16]); gkvb = din("gkvb", [128, 256]); fgb = din("fgb", [8, 1])
    convw = din("convw", [128, 88, 3]); convb = din("convb", [128, 88]); gfb = din("gfb", [128, D]); b31 = din("b31", [128, 8])
    identb = din("identb", [128, 128], BF16); antib = din("antib", [128, 128], BF16)
    identf = din("identf", [128, 128]); onesf = din("onesf", [128, 128])
    hank_fox = din("hank_fox", [NT, NU, 640], BF16); hank_t5 = din("hank_t5", [NT, NU, 8, 640])
    hank_idx = din("hank_idx", [NT, 4, NMC, 640], BF16); ohk = din("ohk", [NT, 128, NKB * 8])
    out = nc.dram_tensor("out", [NT * 512, D], F32, kind="ExternalOutput").ap()

    WB128 = dsc("wb128", [35, 128, 16, 128]); WB256 = dsc("wb256", [13, 128, 16, 256])
    WBUP = dsc("wbup", [88, 128, 16, 128]); WBDN = dsc("wbdn", [DFF, D])
    WUK = dsc("wukb", [8, 128, 256]); WUV = dsc("wuvb", [8, 256, 128])
    KTs = dsc("kts", [8, 128, L]); VAs = dsc("vas", [8, L, 130]); CTs = dsc("cts", [2, 128, L]); CAs = dsc("cas", [L, 258])
    KITs = dsc("kits", [128, L])
    BQTs = dsc("bqts", [8, 128, 512]); QLTs = dsc("qlts", [8, 128, 2, 512]); QITs = dsc("qits", [8, 128, 512])
    AIWs = dsc("aiws", [512, 16], F32)
    NSs = dsc("nss", [4, 128, L]); MIXs = dsc("mixs", [16, 128, 512]); XMs = dsc("xms", [512, D], F32)
    H2s = dsc("h2s", [128, 16, 512])

    P = Prog(nc, es)

    def sbg(name, shape, dt):
        return es.enter_context(nc.sbuf_tensor(name, shape, dt))

    IDB = sbg("IDB", [128, 128], BF16); ANB = sbg("ANB", [128, 128], BF16)
    IDF = sbg("IDF", [128, 128], F32); ONF = sbg("ONF", [128, 128], F32)
    G1B = sbg("G1B", [128, 16, 128], BF16); G2B = sbg("G2B", [128, 16, 128], BF16)
    GT = sbg("GT", [128, 32], F32); GKV = sbg("GKV", [128, 256], F32); FGB = sbg("FGB", [8, 1], F32)
    CW = sbg("CW", [128, 88, 3], F32); CB = sbg("CB", [128, 88], F32); B31 = sbg("B31", [128, 8], F32)
    CONST = sbg("CONST", [128, 8], F32)
    ONB = sbg("ONB", [128, 128], BF16)
    CUMK = sbg("CUMK", [128, NKB, 8], F32)
    ZERO, ONE, EPS, TINY = CONST[:, 0:1], CONST[:, 1:2], CONST[:, 2:3], CONST[:, 3:4]

    def mm(out_, lhsT, rhs, st, sp, r, w, skip=False):
        P.op('pe', lambda e: e.matmul(out_, lhsT=lhsT, rhs=rhs, start=st, stop=sp, skip_group_check=skip), r, w)

    def trb(out_, in_, r, w):
        P.op('pe', lambda e: e.transpose(out_, in_, IDB[:]), r, w)

    def act(out_, in_, func, r, w, bias=None, scale=None, accum=None):
        kw = {}
        if bias is not None:
            kw['bias'] = bias
        if scale is not None:
            kw['scale'] = scale
        if accum is not None:
            kw['accum_out'] = accum
        P.op('act', lambda e: e.activation(out=out_, in_=in_, func=func, **kw), r, w)

    def dma(q, out_, in_, r, w, sem):
        P.dma(q, lambda e: e.dma_start(out=out_, in_=in_), r, w, sem)

    def vop(eng, name, r, w, *a, **k):
        P.op(eng, lambda e: getattr(e, name)(*a, **k), r, w)

    def stage_consts():
        dma('sp', IDB[:], identb, [], ['C'], 'c0'); dma('sp', ANB[:], antib, [], ['C'], 'c0')
        dma('sp', IDF[:], identf, [], ['C'], 'c0'); dma('sp', ONF[:], onesf, [], ['C'], 'c0')
        dma('sp', GT[:, 0:16], g1T, [], ['C'], 'c0'); dma('sp', GT[:, 16:32], g2T, [], ['C'], 'c0')
        dma('sp', GKV[:], gkvb, [], ['C'], 'c0'); dma('sp', FGB[:], fgb, [], ['C'], 'c0')
        dma('sp', CW[:], convw, [], ['C'], 'c0'); dma('sp', CB[:], convb, [], ['C'], 'c0'); dma('sp', B31[:], b31, [], ['C'], 'c0')
        for i, v in enumerate([0.0, 1.0, 1e-6, 1e-18, -1.0]):
            vop('dve', 'memset', [], ['C2'], CONST[:, i:i + 1], v)
        vop('dve', 'memset', [], ['C2'], ONB[:], 1.0)
        for k in range(16):
            vop('dve', 'tensor_scalar', ['C', 'C2'], ['C3'], out=G1B[:, k, :], in0=ONB[:], scalar1=GT[:, k:k + 1], scalar2=None, op0=ALU.mult)
            vop('dve', 'tensor_scalar', ['C', 'C2'], ['C3'], out=G2B[:, k, :], in0=ONB[:], scalar1=GT[:, 16 + k:17 + k], scalar2=None, op0=ALU.mult)
        vop('dve', 'tensor_scalar', ['C'], ['C4'], out=FGB[:], in0=FGB[:], scalar1=-1.0, scalar2=None, op0=ALU.mult)
        win3 = w_in.rearrange("(k p) c -> p k c", p=128)

        def cvt(dst, src):
            dma('pool', dst, src, [], ['W'], 'wc')
        blk = 0
        for (c0, n, w) in [(C_AQ, 8, 128), (C_AIQ, 8, 128)]:
            for i in range(n):
                cvt(WB128[blk], win3[:, :, c0 + i * w:c0 + (i + 1) * w]); blk += 1
        cvt(WB128[16][:, :, 0:64], win3[:, :, C_AIK:C_AIK + 64]); cvt(WB128[16][:, :, 64:128], win3[:, :, C_AIK:C_AIK + 64])
        blk = 17
        for (c0, n, w) in [(C_BQ, 8, 128), (C_BK, 8, 128)]:
            for i in range(n):
                cvt(WB128[blk], win3[:, :, c0 + i * w:c0 + (i + 1) * w]); blk += 1
        cvt(WB128[33][:, :, 0:8], win3[:, :, C_BF:C_BF + 8])
        cvt(WB128[34][:, :, 0:16], win3[:, :, C_AIW:C_AIW + 16])
        cvt(WB256[0], win3[:, :, C_AKV:C_AKV + 256])
        for i in range(4):
            cvt(WB256[1 + i], win3[:, :, C_BV + i * 256:C_BV + (i + 1) * 256])
        wo3 = w_o.rearrange("(k p) c -> p k c", p=128)
        for i in range(8):
            cvt(WB256[5 + i], wo3[:, :, i * 256:(i + 1) * 256])
        wup3 = w_up.rearrange("(k p) c -> p k c", p=128)
        for i in range(88):
            cvt(WBUP[i], wup3[:, :, i * 128:(i + 1) * 128])
        for i in range(NFB):
            cvt(WBDN[i * 128:(i + 1) * 128, :], w_down[i * 128:(i + 1) * 128, :])
        for h in range(8):
            cvt(WUK[h], w_uk[h]); cvt(WUV[h], w_uv[h])
        P.flush()

    def norm_sub(st, src_fn, GB, hT, hkey, sub):
        XS, XB, ST, PB = st['XS'], st['XB'], st['ST'], st['PB']
        PBb = PB[:].bitcast(BF16)
        s2 = sub % 2
        xs = XS[:, s2, :]
        dma('sp', xs, src_fn(sub), [], ['XS%d' % s2], 'xs%d' % s2)
        act(XB[:], xs, AF.Square, ['XS%d' % s2], ['XB', 'SSQ'], accum=ST[:, 0:1])
        act(ST[:, 1:2], ST[:, 0:1], AF.Sqrt, ['SSQ', 'C2'], ['SQ'], bias=EPS, scale=1.0 / D)
        vop('dve', 'reciprocal', ['SQ'], ['RSTD'], out=ST[:, 2:3], in_=ST[:, 1:2])
        act(XB[:], xs, AF.Copy, ['XS%d' % s2, 'RSTD'], ['XB'], scale=ST[:, 2:3])
        for k in range(16):
            trb(PBb[:, k // 8, (k % 8) * 128:(k % 8 + 1) * 128], XB[:, k * 128:(k + 1) * 128], ['XB', 'C'], ['PB'])
        vop('dve', 'tensor_tensor', ['PB', 'C3'], [hkey], out=hT[:, :, sub * 128:(sub + 1) * 128],
            in0=PBb.rearrange("p a (k c) -> p (a k) c", c=128), in1=GB[:], op=ALU.mult)

    def norm_T(st, src_fn, GB, hT, hkey, tag):
        for sub in range(4):
            norm_sub(st, src_fn, GB, hT, hkey, sub)

    uniq = [0]

    def alloc(stack, name, shape, dt):
        uniq[0] += 1
        return stack.enter_context(nc.sbuf_tensor("%s_%d" % (name, uniq[0]), shape, dt))

    def palloc(stack, name, shape, dt=F32):
        uniq[0] += 1
        return stack.enter_context(nc.psum_tensor("%s_%d" % (name, uniq[0]), shape, dt))

    def stage_K():
        with ExitStack() as s:
            st = dict(XS=alloc(s, "kXS", [128, 2, D], F32), XB=alloc(s, "kXB", [128, D], BF16), ST=alloc(s, "kST", [128, 8], F32),
                      PB=palloc(s, "kPB", [128, 2, 512]))
            PA = palloc(s, "kPA", [128, 2, 512]); PC = palloc(s, "kPC", [128, 2, 512])
            HT = alloc(s, "kHT", [128, 2, 16, 512], BF16)
            W1 = alloc(s, "kW1", [128, 3, 16, 128], BF16); W2 = alloc(s, "kW2", [128, 2, 16, 256], BF16)
            SK = alloc(s, "kSK", [128, 2, 512], BF16); SV = alloc(s, "kSV", [128, 2, 4, 8, 130], BF16)
            SCA = alloc(s, "kSCA", [128, 2, 258], BF16); SCT = alloc(s, "kSCT", [128, 2, 2, 512], BF16)
            AK = alloc(s, "kAK", [128, 256], F32); JK = alloc(s, "kJK", [128, 256], BF16); ST2 = alloc(s, "kST2", [128, 8], F32)
            LF = alloc(s, "kLF", [8, 2, 512], F32); CUMR = alloc(s, "kCUMR", [8, 2, 512], F32); ONES8 = alloc(s, "kON8", [8, 512], F32)
            vop('dve', 'memset', [], ['ON8'], ONES8[:], 1.0)
            for sl in range(2):
                for sub in range(4):
                    vop('dve', 'memset', [], ['SV%d_%d' % (sl, sub)], SV[:, sl, sub, :, 128:130], 1.0)
                vop('dve', 'memset', [], ['SCA%d' % sl], SCA[:, sl, 256:258], 1.0)
            cnt = {'w1': 0, 'w2': 0, 'pa': 0, 'pc': 0, 'sk': 0, 'sv': 0, 'sca': 0}

            def nxt(k, n):
                v = cnt[k] % n
                cnt[k] += 1
                return v

            def loadw1(blk):
                sl = nxt('w1', 3)
                dma('sp', W1[:, sl], WB128[blk], [], ['W1_%d' % sl], 'w1_%d' % sl)
                return sl

            def loadw2(blk):
                sl = nxt('w2', 2)
                dma('sp', W2[:, sl], WB256[blk], [], ['W2_%d' % sl], 'w2_%d' % sl)
                return sl
            def proj_tile(i, steps):
                hs = i % 2
                hT = HT[:, hs]
                hk = 'HT%d' % hs
                for blk, kind, h in [(25 + h, 'k', h) for h in range(8)] + [(16, 'ki', 0)]:
                    ws = loadw1(blk)
                    pa = nxt('pa', 2)
                    for k in range(16):
                        mm(PA[:, pa, :], W1[:, ws, k, :], hT[:, k, :], k == 0, k == 15, ['W1_%d' % ws, hk], ['PA%d' % pa])
                    sk = nxt('sk', 2)
                    act(SK[:, sk, :], PA[:, pa, :], AF.Copy, ['PA%d' % pa], ['SK%d' % sk])
                    dst = KTs[h][:, i * 512:(i + 1) * 512] if kind == 'k' else KITs[:, i * 512:(i + 1) * 512]
                    dma('sp', dst, SK[:, sk, :], ['SK%d' % sk], [], 'sko%d' % sk)
                steps(0)
                ws = loadw1(33)
                pa = nxt('pa', 2)
                for k in range(16):
                    mm(PA[0:8, pa, :], W1[:, ws, k, 0:8], hT[:, k, :], k == 0, k == 15, ['W1_%d' % ws, hk], ['PA%d' % pa])
                cs = i % 2
                act(LF[:, cs, :], PA[0:8, pa, :], AF.Exp, ['PA%d' % pa, 'C4'], ['LF%d' % cs], bias=FGB[:, 0:1], scale=-1.0)
                act(LF[:, cs, :], LF[:, cs, :], AF.Ln, ['LF%d' % cs, 'C2'], ['LF%d' % cs], bias=ONE[0:8, :])
                init = 0.0 if i == 0 else CUMR[:, 1 - cs, 511:512]
                vop('dve', 'tensor_tensor_scan', ['LF%d' % cs, 'ON8', 'CR%d' % (1 - cs)], ['CR%d' % cs], out=CUMR[:, cs, :], data0=ONES8[:], data1=LF[:, cs, :],
                    initial=init, op0=ALU.mult, op1=ALU.subtract)
                pc = nxt('pc', 2)
                for j in range(4):
                    P.op('pe', (lambda j=j, pc=pc, cs=cs: (lambda e: e.transpose(PC[:, pc, j * 8:(j + 1) * 8], CUMR[:, cs, j * 128:(j + 1) * 128], IDF[0:8, 0:8])))(),
                         ['CR%d' % cs, 'C'], ['PC%d' % pc])
                vop('dve', 'tensor_copy', ['PC%d' % pc], ['CUMK'], out=CUMK[:, i * 4:(i + 1) * 4, :], in_=PC[:, pc, 0:32].rearrange("p (j h) -> p j h", h=8))
                steps(1)
                for vb in range(4):
                    ws = loadw2(1 + vb)
                    for sub in range(4):
                        pa = nxt('pa', 2)
                        for k in range(16):
                            mm(PA[:, pa, 0:256], hT[:, k, sub * 128:(sub + 1) * 128], W2[:, ws, k, :], k == 0, k == 15, ['W2_%d' % ws, hk], ['PA%d' % pa])
                        act(SV[:, hs, sub, 2 * vb:2 * vb + 2, 0:128], PA[:, pa, 0:256].rearrange("p (h c) -> p h c", c=128), AF.Copy, ['PA%d' % pa],
                            ['SV%d_%d' % (hs, sub)])
                for sub in range(4):
                    dma('sp', VAs[:, i * 512 + sub * 128:i * 512 + (sub + 1) * 128, :].rearrange("h p c -> p h c"), SV[:, hs, sub], ['SV%d_%d' % (hs, sub)], [],
                        'svo%d_%d' % (hs, sub))
                steps(2)
                ws = loadw2(0)
                PCb = PC[:].bitcast(BF16)
                for sub in range(4):
                    pa = nxt('pa', 2)
                    for k in range(16):
                        mm(PA[:, pa, 0:256], hT[:, k, sub * 128:(sub + 1) * 128], W2[:, ws, k, :], k == 0, k == 15, ['W2_%d' % ws, hk], ['PA%d' % pa])
                    act(JK[:], PA[:, pa, 0:256], AF.Square, ['PA%d' % pa], ['JK', 'S2a'], accum=ST2[:, 0:1])
                    act(ST2[:, 1:2], ST2[:, 0:1], AF.Sqrt, ['S2a', 'C2'], ['S2b'], bias=EPS, scale=1.0 / 256)
                    vop('dve', 'reciprocal', ['S2b'], ['S2c'], out=ST2[:, 2:3], in_=ST2[:, 1:2])
                    cas = nxt('sca', 2)
                    vop('dve', 'scalar_tensor_tensor', ['PA%d' % pa, 'S2c', 'C'], ['SCA%d' % cas], out=SCA[:, cas, 0:256], in0=PA[:, pa, 0:256], scalar=ST2[:, 2:3],
                        in1=GKV[:], op0=ALU.mult, op1=ALU.mult)
                    dma('sp', CAs[i * 512 + sub * 128:i * 512 + (sub + 1) * 128, :], SCA[:, cas, :], ['SCA%d' % cas], [], 'cao%d' % cas)
                    pc = nxt('pc', 2)
                    for a in range(2):
                        trb(PCb[:, pc, a * 128:(a + 1) * 128], SCA[:, cas, a * 128:(a + 1) * 128], ['SCA%d' % cas, 'C'], ['PC%d' % pc])
                    act(SCT[:, hs, :, sub * 128:(sub + 1) * 128], PCb[:, pc, 0:256].rearrange("p (a c) -> p a c", c=128), AF.Copy, ['PC%d' % pc], ['SCT%d' % hs])
                dma('sp', CTs[:, :, i * 512:(i + 1) * 512].rearrange("a p c -> p a c"), SCT[:, hs], ['SCT%d' % hs], [], 'cto%d' % hs)

            def mk_norm(i):
                hs_ = i % 2
                return lambda sub: norm_sub(st, lambda s_: xk[i * 512 + s_ * 128:i * 512 + (s_ + 1) * 128, :], G1B, HT[:, hs_], 'HT%d' % hs_, sub)
            n0 = mk_norm(0)
            for sub in range(4):
                n0(sub)
            for i in range(NCH):
                nn = mk_norm(i + 1) if i + 1 < NCH else None
                proj_tile(i, nn if nn else (lambda sub: None))
                if nn:
                    nn(3)
            P.flush()


    def hank(t, off):
        return bass.AP(tensor=t.tensor, offset=off, ap=[[1, 128], [1, 512]])

    class Ctr:
        def __init__(self):
            self.c = {}

        def __call__(self, k, n):
            v = self.c.get(k, 0)
            self.c[k] = v + 1
            return v % n

    def stage_Q(m):
        with ExitStack() as s:
            st = dict(XS=alloc(s, "qXS", [128, 2, D], F32), XB=alloc(s, "qXB", [128, D], BF16), ST=alloc(s, "qST", [128, 8], F32),
                      PB=palloc(s, "qPB", [128, 2, 512]))
            PA = palloc(s, "qPA", [128, 2, 512]); PC = palloc(s, "qPC", [128, 2, 512])
            HT = alloc(s, "qHT", [128, 16, 512], BF16)
            W1 = alloc(s, "qW1", [128, 3, 16, 128], BF16)
            SK = alloc(s, "qSK", [128, 2, 512], BF16); AQ = alloc(s, "qAQ", [128, 2, 512], BF16)
            WK = alloc(s, "qWK", [128, 2, 256], BF16); QLS = alloc(s, "qQLS", [128, 2, 2, 512], BF16)
            AW = alloc(s, "qAW", [128, 2, 16], F32)
            nx = Ctr()
            norm_T(st, lambda sub: xq[m * 512 + sub * 128:m * 512 + (sub + 1) * 128, :], G1B, HT, 'HT', 'q')

            def proj(blk):
                ws = nx('w1', 3)
                dma('sp', W1[:, ws], WB128[blk], [], ['W1_%d' % ws], 'w1_%d' % ws)
                pa = nx('pa', 2)
                for k in range(16):
                    mm(PA[:, pa, :], W1[:, ws, k, :], HT[:, k, :], k == 0, k == 15, ['W1_%d' % ws, 'HT'], ['PA%d' % pa])
                return pa
            for h in range(8):
                pa = proj(17 + h)
                sk = nx('sk', 2)
                act(SK[:, sk, :], PA[:, pa, :], AF.Copy, ['PA%d' % pa], ['SK%d' % sk])
                dma('sp', BQTs[h], SK[:, sk, :], ['SK%d' % sk], [], 'sko%d' % sk)
            for hp in range(8):
                pa = proj(8 + hp)
                sk = nx('sk', 2)
                act(SK[:, sk, :], PA[:, pa, :], AF.Copy, ['PA%d' % pa], ['SK%d' % sk])
                dma('sp', QITs[hp], SK[:, sk, :], ['SK%d' % sk], [], 'sko%d' % sk)
            for h in range(8):
                pa = proj(h)
                aq = nx('aq', 2)
                act(AQ[:, aq, :], PA[:, pa, :], AF.Copy, ['PA%d' % pa], ['AQ%d' % aq])
                wk = nx('wk', 2)
                dma('sp', WK[:, wk, :], WUK[h], [], ['WK%d' % wk], 'wk%d' % wk)
                qs = nx('ql', 2)
                for a in range(2):
                    pc = nx('pc', 2)
                    mm(PC[:, pc, :], WK[:, wk, a * 128:(a + 1) * 128], AQ[:, aq, :], True, True, ['WK%d' % wk, 'AQ%d' % aq], ['PC%d' % pc])
                    act(QLS[:, qs, a, :], PC[:, pc, :], AF.Copy, ['PC%d' % pc], ['QLS%d' % qs])
                dma('sp', QLTs[h], QLS[:, qs], ['QLS%d' % qs], [], 'qlo%d' % qs)
            ws = nx('w1', 3)
            dma('sp', W1[:, ws], WB128[34], [], ['W1_%d' % ws], 'w1_%d' % ws)
            for sub in range(4):
                pa = nx('pa', 2)
                for k in range(16):
                    mm(PA[:, pa, 0:16], HT[:, k, sub * 128:(sub + 1) * 128], W1[:, ws, k, 0:16], k == 0, k == 15, ['W1_%d' % ws, 'HT'], ['PA%d' % pa])
                aw = nx('aw', 2)
                act(AW[:, aw, :], PA[:, pa, 0:16], AF.Copy, ['PA%d' % pa], ['AW%d' % aw])
                dma('sp', AIWs[sub * 128:(sub + 1) * 128, :], AW[:, aw, :], ['AW%d' % aw], [], 'awo%d' % aw)
            P.flush()

    def stage_IF(m):
        nch, nkb, kb0 = G['nch'][m], G['nkb'][m], G['kb0'][m]
        n = nch * 512
        if nch <= 4:
            RD_ = 0
        else:
            mu = 256.0 / nch
            RD_ = int(math.ceil((mu + 8.0 * math.sqrt(mu)) / 8.0)) + 1
        with ExitStack() as s:
            SC = alloc(s, "iSC", [128, 2, L], F32)
            CAND = alloc(s, "iCAND", [128, max(NKB * 8, nch * 8 * RD_)], F32); WKb = alloc(s, "iWK", [128, max(512, NKB * 8)], F32)
            WK = WKb[:, 0:512]
            QI = alloc(s, "iQI", [128, 8, 512], BF16); AWt = alloc(s, "iAW", [128, 4, 16], F32)
            DW = alloc(s, "iDW", [128, 2, 16, 128], BF16); KI = alloc(s, "iKI", [128, 3, 512], BF16)
            RR = alloc(s, "iRR", [128, 4, 512], BF16); HI = alloc(s, "iHI", [128, 2, 512], BF16)
            M8 = alloc(s, "iM8", [128, 8], F32); NSB = alloc(s, "iNSB", [128, 2, 512], BF16)
            BQ = alloc(s, "fBQ", [128, 2, 512], BF16); KTc = alloc(s, "fKT", [128, 3, 512], BF16)
            VAc = alloc(s, "fVA", [128, 3, 4, 130], BF16); PTB = alloc(s, "fPT", [128, 3, 512], BF16)
            HF = alloc(s, "fHF", [128, 2, 512], BF16); FB = alloc(s, "fFB", [128, 8, NKB], F32)
            OHs = WKb[:, 0:NKB * 8]; TMP = CAND[:, 0:NKB * 8]
            CR = alloc(s, "fCR", [128, 16], F32); OL = alloc(s, "fOL", [128, 2, 128], BF16)
            MX = alloc(s, "fMX", [128, 2, 512], BF16); LD = alloc(s, "fLD", [128, 4], F32)
            PD = palloc(s, "iPD", [128, 2, 512]); PSc = palloc(s, "iPS", [128, 2, 512]); ACC = palloc(s, "iACC", [128, 4, 512])
            PSb = PSc[:].bitcast(BF16)
            nx = Ctr()
            dma('sp', QI[:], QITs.rearrange("h p c -> p h c"), [], ['QI'], 'qi')
            dma('sp', AWt[:], AIWs.rearrange("(g p) c -> p g c", p=128), [], ['AWt'], 'awt')
            dma('sp', OHs, ohk[m], [], ['WK'], 'oh')
            vop('dve', 'tensor_tensor', ['WK'], ['CAND'], out=TMP, in0=CUMK[:].rearrange("p k h -> p (k h)"), in1=OHs, op=ALU.mult)
            vop('dve', 'tensor_reduce', ['CAND'], ['CR0'], out=CR[:, 0:8], in_=TMP.rearrange("p (k h) -> p h k", h=8), axis=AX.X, op=ALU.add)
            mm(PSc[:, 0, 0:8], ONF[:], CR[:, 0:8], True, True, ['CR0'], ['PS0'])
            act(CR[:, 8:16], PSc[:, 0, 0:8], AF.Copy, ['PS0'], ['CR1'])
            for h in range(8):
                vop('dve', 'tensor_scalar', ['CR1'], ['FB'], out=FB[:, h, 0:nkb], in0=CUMK[:, 0:nkb, h], scalar1=-1.0, scalar2=CR[:, 8 + h:9 + h],
                    op0=ALU.mult, op1=ALU.add)

            def fox_head(h):
                bs = nx('bq', 2)
                dma('sp', BQ[:, bs, :], BQTs[h], [], ['BQ%d' % bs], 'bq%d' % bs)
                pend = None

                def pv(ps, ks, kbl, kb):
                    for g in range(4):
                        mm(ACC[:, g, 0:129], PTB[:, ps, g * 128:(g + 1) * 128], VAc[:, ks, kbl, 0:129], kb == 0, kb == nkb - 1,
                           ['PT%d' % ps, 'VA%d' % ks], ['ACC'])
                for c in range(nch):
                    ks = nx('kt', 3)
                    dma('sp', KTc[:, ks, :], KTs[h][:, c * 512:(c + 1) * 512], [], ['KT%d' % ks], 'kt%d' % ks)
                    dma('sp', VAc[:, ks], VAs[h][c * 512:(c + 1) * 512, :].rearrange("(k p) c -> p k c", p=128), [], ['VA%d' % ks], 'va%d' % ks)
                    for kbl in range(4):
                        kb = 4 * c + kbl
                        near = kb >= kb0
                        pd = nx('pd', 2)
                        mm(PD[:, pd, :], KTc[:, ks, kbl * 128:(kbl + 1) * 128], BQ[:, bs, :], True, not near, ['KT%d' % ks, 'BQ%d' % bs], ['PD%d' % pd])
                        if near:
                            hs = nx('hf', 2)
                            dma('sp', HF[:, hs, :], hank(hank_fox, (m * NU + kb - kb0) * 640), [], ['HF%d' % hs], 'hf%d' % hs)
                            mm(PD[:, pd, :], ANB[:], HF[:, hs, :], False, True, ['HF%d' % hs], ['PD%d' % pd])
                        ps = nx('pt', 3)
                        act(PTB[:, ps, :], PD[:, pd, :], AF.Exp, ['PD%d' % pd, 'FB'], ['PT%d' % ps], bias=FB[:, h, kb:kb + 1], scale=SCALE)
                        if pend is not None:
                            pv(*pend)
                        pend = (ps, ks, kbl, kb)
                pv(*pend)
                ms = nx('mx', 2)
                for g in range(4):
                    act(LD[:, 0:1], ACC[:, g, 128:129], AF.Ln, ['ACC'], ['LD'], bias=TINY)
                    act(LD[:, 1:2], LD[:, 0:1], AF.Exp, ['LD'], ['RD'], scale=-1.0)
                    os_ = nx('ol', 2)
                    act(OL[:, os_, :], ACC[:, g, 0:128], AF.Copy, ['ACC', 'RD'], ['OL%d' % os_], scale=LD[:, 1:2])
                    p2 = nx('ps', 2)
                    trb(PSb[:, p2, 0:128], OL[:, os_, :], ['OL%d' % os_], ['PS%d' % p2])
                    act(MX[:, ms, g * 128:(g + 1) * 128], PSb[:, p2, 0:128], AF.Copy, ['PS%d' % p2], ['MX%d' % ms])
                dma('sp', MIXs[8 + h], MX[:, ms, :], ['MX%d' % ms], [], 'mxo%d' % ms)

            for g in range(4):
                ds = nx('dw', 2)
                sg = g % 2
                SCg = SC[:, sg, :]
                sck = 'SC%d' % sg
                for h in range(16):
                    vop('pool', 'tensor_scalar', ['AWt'], ['DW%d' % ds], out=DW[:, ds, h, :], in0=IDB[:], scalar1=AWt[:, g, h:h + 1], scalar2=None, op0=ALU.mult)
                pend = None

                def fin(c, h, rs, p2, ki):
                    masked = c >= G['c0'][m][g]
                    mm(PSc[:, p2, :], DW[:, ds, h, :], RR[:, rs, :], h == 0, (h == 15 and not masked), ['DW%d' % ds, 'RR%d' % rs], ['PS%d' % p2])
                    if h == 15:
                        if masked:
                            hs = nx('hi', 2)
                            dma('sp', HI[:, hs, :], hank(hank_idx, ((m * 4 + g) * NMC + c - G['c0'][m][g]) * 640), [], ['HI%d' % hs], 'hi%d' % hs)
                            mm(PSc[:, p2, :], ANB[:], HI[:, hs, :], False, True, ['HI%d' % hs], ['PS%d' % p2])
                        ck = '%s_%d' % (sck, c)
                        act(SCg[:, c * 512:(c + 1) * 512], PSc[:, p2, :], AF.Copy, ['PS%d' % p2], [ck])
                        if RD_ > 0:
                            for j in range(RD_):
                                cand = CAND[:, c * 8 * RD_ + 8 * j:c * 8 * RD_ + 8 * j + 8]
                                srcv = SCg[:, c * 512:(c + 1) * 512] if j == 0 else WK
                                vop('dve', 'max', [ck, 'WK'], ['CAND'], out=cand, in_=srcv)
                                if j < RD_ - 1:
                                    vop('dve', 'match_replace', [ck, 'WK', 'CAND'], ['WK'], out=WK, in_to_replace=cand, in_values=srcv, imm_value=-3.0e38)
                for c in range(nch):
                    ki = nx('ki', 3)
                    dma('sp', KI[:, ki, :], KITs[:, c * 512:(c + 1) * 512], [], ['KI%d' % ki], 'ki%d' % ki)
                    p2 = nx('ps', 2)
                    for h in range(16):
                        hp, half = h // 2, h % 2
                        pd = nx('pd', 2)
                        mm(PD[:, pd, :], QI[half * 64:(half + 1) * 64, hp, g * 128:(g + 1) * 128], KI[half * 64:(half + 1) * 64, ki, :], True, True,
                           ['QI', 'KI%d' % ki], ['PD%d' % pd])
                        rs = nx('rr', 4)
                        act(RR[:, rs, :], PD[:, pd, :], AF.Relu, ['PD%d' % pd], ['RR%d' % rs])
                        if pend is not None:
                            fin(*pend)
                        pend = (c, h, rs, p2, ki)
                fin(*pend)
                fox_head(2 * g)
                fox_head(2 * g + 1)
                allk = ['%s_%d' % (sck, c) for c in range(nch)]
                if RD_ == 0:
                    for r in range(TOPK // 8):
                        vop('dve', 'max', allk, ['M8'], out=M8[:], in_=SCg[:, 0:n])
                        vop('dve', 'match_replace', allk + ['M8'], allk, out=SCg[:, 0:n], in_to_replace=M8[:], in_values=SCg[:, 0:n], imm_value=-3.0e38)
                else:
                    ncand = nch * 8 * RD_
                    for r in range(TOPK // 8):
                        vop('dve', 'max', ['CAND'], ['M8'], out=M8[:], in_=CAND[:, 0:ncand])
                        if r < TOPK // 8 - 1:
                            vop('dve', 'match_replace', ['CAND', 'M8'], ['CAND'], out=CAND[:, 0:ncand], in_to_replace=M8[:], in_values=CAND[:, 0:ncand], imm_value=-3.0e38)
                for c2 in range(0, n, 512):
                    w_ = min(512, n - c2)
                    ns = nx('nsb', 2)
                    if RD_ == 0:
                        vop('dve', 'tensor_scalar', allk, ['NSB%d' % ns], out=NSB[:, ns, 0:w_], in0=SCg[:, c2:c2 + w_], scalar1=-1.0e35, scalar2=NEGBIG,
                            op0=ALU.is_gt, op1=ALU.mult)
                    else:
                        vop('dve', 'tensor_scalar', allk + ['M8'], ['NSB%d' % ns], out=NSB[:, ns, 0:w_], in0=SCg[:, c2:c2 + w_], scalar1=M8[:, 7:8], scalar2=NEGBIG,
                            op0=ALU.is_lt, op1=ALU.mult)
                    dma('sp', NSs[g][:, c2:c2 + w_], NSB[:, ns, 0:w_], ['NSB%d' % ns], [], 'nso%d' % ns)
            P.flush()

    def stage_D(m):
        nch, nkb, kb0 = G['nch'][m], G['nkb'][m], G['kb0'][m]
        with ExitStack() as s:
            QL = alloc(s, "dQL", [128, 2, 2, 512], BF16); CTc = alloc(s, "dCT", [128, 3, 2, 512], BF16)
            CAc = alloc(s, "dCA", [128, 3, 4, 258], BF16); NSC = alloc(s, "dNS", [128, 3, 4, 512], BF16)
            H5f = alloc(s, "dH5f", [128, 2, 512], F32); H5b = alloc(s, "dH5b", [128, 2, 512], BF16)
            PTB = alloc(s, "dPT", [128, 3, 512], BF16); OLa = alloc(s, "dOL", [128, 2, 256], BF16)
            OLT = alloc(s, "dOLT", [128, 2, 512], BF16); WV = alloc(s, "dWV", [128, 2, 2, 128], BF16)
            MX = alloc(s, "dMX", [128, 2, 512], BF16); LD = alloc(s, "dLD", [128, 4], F32)
            PD = palloc(s, "dPD", [128, 2, 512]); PSc = palloc(s, "dPS", [128, 2, 512]); ACC = palloc(s, "dACC", [128, 4, 512])
            PSb = PSc[:].bitcast(BF16)
            nx = Ctr()
            for h in range(8):
                qs = nx('ql', 2)
                dma('sp', QL[:, qs], QLTs[h], [], ['QL%d' % qs], 'ql%d' % qs)
                wv = nx('wv', 2)
                dma('sp', WV[:, wv], WUV[h].rearrange("(a p) d -> p a d", p=128), [], ['WV%d' % wv], 'wv%d' % wv)
                pend = None

                def pvd(ps, cs, kbl, kb):
                    for g in range(4):
                        mm(ACC[:, g, 0:257], PTB[:, ps, g * 128:(g + 1) * 128], CAc[:, cs, kbl, 0:257], kb == 0, kb == nkb - 1,
                           ['PT%d' % ps, 'CA%d' % cs], ['ACC'])
                for c in range(nch):
                    cs = nx('ct', 3)
                    dma('sp', CTc[:, cs], CTs[:, :, c * 512:(c + 1) * 512].rearrange("a p c -> p a c"), [], ['CT%d' % cs], 'ct%d' % cs)
                    dma('sp', CAc[:, cs], CAs[c * 512:(c + 1) * 512, :].rearrange("(k p) c -> p k c", p=128), [], ['CA%d' % cs], 'ca%d' % cs)
                    dma('sp', NSC[:, cs], NSs[:, :, c * 512:(c + 1) * 512].rearrange("g q c -> q g c"), [], ['NS%d' % cs], 'ns%d' % cs)
                    for kbl in range(4):
                        kb = 4 * c + kbl
                        near = kb >= kb0
                        pd = nx('pd', 2)
                        ksl = slice(kbl * 128, (kbl + 1) * 128)
                        mm(PD[:, pd, :], CTc[:, cs, 0, ksl], QL[:, qs, 0, :], True, False, ['CT%d' % cs, 'QL%d' % qs], ['PD%d' % pd])
                        mm(PD[:, pd, :], CTc[:, cs, 1, ksl], QL[:, qs, 1, :], False, False, ['CT%d' % cs, 'QL%d' % qs], ['PD%d' % pd])
                        for g in range(4):
                            mm(PD[:, pd, g * 128:(g + 1) * 128], NSC[:, cs, g, ksl], IDB[:], False, (g == 3 and not near), ['NS%d' % cs], ['PD%d' % pd], skip=True)
                        if near:
                            hs = nx('h5', 2)
                            dma('sp', H5f[:, hs, :], hank(hank_t5, ((m * NU + kb - kb0) * 8 + h) * 640), [], ['H5f%d' % hs], 'h5f%d' % hs)
                            vop('pool', 'tensor_scalar', ['H5f%d' % hs], ['H5b%d' % hs], out=H5b[:, hs, :], in0=H5f[:, hs, :], scalar1=1.0 / SCALE, scalar2=None, op0=ALU.mult)
                            mm(PD[:, pd, :], ANB[:], H5b[:, hs, :], False, True, ['H5b%d' % hs], ['PD%d' % pd], skip=True)
                        ps = nx('pt', 3)
                        act(PTB[:, ps, :], PD[:, pd, :], AF.Exp, ['PD%d' % pd], ['PT%d' % ps], bias=(ZERO if near else B31[:, h:h + 1]), scale=SCALE)
                        if pend is not None:
                            pvd(*pend)
                        pend = (ps, cs, kbl, kb)
                pvd(*pend)
                for g in range(4):
                    act(LD[:, 0:1], ACC[:, g, 256:257], AF.Ln, ['ACC'], ['LD'], bias=TINY)
                    act(LD[:, 1:2], LD[:, 0:1], AF.Exp, ['LD'], ['RD'], scale=-1.0)
                    os_ = nx('ol', 2)
                    act(OLa[:, os_, :], ACC[:, g, 0:256], AF.Copy, ['ACC', 'RD'], ['OL%d' % os_], scale=LD[:, 1:2])
                    p2 = nx('ps', 2)
                    for a in range(2):
                        trb(PSb[:, p2, a * 128:(a + 1) * 128], OLa[:, os_, a * 128:(a + 1) * 128], ['OL%d' % os_], ['PS%d' % p2])
                    act(OLT[:, :, g * 128:(g + 1) * 128], PSb[:, p2, 0:256].rearrange("p (a c) -> p a c", c=128), AF.Copy, ['PS%d' % p2], ['OLT'])
                p2 = nx('ps', 2)
                for a in range(2):
                    mm(PSc[:, p2, :], WV[:, wv, a, :], OLT[:, a, :], a == 0, a == 1, ['WV%d' % wv, 'OLT'], ['PS%d' % p2])
                ms = nx('mx', 2)
                act(MX[:, ms, :], PSc[:, p2, :], AF.Copy, ['PS%d' % p2], ['MX%d' % ms])
                dma('sp', MIXs[h], MX[:, ms, :], ['MX%d' % ms], [], 'mxo%d' % ms)
            P.flush()

    def stage_F(m):
        with ExitStack() as s:
            st = dict(XB=alloc(s, "oXB", [128, D], BF16), ST=alloc(s, "oST", [128, 8], F32), PB=palloc(s, "oPB", [128, 2, 512]))
            PA = palloc(s, "oPA", [128, 2, 512]); ACC = palloc(s, "oACC", [128, 4, 512])
            MT = alloc(s, "oMT", [128, 16, 512], BF16); W2 = alloc(s, "oW2", [128, 2, 16, 256], BF16)
            XM = alloc(s, "oXM", [128, 4, D], F32); H2 = alloc(s, "oH2", [128, 16, 512], BF16)
            W1 = alloc(s, "oW1", [128, 4, 16, 128], BF16)
            GA = alloc(s, "oGA", [128, 512], F32); VV = alloc(s, "oVV", [128, 512], F32); SG = alloc(s, "oSG", [128, 512], F32)
            AT = alloc(s, "oAT", [128, NFB, 512], BF16); WD = alloc(s, "oWD", [128, 3, 512], BF16)
            YO = alloc(s, "oYO", [128, 2, D], F32); GF = alloc(s, "oGF", [128, D], F32)
            nx = Ctr()
            PBb = st['PB'][:].bitcast(BF16)
            dma('sp', MT[:], MIXs.rearrange("k p c -> p k c"), [], ['MT'], 'mt')
            dma('sp', GF[:], gfb, [], ['GF'], 'gf')
            for sub in range(4):
                dma('sp', XM[:, sub, :], xq[m * 512 + sub * 128:m * 512 + (sub + 1) * 128, :], [], ['XM%d' % sub], 'xm%d' % sub)
            vop('dve', 'memset', [], ['AT'], AT[:, :, 0:2], 0.0)
            for oc in range(8):
                ws = nx('w2', 2)
                dma('sp', W2[:, ws], WB256[5 + oc], [], ['W2_%d' % ws], 'w2_%d' % ws)
                for sub in range(4):
                    for k in range(16):
                        mm(ACC[:, sub, 0:256], MT[:, k, sub * 128:(sub + 1) * 128], W2[:, ws, k, :], k == 0, k == 15, ['MT', 'W2_%d' % ws], ['ACC%d' % sub])
                    vop('dve', 'tensor_tensor', ['ACC%d' % sub, 'XM%d' % sub], ['XM%d' % sub], out=XM[:, sub, oc * 256:(oc + 1) * 256], in0=ACC[:, sub, 0:256],
                        in1=XM[:, sub, oc * 256:(oc + 1) * 256], op=ALU.add)
            XB, ST = st['XB'], st['ST']
            for sub in range(4):
                xs = XM[:, sub, :]
                act(XB[:], xs, AF.Square, ['XM%d' % sub], ['XB', 'SSQ'], accum=ST[:, 0:1])
                act(ST[:, 1:2], ST[:, 0:1], AF.Sqrt, ['SSQ'], ['SQ'], bias=EPS, scale=1.0 / D)
                vop('dve', 'reciprocal', ['SQ'], ['RSTD'], out=ST[:, 2:3], in_=ST[:, 1:2])
                act(XB[:], xs, AF.Copy, ['XM%d' % sub, 'RSTD'], ['XB'], scale=ST[:, 2:3])
                for k in range(16):
                    trb(PBb[:, k // 8, (k % 8) * 128:(k % 8 + 1) * 128], XB[:, k * 128:(k + 1) * 128], ['XB'], ['PB'])
                vop('dve', 'tensor_tensor', ['PB'], ['H2'], out=H2[:, :, sub * 128:(sub + 1) * 128],
                    in0=PBb.rearrange("p a (k c) -> p (a k) c", c=128), in1=G2B[:], op=ALU.mult)
            for fb in range(NFB):
                for which, dst in ((0, GA), (1, VV)):
                    blk = fb + which * NFB
                    ws = nx('w1', 4)
                    dma('sp', W1[:, ws], WBUP[blk], [], ['W1_%d' % ws], 'w1_%d' % ws)
                    pa = nx('pa', 2)
                    for k in range(16):
                        mm(PA[:, pa, :], W1[:, ws, k, :], H2[:, k, :], k == 0, k == 15, ['W1_%d' % ws, 'H2'], ['PA%d' % pa])
                    key = 'GA' if which == 0 else 'VV'
                    act(dst[:, 2:512], PA[:, pa, 2:512], AF.Identity, ['PA%d' % pa], [key], bias=CB[:, blk:blk + 1], scale=CW[:, blk, 2:3])
                    vop('dve', 'scalar_tensor_tensor', ['PA%d' % pa, key], [key], out=dst[:, 2:512], in0=PA[:, pa, 1:511], scalar=CW[:, blk, 1:2],
                        in1=dst[:, 2:512], op0=ALU.mult, op1=ALU.add)
                    vop('dve', 'scalar_tensor_tensor', ['PA%d' % pa, key], [key], out=dst[:, 2:512], in0=PA[:, pa, 0:510], scalar=CW[:, blk, 0:1],
                        in1=dst[:, 2:512], op0=ALU.mult, op1=ALU.add)
                act(SG[:, 2:512], GA[:, 2:512], AF.Silu, ['GA'], ['SG'])
                vop('dve', 'tensor_tensor', ['SG', 'VV', 'AT'], ['AT%d' % fb], out=AT[:, fb, 2:512], in0=SG[:, 2:512], in1=VV[:, 2:512], op=ALU.mult)
            for oc in range(4):
                for fb in range(NFB):
                    ws = nx('wd', 3)
                    dma('sp', WD[:, ws, :], WBDN[fb * 128:(fb + 1) * 128, oc * 512:(oc + 1) * 512], [], ['WD%d' % ws], 'wd%d' % ws)
                    for sub in range(4):
                        mm(ACC[:, sub, :], AT[:, fb, sub * 128:(sub + 1) * 128], WD[:, ws, :], fb == 0, fb == NFB - 1, ['AT%d' % fb, 'AT', 'WD%d' % ws], ['ACC%d' % sub])
                for sub in range(4):
                    vop('dve', 'tensor_tensor', ['ACC%d' % sub, 'XM%d' % sub], ['XM%d' % sub], out=XM[:, sub, oc * 512:(oc + 1) * 512], in0=ACC[:, sub, :],
                        in1=XM[:, sub, oc * 512:(oc + 1) * 512], op=ALU.add)
            for sub in range(4):
                xs = XM[:, sub, :]
                ys = nx('yo', 2)
                act(YO[:, ys, :], xs, AF.Square, ['XM%d' % sub], ['YO%d' % ys, 'SSQ'], accum=ST[:, 0:1])
                act(ST[:, 1:2], ST[:, 0:1], AF.Sqrt, ['SSQ'], ['SQ'], bias=EPS, scale=1.0 / D)
                vop('dve', 'reciprocal', ['SQ'], ['RSTD'], out=ST[:, 2:3], in_=ST[:, 1:2])
                vop('dve', 'scalar_tensor_tensor', ['XM%d' % sub, 'RSTD', 'GF'], ['YO%d' % ys], out=YO[:, ys, :], in0=xs, scalar=ST[:, 2:3], in1=GF[:],
                    op0=ALU.mult, op1=ALU.mult)
                dma('sp', out[m * 512 + sub * 128:m * 512 + (sub + 1) * 128, :], YO[:, ys, :], ['YO%d' % ys], [], 'yoo%d' % ys)
            P.flush()

    stage_consts()
    stage_K()
    for m in range(NT):
        stage_Q(m)
        stage_IF(m)
        stage_D(m)
        stage_F(m)
    es.close()
    return nc, G


def t5_bucket_np(n):
    n = np.asarray(n)
    max_exact = 16
    nf = np.maximum(n, 1).astype(np.float32)
    large = max_exact + (np.log(nf / np.float32(max_exact)) / np.float32(math.log(128 / max_exact)) * np.float32(32 - max_exact)).astype(np.int32)
    large = np.minimum(large, 31)
    return np.where(n < max_exact, n, large)


def host_prep(inputs, L, NT, G):
    bf = ml_dtypes.bfloat16
    x = np.asarray(inputs['x'], np.float32)
    NKB = L // 128
    NU, NMC = G['NU'], G['NMC']
    t5 = np.asarray(inputs['t5_table'], np.float32)
    common = {
        'w_in': np.ascontiguousarray(inputs['w_in'][0]), 'w_o': np.ascontiguousarray(inputs['w_o'][0]),
        'w_up': np.ascontiguousarray(inputs['w_up'][0]), 'w_down': np.ascontiguousarray(inputs['w_down'][0]),
        'w_uk': np.ascontiguousarray(inputs['w_uk'][0]), 'w_uv': np.ascontiguousarray(inputs['w_uv'][0]),
        'g1T': np.ascontiguousarray(np.asarray(inputs['norm1_g'][0]).reshape(16, 128).T),
        'g2T': np.ascontiguousarray(np.asarray(inputs['norm2_g'][0]).reshape(16, 128).T),
        'gkvb': np.ascontiguousarray(np.broadcast_to(np.asarray(inputs['kv_norm_g'][0])[None, :], (128, 256))),
        'fgb': np.ascontiguousarray(np.asarray(inputs['fgate_b'][0]).reshape(8, 1)),
        'convw': np.ascontiguousarray(np.asarray(inputs['conv_w'][0]).reshape(3, 88, 128).transpose(2, 1, 0)),
        'convb': np.ascontiguousarray(np.asarray(inputs['conv_b'][0]).reshape(88, 128).T),
        'gfb': np.ascontiguousarray(np.broadcast_to(np.asarray(inputs['final_g'])[None, :], (128, D))),
        'b31': np.ascontiguousarray(np.broadcast_to(t5[31][None, :], (128, 8))),
        'identb': np.eye(128, dtype=np.float32).astype(bf), 'antib': np.eye(128, dtype=np.float32)[::-1].copy().astype(bf),
        'identf': np.eye(128, dtype=np.float32), 'onesf': np.ones((128, 128), np.float32),
    }
    common = {k: np.asarray(v, np.float32) if v.dtype not in (bf,) else v for k, v in common.items()}
    v = np.arange(640)
    in_maps = []
    for c in range(8):
        b, j = c // 4, c % 4
        xqc = np.zeros((NT * 512, D), np.float32)
        hf = np.zeros((NT, NU, 640), np.float32)
        ht = np.zeros((NT, NU, 8, 640), np.float32)
        hi = np.zeros((NT, 4, NMC, 640), np.float32)
        oh = np.zeros((NT, 128, NKB, 8), np.float32)
        for m in range(NT):
            p0 = 510 * (4 * m + j) - 2
            pos = p0 + np.arange(512)
            ok = (pos >= 0) & (pos < L)
            xqc[m * 512:(m + 1) * 512][ok] = x[b, pos[ok]]
            pref = min(max(p0 + 256, 0), L - 1)
            oh[m, pref % 128, pref // 128, :] = 1.0
            for i in range(G['nu'][m]):
                kb = G['kb0'][m] + i
                dist = v - 127 + (p0 - 128 * kb)
                hf[m, i] = np.where(dist >= 0, 0.0, NEGBIG)
                bk = t5_bucket_np(np.maximum(dist, 0))
                ht[m, i] = np.where(dist[None, :] >= 0, t5[bk].T, NEGBIG)
            for g in range(4):
                for ci in range(G['nch'][m] - G['c0'][m][g]):
                    cc = G['c0'][m][g] + ci
                    dist = 127 - v + (p0 + 128 * g - 512 * cc)
                    hi[m, g, ci] = np.where(dist >= 0, 0.0, -1e30)
        d = dict(common)
        d.update({'xk': np.ascontiguousarray(x[b]), 'xq': xqc, 'hank_fox': hf.astype(bf), 'hank_t5': ht,
                  'hank_idx': hi.astype(bf), 'ohk': oh.reshape(NT, 128, NKB * 8)})
        in_maps.append(d)
    return in_maps


_CACHE = {}


def kernel(**inputs):
    x = np.asarray(inputs['x'])
    B, L, _ = x.shape
    ntile = (L + 2 + 509) // 510
    NT = (ntile + 3) // 4
    key = (L, NT)
    if key not in _CACHE:
        _CACHE[key] = build(L, NT)
    nc, G = _CACHE[key]
    in_maps = host_prep(inputs, L, NT, G)
    res = run_bass_kernel_spmd(nc, in_maps, core_ids=list(range(8)))
    if DEBUG:
        return res
    outp = np.zeros((B, L, D), np.float32)
    for c in range(8):
        b, j = c // 4, c % 4
        o = res.results[c]['out']
        for m in range(NT):
            p0 = 510 * (4 * m + j) - 2
            pos = p0 + np.arange(2, 512)
            ok = (pos >= 0) & (pos < L)
            outp[b, pos[ok]] = o[m * 512 + 2:(m + 1) * 512][ok]
    return outp
```
